# Optimizing a Trainium2 kernel written in Bass

```python
import math
import jax, jax.numpy as jnp
from jax import lax
import numpy as np

D_MODEL = 2048
BATCH = 1
SEQ = 16384
DEPTH = 2

HEAD_DIM = 64
MIX_WIDTH = D_MODEL
A_WIDTH = MIX_WIDTH // 4
B_WIDTH = MIX_WIDTH // 4
C_WIDTH = MIX_WIDTH // 2
A_HEADS = A_WIDTH // HEAD_DIM
B_HEADS = B_WIDTH // HEAD_DIM
C_VDIM = 2 * HEAD_DIM
C_HEADS = C_WIDTH // C_VDIM
IN_WIDTH = 3 * A_WIDTH + 3 * B_WIDTH + 3 * C_WIDTH
A_PATTERNS = ((128, 1), (512, 4), (2048, 16))
A_BLOCK = 128
MOBA_BLOCK = 256
MOBA_TOPK = 3
QBLOCK = 128
ROPE_THETA = 10000.0
N_EXPERTS = 16
N_GROUPS = 4
EXPERTS_PER_GROUP = N_EXPERTS // N_GROUPS
TOP_K = 2
D_EXPERT = D_MODEL // 2
RMS_EPS = 1e-6

kernel_name = "hybrid_dilated_moba_diffattn_groupmoe"


def _rms_norm(x, g):
    xf = x.astype(jnp.float32)
    y = xf * lax.rsqrt(jnp.mean(xf * xf, axis=-1, keepdims=True) + RMS_EPS)
    return (y * g.astype(jnp.float32)).astype(x.dtype)


def _rope_tables(positions):
    inv = ROPE_THETA ** (-jnp.arange(0, HEAD_DIM, 2, dtype=jnp.float32) / HEAD_DIM)
    ang = positions.astype(jnp.float32)[..., None] * inv
    return jnp.cos(ang)[:, :, None, :], jnp.sin(ang)[:, :, None, :]


def _rope(x, cos, sin):
    xf = x.astype(jnp.float32)
    x1, x2 = xf[..., : HEAD_DIM // 2], xf[..., HEAD_DIM // 2:]
    return jnp.concatenate([x1 * cos - x2 * sin, x2 * cos + x1 * sin], axis=-1).astype(x.dtype)


def _banded_window_attention(q, k, v, span):
    L, hd = q.shape[-2], q.shape[-1]
    lead = q.shape[:-2]
    nb = -(-L // A_BLOCK)
    pad = nb * A_BLOCK - L
    qb = jnp.pad(q, [(0, 0)] * len(lead) + [(0, pad), (0, 0)]).reshape(*lead, nb, A_BLOCK, hd)

    def band(t):
        tb = jnp.pad(t, [(0, 0)] * len(lead) + [(A_BLOCK, pad), (0, 0)])
        tb = tb.reshape(*lead, nb + 1, A_BLOCK, t.shape[-1])
        return jnp.concatenate([tb[..., :-1, :, :], tb[..., 1:, :, :]], axis=-2)

    kb, vb = band(k), band(v)
    s = jnp.einsum('...nqd,...nkd->...nqk', qb, kb).astype(jnp.float32) * (hd ** -0.5)
    dist = A_BLOCK + jnp.arange(A_BLOCK)[:, None] - jnp.arange(2 * A_BLOCK)[None, :]
    kpos = (jnp.arange(nb)[:, None, None] - 1) * A_BLOCK + jnp.arange(2 * A_BLOCK)[None, None, :]
    mask = (dist >= 0) & (dist <= span) & (kpos >= 0)
    s = jnp.where(mask, s, -jnp.inf)
    m = jnp.max(s, axis=-1, keepdims=True)
    p = jnp.exp(s - m)
    den = jnp.sum(p, axis=-1)
    o = jnp.einsum('...nqk,...nkd->...nqd', p.astype(v.dtype), vb).astype(jnp.float32) / den[..., None]
    lse = m[..., 0] + jnp.log(den)
    o = o.reshape(*lead, nb * A_BLOCK, v.shape[-1])[..., :L, :]
    lse = lse.reshape(*lead, nb * A_BLOCK)[..., :L]
    return o, lse


def _dilated_attention(q, k, v):
    B, H, S, hd = q.shape
    outs, lses = [], []
    for window, dil in A_PATTERNS:
        def to_sub(t):
            return t.reshape(B, H, S // dil, dil, t.shape[-1]).swapaxes(2, 3)
        o, lse = _banded_window_attention(to_sub(q), to_sub(k), to_sub(v), window // dil)
        outs.append(o.swapaxes(2, 3).reshape(B, H, S, v.shape[-1]))
        lses.append(lse.swapaxes(2, 3).reshape(B, H, S))
    w = jax.nn.softmax(jnp.stack(lses), axis=0)
    return jnp.sum(w[..., None] * jnp.stack(outs), axis=0)


def _moba_attention(q, k, v):
    B, H, S, hd = q.shape
    nb = -(-S // MOBA_BLOCK)
    sp = nb * MOBA_BLOCK
    kp = jnp.pad(k, [(0, 0), (0, 0), (0, sp - S), (0, 0)])
    vp = jnp.pad(v, [(0, 0), (0, 0), (0, sp - S), (0, 0)])
    kblk = kp.reshape(B, H, nb, MOBA_BLOCK, hd)
    vblk = vp.reshape(B, H, nb, MOBA_BLOCK, hd)
    kmean = jnp.mean(kblk.astype(jnp.float32), axis=3)
    gate = jnp.einsum('bhsd,bhnd->bhsn', q.astype(jnp.float32), kmean)
    own = jnp.arange(S) // MOBA_BLOCK
    past = jnp.arange(nb)[None, :] < own[:, None]
    gate = jnp.where(past, gate, -jnp.inf)
    topk = min(MOBA_TOPK, nb)
    _, idx = lax.top_k(gate, topk)
    valid = jnp.arange(topk)[None, :] < own[:, None]
    nc = S // QBLOCK
    q_c = q.reshape(B, H, nc, QBLOCK, hd).transpose(2, 0, 1, 3, 4)
    idx_c = idx.reshape(B, H, nc, QBLOCK, topk).transpose(2, 0, 1, 3, 4)
    valid_c = valid.reshape(nc, QBLOCK, topk)
    starts = jnp.arange(nc) * QBLOCK
    bi = jnp.arange(B)[:, None, None, None]
    hi = jnp.arange(H)[None, :, None, None]
    scale = hd ** -0.5
    nsel = topk * MOBA_BLOCK

    def one_block(args):
        qc, ic, vc, st = args
        kg = kblk[bi, hi, ic]
        vg = vblk[bi, hi, ic]
        s_r = jnp.einsum('bhqd,bhqnmd->bhqnm', qc, kg).astype(jnp.float32) * scale
        s_r = jnp.where(vc[None, None, :, :, None], s_r, -jnp.inf).reshape(B, H, QBLOCK, nsel)
        own_start = (st // MOBA_BLOCK) * MOBA_BLOCK
        ko = lax.dynamic_slice_in_dim(kp, own_start, MOBA_BLOCK, axis=2)
        vo = lax.dynamic_slice_in_dim(vp, own_start, MOBA_BLOCK, axis=2)
        s_o = jnp.einsum('bhqd,bhmd->bhqm', qc, ko).astype(jnp.float32) * scale
        causal = (own_start + jnp.arange(MOBA_BLOCK))[None, :] <= (st + jnp.arange(QBLOCK))[:, None]
        s_o = jnp.where(causal, s_o, -jnp.inf)
        p = jax.nn.softmax(jnp.concatenate([s_r, s_o], axis=-1), axis=-1)
        p_r = p[..., :nsel].reshape(B, H, QBLOCK, topk, MOBA_BLOCK).astype(v.dtype)
        p_o = p[..., nsel:].astype(v.dtype)
        return (jnp.einsum('bhqnm,bhqnmd->bhqd', p_r, vg).astype(jnp.float32)
                + jnp.einsum('bhqm,bhmd->bhqd', p_o, vo).astype(jnp.float32))

    out = lax.map(one_block, (q_c, idx_c, valid_c, starts))
    return out.transpose(1, 2, 0, 3, 4).reshape(B, H, S, hd)


def _diff_attention(q1, q2, k1, k2, v, lam):
    B, H, S, hd = q1.shape
    nc = S // QBLOCK
    scale = hd ** -0.5
    kpos = jnp.arange(S)

    def chunks(t):
        return t.reshape(B, H, nc, QBLOCK, hd).transpose(2, 0, 1, 3, 4)

    def one_block(args):
        q1c, q2c, st = args
        causal = kpos[None, :] <= (st + jnp.arange(QBLOCK))[:, None]

        def attn_map(qc, kk):
            s = jnp.einsum('bhqd,bhkd->bhqk', qc, kk).astype(jnp.float32) * scale
            return jax.nn.softmax(jnp.where(causal, s, -jnp.inf), axis=-1)

        a = attn_map(q1c, k1) - lam * attn_map(q2c, k2)
        return jnp.einsum('bhqk,bhkd->bhqd', a.astype(v.dtype), v).astype(jnp.float32)

    out = lax.map(one_block, (chunks(q1), chunks(q2), jnp.arange(nc) * QBLOCK))
    return out.transpose(1, 2, 0, 3, 4).reshape(B, H, S, v.shape[-1])


def _token_mixer(h, cos, sin, w_in, w_out, lq1, lk1, lq2, lk2, g_sub, lam_init):
    B, S, _ = h.shape
    sizes = [A_WIDTH] * 3 + [B_WIDTH] * 3 + [C_WIDTH] * 3
    splits = [int(v) for v in np.cumsum(sizes)[:-1]]
    proj = jnp.dot(h, w_in)
    qa, ka, va, qb, kb, vb, qc, kc, vc = jnp.split(proj, splits, axis=-1)

    def heads(t, n):
        return t.reshape(B, S, n, -1)

    def bhsd(t):
        return t.transpose(0, 2, 1, 3)

    def flat(o):
        return o.transpose(0, 2, 1, 3).reshape(B, S, -1).astype(h.dtype)

    o_a = _dilated_attention(bhsd(_rope(heads(qa, A_HEADS), cos, sin)),
                             bhsd(_rope(heads(ka, A_HEADS), cos, sin)),
                             bhsd(heads(va, A_HEADS)))
    o_b = _moba_attention(bhsd(_rope(heads(qb, B_HEADS), cos, sin)),
                          bhsd(_rope(heads(kb, B_HEADS), cos, sin)),
                          bhsd(heads(vb, B_HEADS)))
    qc, kc = heads(qc, C_HEADS), heads(kc, C_HEADS)
    q1 = _rope(qc[..., :HEAD_DIM], cos, sin)
    q2 = _rope(qc[..., HEAD_DIM:], cos, sin)
    k1 = _rope(kc[..., :HEAD_DIM], cos, sin)
    k2 = _rope(kc[..., HEAD_DIM:], cos, sin)
    f32 = jnp.float32
    lam = (jnp.exp(jnp.sum(lq1.astype(f32) * lk1.astype(f32)))
           - jnp.exp(jnp.sum(lq2.astype(f32) * lk2.astype(f32))) + lam_init)
    o_c = _diff_attention(bhsd(q1), bhsd(q2), bhsd(k1), bhsd(k2), bhsd(heads(vc, C_HEADS)), lam)
    o_c = _rms_norm(o_c, g_sub) * (1.0 - lam_init)

    merged = jnp.concatenate([flat(o_a), flat(o_b), flat(o_c)], axis=-1)
    return jnp.dot(merged, w_out)


def _moe(h, w_router, b_router, w_gate, w_up, w_down):
    B, S, D = h.shape
    T = B * S
    tok = h.reshape(T, D)
    s = jax.nn.sigmoid(jnp.dot(tok, w_router).astype(jnp.float32))
    sg = (s + b_router.astype(jnp.float32)).reshape(T, N_GROUPS, EXPERTS_PER_GROUP)
    group_score = jnp.sum(lax.top_k(sg, TOP_K)[0], axis=-1)
    g = jnp.argmax(group_score, axis=-1)
    cand = jnp.take_along_axis(sg, g[:, None, None], axis=1)[:, 0]
    _, loc = lax.top_k(cand, TOP_K)
    eid = g[:, None] * EXPERTS_PER_GROUP + loc
    wts = jnp.take_along_axis(s, eid, axis=1)
    wts = wts / jnp.sum(wts, axis=-1, keepdims=True)
    flat_e = eid.reshape(-1)
    order = jnp.argsort(flat_e)
    tok_idx = order // TOP_K
    xs = tok[tok_idx]
    gs = jnp.bincount(flat_e, length=N_EXPERTS).astype(jnp.int32)
    a = lax.ragged_dot(xs, w_gate, gs)
    b = lax.ragged_dot(xs, w_up, gs)
    y = lax.ragged_dot(jax.nn.silu(a) * b, w_down, gs)
    y = y * wts.reshape(-1)[order][:, None].astype(y.dtype)
    out = jnp.zeros_like(tok).at[tok_idx].add(y)
    return out.reshape(B, S, D)


def setup_inputs(seed: int = 0) -> dict:
    key = jax.random.key(seed)
    ks = jax.random.split(key, 20)
    D = D_MODEL

    def nrm(k, shape, std):
        return jax.random.normal(k, shape, jnp.float32) * std

    return {
        "x": nrm(ks[0], (BATCH, SEQ, D), 1.0),
        "c": nrm(ks[1], (BATCH, D), 1.0),
        "positions": jnp.broadcast_to(jnp.arange(SEQ, dtype=jnp.int32)[None, :], (BATCH, SEQ)),
        "w_ada": nrm(ks[2], (DEPTH, D, 6 * D), 0.5 * D ** -0.5),
        "b_ada": nrm(ks[3], (DEPTH, 6 * D), 0.01),
        "g_attn": 1.0 + nrm(ks[4], (DEPTH, D), 0.05),
        "g_mlp": 1.0 + nrm(ks[5], (DEPTH, D), 0.05),
        "w_in": nrm(ks[6], (DEPTH, D, IN_WIDTH), D ** -0.5),
        "w_out": nrm(ks[7], (DEPTH, MIX_WIDTH, D), MIX_WIDTH ** -0.5),
        "lam_q1": nrm(ks[8], (DEPTH, HEAD_DIM), 0.1),
        "lam_k1": nrm(ks[9], (DEPTH, HEAD_DIM), 0.1),
        "lam_q2": nrm(ks[10], (DEPTH, HEAD_DIM), 0.1),
        "lam_k2": nrm(ks[11], (DEPTH, HEAD_DIM), 0.1),
        "g_subln": 1.0 + nrm(ks[12], (DEPTH, C_VDIM), 0.05),
        "w_gate": nrm(ks[13], (DEPTH, N_EXPERTS, D, D_EXPERT), D ** -0.5),
        "w_up": nrm(ks[14], (DEPTH, N_EXPERTS, D, D_EXPERT), D ** -0.5),
        "w_down": nrm(ks[15], (DEPTH, N_EXPERTS, D_EXPERT, D), D_EXPERT ** -0.5),
        "w_router": nrm(ks[16], (D, N_EXPERTS), D ** -0.5),
        "b_router": nrm(ks[17], (N_EXPERTS,), 0.01),
        "g_final": 1.0 + nrm(ks[18], (D,), 0.05),
    }


def reference(x, c, positions, w_ada, b_ada, g_attn, g_mlp, w_in, w_out,
              lam_q1, lam_k1, lam_q2, lam_k2, g_subln, w_gate, w_up, w_down,
              w_router, b_router, g_final):
    cos, sin = _rope_tables(positions)
    for l in range(DEPTH):
        lam_init = 0.8 - 0.6 * math.exp(-0.3 * l)
        mod = jnp.dot(jax.nn.silu(c), w_ada[l]) + b_ada[l]
        sh_a, sc_a, gt_a, sh_m, sc_m, gt_m = jnp.split(mod, 6, axis=-1)
        h = _rms_norm(x, g_attn[l]) * (1.0 + sc_a[:, None, :]) + sh_a[:, None, :]
        x = x + gt_a[:, None, :] * _token_mixer(h, cos, sin, w_in[l], w_out[l], lam_q1[l], lam_k1[l],
                                                lam_q2[l], lam_k2[l], g_subln[l], lam_init)
        h = _rms_norm(x, g_mlp[l]) * (1.0 + sc_m[:, None, :]) + sh_m[:, None, :]
        x = x + gt_m[:, None, :] * _moe(h, w_router, b_router, w_gate[l], w_up[l], w_down[l])
    return _rms_norm(x, g_final)
```

```python
import math
import numpy as np
import ml_dtypes
import concourse.bass as bass
import concourse.mybir as mybir
from concourse.bass_utils import run_bass_kernel_spmd

F32 = mybir.dt.float32
BF16 = mybir.dt.bfloat16
I32 = mybir.dt.int32
AF = mybir.ActivationFunctionType
ALU = mybir.AluOpType
AX = mybir.AxisListType
NPBF = ml_dtypes.bfloat16

D = 2048
S = 16384
NCORE = 8
TPC = S // NCORE
KD = D // 128
EPS = 1e-6
NEG = -30000.0


class _B:
    __slots__ = ("w", "r")

    def __init__(self):
        self.w = None
        self.r = {}


class Sched:
    ENG = ("pe", "act", "dve", "pool", "sp")
    NDS = 40

    def __init__(self, nc):
        self.nc = nc
        self.e = dict(pe=nc.tensor, act=nc.scalar, dve=nc.vector, pool=nc.gpsimd, sp=nc.sync)
        self.sem = {k: nc.alloc_semaphore(f"s_{k}") for k in self.ENG}
        self.cnt = {k: 0 for k in self.ENG}
        self.seen = {k: {} for k in self.ENG}
        self.dsem = [nc.alloc_semaphore(f"d{i}") for i in range(self.NDS)]
        self.dcnt = [0] * self.NDS
        self.dnext = 0
        self.bufs = {}
        self.out_toks = []

    def buf(self, k):
        b = self.bufs.get(k)
        if b is None:
            b = self.bufs[k] = _B()
        return b

    def _semof(self, key):
        return self.sem[key[1]] if key[0] == "e" else self.dsem[key[1]]

    def _wait(self, eng, toks):
        need = {}
        for t in toks:
            if t is None:
                continue
            key = (t[0], t[1])
            if key == ("e", "pe") and eng == "pe":
                continue
            if need.get(key, 0) < t[2]:
                need[key] = t[2]
        for key, val in need.items():
            if self.seen[eng].get(key, 0) >= val:
                continue
            self.e[eng].wait_ge(self._semof(key), val)
            self.seen[eng][key] = val

    def _deps(self, r, w):
        toks = []
        for k in r:
            toks.append(self.buf(k).w)
        for k in w:
            b = self.buf(k)
            toks.append(b.w)
            for key, val in b.r.items():
                toks.append((key[0], key[1], val))
        return toks

    def _mark(self, tok, r, w):
        key = (tok[0], tok[1])
        for k in r:
            b = self.buf(k)
            if b.r.get(key, 0) < tok[2]:
                b.r[key] = tok[2]
        for k in w:
            b = self.buf(k)
            b.w = tok
            b.r = {}

    def op(self, eng, fn, r=(), w=()):
        self._wait(eng, self._deps(r, w))
        ins = fn(self.e[eng])
        self.cnt[eng] += 1
        ins.then_inc(self.sem[eng], 1)
        tok = ("e", eng, self.cnt[eng])
        self._mark(tok, r, w)
        return tok

    def dma(self, out, in_, r=(), w=(), q="sp", is_out=False, **kw):
        j = self.dnext
        self.dnext = (self.dnext + 1) % self.NDS
        toks = self._deps(r, w)
        if self.dcnt[j] > 0:
            toks.append(("d", j, self.dcnt[j]))
        self._wait(q, toks)
        ins = self.e[q].dma_start(out=out, in_=in_, **kw)
        self.dcnt[j] += 16
        ins.then_inc(self.dsem[j], 16)
        tok = ("d", j, self.dcnt[j])
        self._mark(tok, r, w)
        if is_out:
            self.out_toks.append(tok)
        return tok

    def barrier(self):
        toks = [("e", k, self.cnt[k]) for k in ("pe", "act", "dve", "pool") if self.cnt[k] > 0]
        toks += [("d", j, self.dcnt[j]) for j in range(self.NDS) if self.dcnt[j] > 0]
        for eng in self.ENG:
            self._wait(eng, toks)

    def finish(self):
        self._wait("sp", self.out_toks)
        self._wait("sp", [("e", k, self.cnt[k]) for k in ("pe", "act", "dve", "pool") if self.cnt[k] > 0])


def _din(nc, name, shape, dt):
    return nc.dram_tensor(name, list(shape), dt, kind="ExternalInput").ap()


def _dout(nc, name, shape, dt):
    return nc.dram_tensor(name, list(shape), dt, kind="ExternalOutput").ap()


MCOL = 6 * D // NCORE


def build_M():
    nc = bass.Bass("TRN2", target_bir_lowering=False)
    cT = _din(nc, "cT", [128, KD], F32)
    wada = _din(nc, "wada", [2 * D, MCOL], F32)
    bada = _din(nc, "bada", [1, 2 * MCOL], F32)
    mod = _dout(nc, "mod", [1, 2 * MCOL], F32)
    s = Sched(nc)
    ct = nc.alloc_sbuf_tensor("ct", [128, KD], F32)
    sc = nc.alloc_sbuf_tensor("sc", [128, KD], F32)
    sc2 = nc.alloc_sbuf_tensor("sc2", [128, KD, 2], F32)
    bt = nc.alloc_sbuf_tensor("bt", [2, 2 * MCOL], F32)
    res = nc.alloc_sbuf_tensor("res", [2, 2 * MCOL], F32)
    wt = [nc.alloc_sbuf_tensor(f"wt{i}", [128, KD, 512], F32) for i in range(2)]
    pp = [nc.alloc_psum_tensor(f"pp{i}", [128, 512], F32) for i in range(2)]
    s.dma(ct[:, :], cT, w=["ct"])
    s.dma(bt[0:1, :], bada, w=["bt"])
    s.op("act", lambda e: e.activation(out=sc[:, :], in_=ct[:, :], func=AF.Silu), r=["ct"], w=["sc"])
    s.op("dve", lambda e: e.tensor_copy(out=sc2[:, :, 0], in_=sc[:, :]), r=["sc"], w=["sc2a"])
    s.op("dve", lambda e: e.tensor_copy(out=sc2[:, :, 1], in_=sc[:, :]), r=["sc"], w=["sc2b"])
    i = 0
    for l in range(2):
        for blk in range(MCOL // 512):
            b = i % 2
            src = wada[l * D:(l + 1) * D, blk * 512:(blk + 1) * 512].rearrange("(k p) c -> p k c", p=128)
            s.dma(wt[b][:, :, :], src, w=[f"wt{b}"])
            for k in range(KD):
                s.op("pe", lambda e, k=k, b=b: e.matmul(pp[b][0:2, :], lhsT=sc2[:, k, :], rhs=wt[b][:, k, :],
                                                         start=(k == 0), stop=(k == KD - 1)),
                     r=["sc2a", "sc2b", f"wt{b}"], w=[f"pp{b}"])
            c0 = l * MCOL + blk * 512
            s.op("dve", lambda e, b=b, c0=c0: e.tensor_tensor(out=res[0:1, c0:c0 + 512], in0=pp[b][0:1, :],
                                                              in1=bt[0:1, c0:c0 + 512], op=ALU.add),
                 r=[f"pp{b}", "bt"], w=["res"])
            i += 1
    s.dma(mod, res[0:1, :], r=["res"], is_out=True)
    s.finish()
    return nc


NG = 48
TWO_PI_HI = 6.28125
TWO_PI_LO = 2.0 * math.pi - 6.28125
MAGIC = 12582912.0


def _rms_rstd(s, nc, ss, rstd, tmp, tagr, tagw, ncols):
    s.op("dve", lambda e: e.tensor_scalar(out=tmp, in0=ss, scalar1=1.0 / ncols, scalar2=EPS,
                                          op0=ALU.mult, op1=ALU.add), r=[tagr], w=[tagw + "_t"])
    s.op("act", lambda e: e.activation(out=tmp, in_=tmp, func=AF.Sqrt), r=[tagw + "_t"], w=[tagw + "_t"])
    s.op("dve", lambda e: e.reciprocal(out=rstd, in_=tmp), r=[tagw + "_t"], w=[tagw])


def build_PA1(add_moe):
    nc = bass.Bass("TRN2", target_bir_lowering=False)
    x = _din(nc, "x", [TPC, D], F32)
    win = _din(nc, "win", [NG * 128, KD * 128], F32)
    gA = _din(nc, "gA", [128, KD], F32)
    scA = _din(nc, "scA", [128, KD], F32)
    shA = _din(nc, "shA", [128, KD], F32)
    pos = _din(nc, "pos", [1, TPC], I32)
    invf = _din(nc, "invf", [128, 1], F32)
    perm = _din(nc, "perm", [128, 128], F32)
    ident = _din(nc, "ident", [128, 128], F32)
    bones = _din(nc, "bones", [128, 128], F32)
    if add_moe:
        ys = [_din(nc, f"y{g}", [TPC, D], BF16) for g in range(4)]
        gtm = _din(nc, "gtm", [128, D], F32)
        xnew = _dout(nc, "xnew", [TPC, D], F32)
    qkvT = _dout(nc, "qkvT", [NG * 128, TPC], BF16)
    nrm = _dout(nc, "nrm", [128, 32], F32)
    s = Sched(nc)
    A = nc.alloc_sbuf_tensor
    xnT = A("xnT", [128, KD, TPC], BF16)
    cosT = A("cosT", [128, TPC], F32)
    sinT = A("sinT", [128, TPC], F32)
    xt = [A(f"xt{i}", [128, D], F32) for i in range(2)]
    wst = [A(f"wst{i}", [128, KD, 128], F32) for i in range(2)]
    wb = [A(f"wb{i}", [128, KD, 128], BF16) for i in range(2)]
    qf = [A(f"qf{i}", [128, 512], F32) for i in range(2)]
    t1 = A("t1", [128, 512], F32)
    t2 = A("t2", [128, 512], F32)
    qo = [A(f"qo{i}", [128, 512], BF16) for i in range(2)]
    sq = A("sq", [128, 512], BF16)
    qo3 = [A(f"qo3_{i}", [128, 512], BF16) for i in range(3)]
    t2b = [A(f"t2b{i}", [128, 512], F32) for i in range(2)]
    sqb = [A(f"sqb{i}", [128, 512], BF16) for i in range(2)]
    ga = A("ga", [128, KD], F32)
    sca = A("sca", [128, KD], F32)
    sha = A("sha", [128, KD], F32)
    sh2 = A("sh2", [128, KD, 2], F32)
    aS = A("aS", [128, KD], F32)
    invt = A("invt", [128, 1], F32)
    permt = A("permt", [128, 128], F32)
    idt = A("idt", [128, 128], F32)
    bon = A("bon", [128, 128], BF16)
    posi = A("posi", [128, TPC], I32)
    small = A("small", [128, 8], F32)
    biasg = A("biasg", [128, NG], F32)
    nm = A("nm", [128, 32, 4], F32)
    nmr = A("nmr", [128, 32], F32)
    junk = A("junk", [128, D], F32)
    if add_moe:
        yt = [A(f"yt{g}", [128, D], BF16) for g in range(4)]
        gtt = A("gtt", [128, D], F32)
    P = [nc.alloc_psum_tensor(f"P{i}", [128, 512], F32) for i in range(8)]

    s.dma(ga[:, :], gA, w=["ga"])
    s.dma(sca[:, :], scA, w=["sca"])
    s.dma(sha[:, :], shA, w=["sha"])
    s.dma(invt[:, :], invf, w=["invt"])
    s.dma(permt[:, :], perm, w=["permt"])
    s.dma(idt[:, :], ident, w=["idt"])
    s.dma(bon[:, :], bones, w=["bon"], q="pool")
    s.dma(posi[:, :], pos[0:1, :].broadcast_to([128, TPC]), w=["posi"])
    if add_moe:
        s.dma(gtt[:, :], gtm, w=["gtt"])
    s.op("dve", lambda e: e.tensor_scalar(out=aS[:, :], in0=sca[:, :], scalar1=1.0, scalar2=None, op0=ALU.add),
         r=["sca"], w=["aS"])
    s.op("dve", lambda e: e.tensor_tensor(out=aS[:, :], in0=aS[:, :], in1=ga[:, :], op=ALU.mult), r=["aS", "ga"], w=["aS"])
    s.op("dve", lambda e: e.tensor_copy(out=sh2[:, :, 0], in_=sha[:, :]), r=["sha"], w=["sh2a"])
    s.op("dve", lambda e: e.tensor_copy(out=sh2[:, :, 1], in_=sha[:, :]), r=["sha"], w=["sh2b"])
    s.op("dve", lambda e: e.tensor_copy(out=cosT[:, :], in_=posi[:, :]), r=["posi"], w=["cosT"])
    s.op("dve", lambda e: e.tensor_scalar(out=cosT[:, :], in0=cosT[:, :], scalar1=invt[:, 0:1], scalar2=None, op0=ALU.mult),
         r=["cosT", "invt"], w=["cosT"])
    s.op("dve", lambda e: e.tensor_scalar(out=sinT[:, :], in0=cosT[:, :], scalar1=1.0 / (2.0 * math.pi), scalar2=None, op0=ALU.mult),
         r=["cosT"], w=["sinT"])
    s.op("dve", lambda e: e.tensor_scalar(out=sinT[:, :], in0=sinT[:, :], scalar1=MAGIC, scalar2=None, op0=ALU.add),
         r=["sinT"], w=["sinT"])
    s.op("dve", lambda e: e.tensor_scalar(out=sinT[:, :], in0=sinT[:, :], scalar1=-MAGIC, scalar2=None, op0=ALU.add),
         r=["sinT"], w=["sinT"])
    s.op("dve", lambda e: e.scalar_tensor_tensor(out=cosT[:, :], in0=sinT[:, :], scalar=-TWO_PI_HI, in1=cosT[:, :],
                                                 op0=ALU.mult, op1=ALU.add), r=["sinT", "cosT"], w=["cosT"])
    s.op("dve", lambda e: e.scalar_tensor_tensor(out=cosT[:, :], in0=sinT[:, :], scalar=-TWO_PI_LO, in1=cosT[:, :],
                                                 op0=ALU.mult, op1=ALU.add), r=["sinT", "cosT"], w=["cosT"])
    s.op("dve", lambda e: e.tensor_scalar(out=cosT[:, :], in0=cosT[:, :], scalar1=-math.pi, scalar2=math.pi, op0=ALU.max, op1=ALU.min),
         r=["cosT"], w=["cosT"])
    s.op("act", lambda e: e.activation(out=sinT[:, :], in_=cosT[:, :], func=AF.Sin), r=["cosT"], w=["sinT"])
    for c4 in range(TPC // 512):
        cs = slice(c4 * 512, (c4 + 1) * 512)
        s.op("dve", lambda e, cs=cs: e.tensor_scalar(out=t1[:, :], in0=cosT[:, cs], scalar1=math.pi / 2, scalar2=None, op0=ALU.is_gt),
             r=["cosT"], w=["t1"])
        s.op("dve", lambda e, cs=cs: e.tensor_scalar(out=t2[:, :], in0=cosT[:, cs], scalar1=math.pi / 2, scalar2=None, op0=ALU.add),
             r=["cosT"], w=["t2"])
        s.op("dve", lambda e, cs=cs: e.scalar_tensor_tensor(out=t2[:, :], in0=t1[:, :], scalar=-2.0 * math.pi, in1=t2[:, :],
                                                            op0=ALU.mult, op1=ALU.add), r=["t1", "t2"], w=["t2"])
        s.op("dve", lambda e: e.tensor_scalar(out=t2[:, :], in0=t2[:, :], scalar1=-math.pi, scalar2=math.pi, op0=ALU.max, op1=ALU.min),
             r=["t2"], w=["t2"])
        s.op("act", lambda e, cs=cs: e.activation(out=cosT[:, cs], in_=t2[:, :], func=AF.Sin), r=["t2", "cosT"], w=["cosT"])

    for t in range(TPC // 128):
        b = t % 2
        rows = slice(t * 128, (t + 1) * 128)
        s.dma(xt[b][:, :], x[rows, :], w=[f"xt{b}"])
        if add_moe:
            for g in range(4):
                s.dma(yt[g][:, :], ys[g][rows, :], w=[f"yt{g}"])
            s.op("pool", lambda e: e.tensor_tensor(out=yt[0][:, :], in0=yt[0][:, :], in1=yt[1][:, :], op=ALU.add), r=["yt0", "yt1"], w=["yt0"])
            s.op("pool", lambda e: e.tensor_tensor(out=yt[2][:, :], in0=yt[2][:, :], in1=yt[3][:, :], op=ALU.add), r=["yt2", "yt3"], w=["yt2"])
            for c4 in range(4):
                cs = slice(c4 * 512, (c4 + 1) * 512)
                s.op("dve", lambda e, cs=cs: e.tensor_tensor(out=t1[:, :], in0=yt[0][:, cs], in1=yt[2][:, cs], op=ALU.add),
                     r=["yt0", "yt2"], w=["t1"])
                s.op("dve", lambda e, cs=cs: e.tensor_tensor(out=t1[:, :], in0=t1[:, :], in1=gtt[:, cs], op=ALU.mult),
                     r=["t1", "gtt"], w=["t1"])
                s.op("dve", lambda e, cs=cs, b=b: e.tensor_tensor(out=xt[b][:, cs], in0=xt[b][:, cs], in1=t1[:, :], op=ALU.add),
                     r=["t1", f"xt{b}"], w=[f"xt{b}"])
            s.dma(xnew[rows, :], xt[b][:, :], r=[f"xt{b}"], is_out=True)
        s.op("act", lambda e, b=b: e.activation(out=junk[:, :], in_=xt[b][:, :], func=AF.Square, accum_out=small[:, 0:1]),
             r=[f"xt{b}"], w=["junk", "ss"])
        _rms_rstd(s, nc, small[:, 0:1], small[:, 1:2], small[:, 2:3], "ss", "rstd", D)
        s.op("dve", lambda e, b=b: e.tensor_scalar(out=xt[b][:, :], in0=xt[b][:, :], scalar1=small[:, 1:2], scalar2=None, op0=ALU.mult),
             r=[f"xt{b}", "rstd"], w=[f"xt{b}"])
        for k4 in range(4):
            pb = P[(t % 2) * 4 + k4]
            for kk in range(4):
                k = k4 * 4 + kk
                s.op("pe", lambda e, pb=pb, kk=kk, k=k, b=b: e.transpose(out=pb[:, kk * 128:(kk + 1) * 128], in_=xt[b][:, k * 128:(k + 1) * 128],
                                                                        identity=idt[:, :]),
                     r=[f"xt{b}", "idt"], w=[f"P{(t % 2) * 4 + k4}"])
            eng = "act" if k4 % 2 == 0 else "dve"
            dst = xnT[:, k4 * 4:(k4 + 1) * 4, t * 128:(t + 1) * 128]
            srcv = pb[:, :].rearrange("p (k t) -> p k t", k=4)
            if eng == "act":
                s.op("act", lambda e, dst=dst, srcv=srcv: e.copy(out=dst, in_=srcv), r=[f"P{(t % 2) * 4 + k4}"], w=["xnT"])
            else:
                s.op("dve", lambda e, dst=dst, srcv=srcv: e.tensor_copy(out=dst, in_=srcv), r=[f"P{(t % 2) * 4 + k4}"], w=["xnT"])

    NCH = TPC // 512
    iters = [(g, ch) for g in range(NG) for ch in range(NCH)]
    NI = len(iters)

    def w_dma(g):
        s.dma(wst[g % 2][:, :, :], win[g * 128:(g + 1) * 128, :].rearrange("p (k c) -> p k c", k=KD), w=[f"wst{g % 2}"])

    def w_prep(g):
        wbuf = g % 2
        for k in range(KD):
            s.op("pe", lambda e, k=k, wbuf=wbuf: e.matmul(P[6][:, 0:2], lhsT=wst[wbuf][:, k, :], rhs=sh2[:, k, :],
                                                           start=(k == 0), stop=(k == KD - 1)),
                 r=[f"wst{wbuf}", "sh2a", "sh2b"], w=["P6"])
        s.op("dve", lambda e, g=g: e.tensor_copy(out=biasg[:, g:g + 1], in_=P[6][:, 0:1]), r=["P6"], w=[f"biasg{g}"])
        s.op("dve", lambda e, wbuf=wbuf: e.tensor_tensor(out=wb[wbuf][:, :, :], in0=wst[wbuf][:, :, :],
                                                          in1=aS[:, :].unsqueeze(2).broadcast_to([128, KD, 128]), op=ALU.mult),
             r=[f"wst{wbuf}", "aS"], w=[f"wb{wbuf}"])

    def stA(it):
        g, ch = iters[it]
        cs = slice(ch * 512, (ch + 1) * 512)
        if ch == 0 and g + 1 < NG:
            w_dma(g + 1)
        pq, pqn = P[it % 2], f"P{it % 2}"
        for k in range(KD):
            s.op("pe", lambda e, k=k, g=g, pq=pq, cs=cs: e.matmul(pq[:, :], lhsT=wb[g % 2][:, k, :], rhs=xnT[:, k, cs],
                                                                  start=(k == 0), stop=(k == KD - 1)),
                 r=[f"wb{g % 2}", "xnT"], w=[pqn])
        if ch == 2 and g + 1 < NG:
            w_prep(g + 1)
        if g % 6 >= 4:
            ob = it % 3
            s.op("act", lambda e, pq=pq, ob=ob, g=g: e.activation(out=qo3[ob][:, :], in_=pq[:, :], func=AF.Identity, bias=biasg[:, g:g + 1]),
                 r=[pqn, f"biasg{g}"], w=[f"qo{ob}"])
            s.dma(qkvT[g * 128:(g + 1) * 128, cs], qo3[ob][:, :], r=[f"qo{ob}"], is_out=True)
        else:
            b2 = it % 2
            s.op("act", lambda e, pq=pq, b2=b2, g=g: e.activation(out=qf[b2][:, :], in_=pq[:, :], func=AF.Identity, bias=biasg[:, g:g + 1]),
                 r=[pqn, f"biasg{g}"], w=[f"qf{b2}"])
            s.op("dve", lambda e, b2=b2, cs=cs: e.tensor_tensor(out=t2b[b2][:, :], in0=qf[b2][:, :], in1=cosT[:, cs], op=ALU.mult),
                 r=[f"qf{b2}", "cosT"], w=[f"t2b{b2}"])

    def stC(it):
        g, ch = iters[it]
        if g % 6 >= 4:
            return
        cs = slice(ch * 512, (ch + 1) * 512)
        b2, ob = it % 2, it % 3
        pr, prn = P[2 + b2], f"P{2 + b2}"
        s.op("pe", lambda e, pr=pr, b2=b2: e.matmul(pr[:, :], lhsT=permt[:, :], rhs=qf[b2][:, :], start=True, stop=True),
             r=["permt", f"qf{b2}"], w=[prn])
        s.op("dve", lambda e, pr=pr, cs=cs: e.tensor_tensor(out=t1[:, :], in0=pr[:, :], in1=sinT[:, cs], op=ALU.mult), r=[prn, "sinT"], w=["t1"])
        s.op("dve", lambda e, ob=ob, b2=b2: e.tensor_tensor(out=qo3[ob][:, :], in0=t1[:, :], in1=t2b[b2][:, :], op=ALU.add),
             r=["t1", f"t2b{b2}"], w=[f"qo{ob}"])
        s.dma(qkvT[g * 128:(g + 1) * 128, cs], qo3[ob][:, :], r=[f"qo{ob}"], is_out=True)
        s.op("act", lambda e, ob=ob, b2=b2: e.activation(out=sqb[b2][:, :], in_=qo3[ob][:, :], func=AF.Square), r=[f"qo{ob}"], w=[f"sqb{b2}"])

    def stE(it):
        g, ch = iters[it]
        if g % 6 >= 4:
            return
        b2 = it % 2
        pn, pnn = P[4 + b2], f"P{4 + b2}"
        s.op("pe", lambda e, pn=pn, b2=b2: e.matmul(pn[:, :], lhsT=bon[:, :], rhs=sqb[b2][:, :], start=True, stop=True), r=["bon", f"sqb{b2}"], w=[pnn])
        gq = (g // 6) * 4 + g % 6
        s.op("dve", lambda e, pn=pn, gq=gq, ch=ch: e.tensor_reduce(out=nm[:, gq, ch:ch + 1], in_=pn[:, :], axis=AX.X, op=ALU.max), r=[pnn], w=["nm"])

    w_dma(0)
    w_prep(0)
    for it in range(NI + 2):
        if it < NI:
            stA(it)
        if 0 <= it - 1 < NI:
            stC(it - 1)
        if 0 <= it - 2 < NI:
            stE(it - 2)
    s.op("dve", lambda e: e.tensor_reduce(out=nmr[:, :], in_=nm[:, :, :], axis=AX.X, op=ALU.max), r=["nm"], w=["nmr"])
    s.dma(nrm, nmr[:, :], r=["nmr"], is_out=True)
    s.finish()
    return nc


def build_A2():
    nc = bass.Bass("TRN2", target_bir_lowering=False)
    qab = _din(nc, "qab", [128, S], BF16)
    kab = _din(nc, "kab", [128, S], BF16)
    qcT = _din(nc, "qcT", [128, S], BF16)
    kcT = _din(nc, "kcT", [128, S], BF16)
    vcP = _din(nc, "vcP", [128, 128 * 128], BF16)
    vbP = _din(nc, "vbP", [128, 128 * 128], BF16)
    vaP = _din(nc, "vaP", [128, 3 * 128 * 64], BF16)
    Ecs = _din(nc, "Ecs", [64, S], BF16)
    nrmb = _din(nc, "nrmb", [128, 64], F32)
    lamt = _din(nc, "lamt", [128, 256], F32)
    lamc = _din(nc, "lamc", [128, 2], F32)
    gsub = _din(nc, "gsub", [128, 1], F32)
    mask2 = _din(nc, "mask2", [128, 256], F32)
    ident = _din(nc, "ident", [128, 128], F32)
    oT = _dout(nc, "oT", [256, S], BF16)
    s = Sched(nc)
    A = nc.alloc_sbuf_tensor
    BIG = [A(f"BIG{i}", [128, S], BF16) for i in range(4)]
    m2 = A("m2", [128, 256], BF16)
    idt = A("idt", [128, 128], F32)
    ones_b = A("ones_b", [128, 128], BF16)
    ones_f = A("ones_f", [128, 128], F32)
    nr_in = A("nr_in", [128, 64], F32)
    nr = A("nr", [128, 8], F32)
    negM = A("negM", [128, 4], F32)
    lt = A("lt", [128, 256], F32)
    lc = A("lc", [128, 2], F32)
    gs = A("gs", [128, 1], F32)
    sm = A("sm", [128, 8], F32)
    pt = [[A(f"pt{m}{i}", [128, 512], BF16) for i in range(2)] for m in range(2)]
    e = [A(f"e{i}", [128, 512], F32) for i in range(4)]
    ob = [A(f"ob{i}", [128, 512], BF16) for i in range(2)]
    P = [nc.alloc_psum_tensor(f"P{i}", [128, 512], F32) for i in range(8)]
    tri = m2[:, 128:256]
    accC = [A(f"accC{i}", [128, 512], F32) for i in range(2)]

    s.dma(m2[:, :], mask2, w=["m2"], q="pool")
    s.dma(idt[:, :], ident, w=["idt"])
    s.dma(nr_in[:, :], nrmb, w=["nr_in"])
    s.dma(lt[:, :], lamt, w=["lt"])
    s.dma(lc[:, :], lamc, w=["lc"])
    s.dma(gs[:, :], gsub, w=["gs"])
    s.op("pool", lambda g: g.memset(ones_b[:, :], 1.0), w=["ones_b"])
    s.op("pool", lambda g: g.memset(ones_f[:, :], 1.0), w=["ones_f"])
    s.op("dve", lambda g: g.tensor_reduce(out=nr[:, :], in_=nr_in[:, :].rearrange("p (q c) -> p q c", c=8), axis=AX.X, op=ALU.max),
         r=["nr_in"], w=["nr"])
    nr4 = nr[:, :].rearrange("p (s t h) -> p s t h", s=2, t=2)
    s.op("dve", lambda g: g.tensor_tensor(out=negM[:, :].rearrange("p (s h) -> p s h", s=2), in0=nr4[:, :, 0, :], in1=nr4[:, :, 1, :], op=ALU.mult),
         r=["nr"], w=["negM"])
    s.op("act", lambda g: g.activation(out=negM[:, :], in_=negM[:, :], func=AF.Sqrt), r=["negM"], w=["negM"])
    s.op("dve", lambda g: g.tensor_scalar(out=negM[:, :], in0=negM[:, :], scalar1=-1.02 * 0.125, scalar2=None, op0=ALU.mult),
         r=["negM"], w=["negM"])
    s.op("dve", lambda g: g.tensor_tensor(out=lt[:, 0:64], in0=lt[:, 0:64], in1=lt[:, 64:128], op=ALU.mult), r=["lt"], w=["lt"])
    s.op("dve", lambda g: g.tensor_tensor(out=lt[:, 128:192], in0=lt[:, 128:192], in1=lt[:, 192:256], op=ALU.mult), r=["lt"], w=["lt"])
    s.op("dve", lambda g: g.tensor_reduce(out=sm[:, 0:1], in_=lt[:, 0:64], axis=AX.X, op=ALU.add), r=["lt"], w=["sm"])
    s.op("dve", lambda g: g.tensor_reduce(out=sm[:, 1:2], in_=lt[:, 128:192], axis=AX.X, op=ALU.add), r=["lt"], w=["sm"])
    s.op("act", lambda g: g.activation(out=sm[:, 0:2], in_=sm[:, 0:2], func=AF.Exp), r=["sm"], w=["sm"])
    s.op("dve", lambda g: g.tensor_tensor(out=sm[:, 2:3], in0=sm[:, 1:2], in1=sm[:, 0:1], op=ALU.subtract), r=["sm"], w=["sm"])
    s.op("dve", lambda g: g.tensor_tensor(out=sm[:, 2:3], in0=sm[:, 2:3], in1=lc[:, 0:1], op=ALU.subtract), r=["sm", "lc"], w=["sm"])
    s.op("dve", lambda g: g.tensor_tensor(out=sm[:, 3:4], in0=gs[:, 0:1], in1=lc[:, 1:2], op=ALU.mult), r=["gs", "lc"], w=["sm"])
    neglam = sm[:, 2:3]
    gsc = sm[:, 3:4]

    def causal_rng(c, kb):
        j = kb - 4 * c
        off = 128 * j if j > 0 else 0
        return j, off, 512 - off

    QC, KC = BIG[0], BIG[1]
    VC = BIG[2][:, :].rearrange("p (t d) -> p t d", d=128)
    s.dma(QC[:, :], qcT, w=["QC"])
    s.dma(KC[:, :], kcT, w=["KC"])
    s.dma(BIG[2][:, :], vcP, w=["VC"])
    SC = [[P[0], P[1]], [P[2], P[3]]]
    SCn = [["P0", "P1"], ["P2", "P3"]]
    O, On = [P[4], P[5]], ["P4", "P5"]
    Dn_, Dnn = [P[6], P[7]], ["P6", "P7"]
    it = 0
    pend_c = None

    def c_tail(c, itx):
        sb, sbn = SC[0][itx % 2], SCn[0][itx % 2]
        s.op("pe", lambda g, sb=sb: g.matmul(sb[:, :], lhsT=ones_f[:, :], rhs=e[1][:, :], start=True, stop=True), r=["ones_f", "e1"], w=[sbn])
        s.op("dve", lambda g, sb=sb: g.tensor_scalar(out=e[2][:, :], in0=sb[:, :], scalar1=1.0 / 128, scalar2=EPS, op0=ALU.mult, op1=ALU.add),
             r=[sbn], w=["e2"])
        s.op("act", lambda g: g.activation(out=e[2][:, :], in_=e[2][:, :], func=AF.Sqrt), r=["e2"], w=["e2"])
        s.op("dve", lambda g: g.reciprocal(out=e[2][:, :], in_=e[2][:, :]), r=["e2"], w=["e2"])
        o_ = ob[c % 2]
        s.op("dve", lambda g, o_=o_: g.scalar_tensor_tensor(out=o_[:, :], in0=e[0][:, :], scalar=gsc, in1=e[2][:, :], op0=ALU.mult, op1=ALU.mult),
             r=["e0", "e2", "sm"], w=[f"ob{c % 2}"])
        s.dma(oT[128:256, c * 512:(c + 1) * 512], o_[:, :], r=[f"ob{c % 2}"], is_out=True)

    for c in range(S // 512):
        nkb = 4 * c + 4

        def qk(kb, itx):
            j, off, n = causal_rng(c, kb)
            q0 = c * 512 + off
            for m in range(2):
                r0 = 64 * m
                sb, sbn = SC[m][itx % 2], SCn[m][itx % 2]
                s.op("pe", lambda g, sb=sb, r0=r0, kb=kb, q0=q0, n=n: g.matmul(sb[:, 0:n], lhsT=KC[r0:r0 + 64, kb * 128:(kb + 1) * 128],
                                                                              rhs=QC[r0:r0 + 64, q0:q0 + n], start=True, stop=True),
                     r=["KC", "QC"], w=[sbn])
        qk(0, it)
        for kb in range(nkb):
            if kb + 1 < nkb:
                qk(kb + 1, it + 1)
            j, off, n = causal_rng(c, kb)
            for m in range(2):
                sb, sbn = SC[m][it % 2], SCn[m][it % 2]
                p_, pn = pt[m][it % 2], f"pt{m}{it % 2}"
                s.op("act", lambda g, sb=sb, p_=p_, n=n, m=m: g.activation(out=p_[:, 0:n], in_=sb[:, 0:n], func=AF.Exp,
                                                                           bias=negM[:, 2 + m:3 + m], scale=0.125),
                     r=[sbn, "negM"], w=[pn])
                if j >= 0:
                    s.op("pool", lambda g, p_=p_: g.tensor_tensor(out=p_[:, 0:128], in0=p_[:, 0:128], in1=tri, op=ALU.mult),
                         r=[pn, "m2"], w=[pn])
                s.op("pe", lambda g, p_=p_, n=n, off=off, kb=kb, m=m: g.matmul(O[m][:, off:512], lhsT=VC[:, kb, :], rhs=p_[:, 0:n],
                                                                              start=(kb == 0), stop=(kb == nkb - 1)),
                     r=[pn, "VC"], w=[On[m]])
                s.op("pe", lambda g, p_=p_, n=n, off=off, kb=kb, m=m: g.matmul(Dn_[m][:, off:512], lhsT=ones_b[:, :], rhs=p_[:, 0:n],
                                                                              start=(kb == 0), stop=(kb == nkb - 1)),
                     r=[pn, "ones_b"], w=[Dnn[m]])
            it += 1
            if pend_c is not None and kb == min(nkb - 1, 10):
                c_tail(pend_c, it + 1)
                pend_c = None
        s.op("act", lambda g: g.copy(out=e[0][:, :], in_=O[0][:, :]), r=[On[0]], w=["e0"])
        s.op("act", lambda g: g.copy(out=e[1][:, :], in_=O[1][:, :]), r=[On[1]], w=["e1"])
        s.op("dve", lambda g: g.tensor_copy(out=e[2][:, :], in_=Dn_[0][:, :]), r=[Dnn[0]], w=["e2"])
        s.op("dve", lambda g: g.tensor_copy(out=e[3][:, :], in_=Dn_[1][:, :]), r=[Dnn[1]], w=["e3"])
        s.op("dve", lambda g: g.reciprocal(out=e[2][:, :], in_=e[2][:, :]), r=["e2"], w=["e2"])
        s.op("dve", lambda g: g.reciprocal(out=e[3][:, :], in_=e[3][:, :]), r=["e3"], w=["e3"])
        s.op("pool", lambda g: g.tensor_tensor(out=e[0][:, :], in0=e[0][:, :], in1=e[2][:, :], op=ALU.mult), r=["e0", "e2"], w=["e0"])
        s.op("pool", lambda g: g.tensor_tensor(out=e[1][:, :], in0=e[1][:, :], in1=e[3][:, :], op=ALU.mult), r=["e1", "e3"], w=["e1"])
        s.op("dve", lambda g: g.scalar_tensor_tensor(out=e[0][:, :], in0=e[1][:, :], scalar=neglam, in1=e[0][:, :], op0=ALU.mult, op1=ALU.add),
             r=["e0", "e1", "sm"], w=["e0"])
        s.op("pool", lambda g: g.tensor_tensor(out=e[1][:, :], in0=e[0][:, :], in1=e[0][:, :], op=ALU.mult), r=["e0"], w=["e1"])
        pend_c = c
    c_tail(pend_c, it)

    s.barrier()
    KBE, QBB, QBL = BIG[0], BIG[1], BIG[3]
    VB = BIG[2][:, :].rearrange("p (t d) -> p t d", d=128)
    s.dma(KBE[0:64, :], Ecs, w=["KBE"])
    s.dma(KBE[64:128, :], kab[64:128, :], w=["KBEk"])
    s.dma(QBB[0:64, :], kab[64:128, :], w=["B1lo"])
    s.dma(QBB[64:128, :], qab[64:128, :], w=["B1hi"])
    s.dma(QBL[0:64, :], qab[64:128, :], w=["QBL"])
    s.dma(BIG[2][:, :], vbP, w=["VB"])
    kmT = A("kmT", [128, 64], F32)
    qgf = [A(f"qgf{i}", [128, 128], F32) for i in range(2)]
    gsb = [A(f"gsb{i}", [128, 64], F32) for i in range(2)]
    top8 = [A(f"top8{i}", [128, 8], F32) for i in range(2)]
    bt = [A(f"bt{i}", [128, 128], F32) for i in range(2)]
    for i in range(2):
        s.op("pool", lambda g, i=i: g.memset(bt[i][:, :], 0.0), w=[f"bt{i}"])
    s.op("dve", lambda g: g.tensor_reduce(out=kmT[0:64, :], in_=QBB[0:64, :].rearrange("p (n t) -> p n t", t=256), axis=AX.X, op=ALU.add),
         r=["B1lo"], w=["kmT"])
    s.op("dve", lambda g: g.tensor_scalar(out=kmT[0:64, :], in0=kmT[0:64, :], scalar1=1.0 / 256, scalar2=None, op0=ALU.mult),
         r=["kmT"], w=["kmT"])
    SB, SBn = [P[0], P[1]], ["P0", "P1"]
    OB, DB, GP, TP = P[2], P[3], [P[4], P[5]], [P[6], P[7]]
    it = 0

    def gate1(c, tls):
        for tl in tls:
            t = 4 * c + tl
            own = t // 2
            b = tl % 2
            s.op("act", lambda g, b=b, t=t: g.copy(out=qgf[b][0:64, :], in_=QBL[0:64, t * 128:(t + 1) * 128]), r=["QBL"], w=[f"qgf{b}"])
            s.op("pe", lambda g, b=b: g.matmul(GP[b][:, 0:64], lhsT=qgf[b][0:64, :], rhs=kmT[0:64, :], start=True, stop=True),
                 r=[f"qgf{b}", "kmT"], w=[f"P{4 + b}"])
            s.op("dve", lambda g, b=b: g.memset(gsb[b][:, :], -1e30), w=[f"gsb{b}"])
            if own > 0:
                s.op("dve", lambda g, b=b, own=own: g.tensor_copy(out=gsb[b][:, 0:own], in_=GP[b][:, 0:own]), r=[f"P{4 + b}"], w=[f"gsb{b}"])
            s.op("dve", lambda g, b=b: g.max(out=top8[b][:, :], in_=gsb[b][:, :]), r=[f"gsb{b}"], w=[f"top8{b}"])
            s.op("dve", lambda g, b=b: g.tensor_scalar(out=bt[b][:, 0:64], in0=gsb[b][:, :], scalar1=top8[b][:, 2:3], scalar2=None, op0=ALU.is_ge),
                 r=[f"gsb{b}", f"top8{b}"], w=[f"bt{b}"])
            s.op("dve", lambda g, b=b: g.tensor_scalar(out=bt[b][:, 0:64], in0=bt[b][:, 0:64], scalar1=-1.0, scalar2=-NEG, op0=ALU.add, op1=ALU.mult),
                 r=[f"bt{b}"], w=[f"bt{b}"])
            s.op("dve", lambda g, b=b, own=own: g.memset(bt[b][:, own:own + 1], 0.0), w=[f"bt{b}"])

    def gate2(c, tls):
        for tl in tls:
            t = 4 * c + tl
            b = tl % 2
            s.op("pe", lambda g, b=b: g.transpose(out=TP[b][:, 0:128], in_=bt[b][:, :], identity=idt[:, :]), r=[f"bt{b}", "idt"], w=[f"P{6 + b}"])
            s.op("act", lambda g, b=b, t=t: g.copy(out=QBB[0:64, t * 128:(t + 1) * 128], in_=TP[b][0:64, 0:128]),
                 r=[f"P{6 + b}", "kmT"], w=[f"B1lo{c}"] if c > 0 else ["B1lo"])

    NCB = S // 512
    gate1(0, (0, 1)); gate2(0, (0, 1)); gate1(0, (2, 3)); gate2(0, (2, 3))
    for c in range(NCB):
        nkb = 4 * c + 4
        half = nkb // 2
        lok = f"B1lo{c}" if c > 0 else "B1lo"

        def qkb(kb, itx):
            j, off, n = causal_rng(c, kb)
            q0 = c * 512 + off
            s.op("pe", lambda g, kb=kb, q0=q0, n=n, itx=itx: g.matmul(SB[itx % 2][:, 0:n], lhsT=KBE[:, kb * 128:(kb + 1) * 128],
                                                                     rhs=QBB[:, q0:q0 + n], start=True, stop=True),
                 r=["KBE", "KBEk", lok, "B1hi"], w=[SBn[itx % 2]])
        qkb(0, it)
        for kb in range(nkb):
            if c + 1 < NCB:
                if kb == 0:
                    gate1(c + 1, (0, 1))
                elif kb == half:
                    gate2(c + 1, (0, 1))
                    gate1(c + 1, (2, 3))
            if kb + 1 < nkb:
                qkb(kb + 1, it + 1)
            j, off, n = causal_rng(c, kb)
            p_, pn = pt[0][it % 2], f"pt0{it % 2}"
            s.op("act", lambda g, p_=p_, n=n, it=it: g.activation(out=p_[:, 0:n], in_=SB[it % 2][:, 0:n], func=AF.Exp, bias=negM[:, 1:2], scale=0.125),
                 r=[SBn[it % 2], "negM"], w=[pn])
            if j >= 0:
                s.op("pool", lambda g, p_=p_: g.tensor_tensor(out=p_[:, 0:128], in0=p_[:, 0:128], in1=tri, op=ALU.mult), r=[pn, "m2"], w=[pn])
            s.op("pe", lambda g, p_=p_, n=n, off=off, kb=kb: g.matmul(OB[:, off:512], lhsT=VB[:, kb, :], rhs=p_[:, 0:n],
                                                                     start=(kb == 0), stop=(kb == nkb - 1)), r=[pn, "VB"], w=["P2"])
            it += 1
        if c + 1 < NCB:
            gate2(c + 1, (2, 3))
        s.op("act", lambda g: g.copy(out=e[1][:, :], in_=OB[:, :]), r=["P2"], w=["e1"])
        s.op("pe", lambda g: g.matmul(DB[0:64, :], lhsT=idt[:, 64:128], rhs=e[1][:, :], start=True, stop=True), r=["idt", "e1"], w=["P3"])
        s.op("dve", lambda g: g.reciprocal(out=e[0][0:64, :], in_=DB[0:64, :]), r=["P3"], w=["e0"])
        o_ = ob[c % 2]
        s.op("dve", lambda g, o_=o_: g.tensor_tensor(out=o_[0:64, :], in0=e[1][0:64, :], in1=e[0][0:64, :], op=ALU.mult), r=["e1", "e0"], w=[f"ob{c % 2}"])
        s.dma(oT[64:128, c * 512:(c + 1) * 512], o_[0:64, :], r=[f"ob{c % 2}"], is_out=True)

    s.barrier()
    KA, QA = BIG[0], BIG[1]
    VA = [BIG[2][:, 0:8192].rearrange("p (t d) -> p t d", d=64), BIG[2][:, 8192:16384].rearrange("p (t d) -> p t d", d=64),
          BIG[3][:, 0:8192].rearrange("p (t d) -> p t d", d=64)]
    s.dma(KA[0:64, :], kab[0:64, :], w=["KA"])
    s.dma(QA[0:64, :], qab[0:64, :], w=["QA"])
    s.dma(BIG[2][:, :], vaP[:, 0:16384], w=["VA"])
    s.dma(BIG[3][:, 0:8192], vaP[:, 16384:24576], w=["VA2"])
    ND = A("ND", [128, 2, 2048], F32)
    oa = A("oa", [128, 2048], BF16)
    SA, SAn = [P[0], P[1], P[2], P[3]], ["P0", "P1", "P2", "P3"]
    OA, OAn = [P[4], P[5]], ["P4", "P5"]
    DA, DAn = [P[6], P[7]], ["P6", "P7"]
    ptA = [pt[0][0], pt[0][1], pt[1][0], pt[1][1]]
    ptAn = ["pt00", "pt01", "pt10", "pt11"]
    for s8 in range(S // 2048):
        units = []
        for pi, d in enumerate((1, 4, 16)):
            nbl = 16 // d
            for r in range(d):
                for bl in range(nbl):
                    units.append((pi, d, r, s8 * nbl + bl))

        def a_qk(u, i):
            pi, d, r, bg = u
            st = 128 * d * bg + r
            qs = slice(st, st + 127 * d + 1, d)
            b4 = i % 4
            if bg > 0:
                ps_ = slice(st - 128 * d, st - 128 * d + 127 * d + 1, d)
                s.op("pe", lambda g, b4=b4, ps_=ps_, qs=qs: g.matmul(SA[b4][:, 0:128], lhsT=KA[0:64, ps_], rhs=QA[0:64, qs], start=True, stop=True),
                     r=["KA", "QA"], w=[SAn[b4]])
            s.op("pe", lambda g, b4=b4, qs=qs: g.matmul(SA[b4][:, 128:256], lhsT=KA[0:64, qs], rhs=QA[0:64, qs], start=True, stop=True),
                 r=["KA", "QA"], w=[SAn[b4]])

        def a_rest(u, i):
            pi, d, r, bg = u
            st = 128 * d * bg + r
            off = st - s8 * 2048
            osl = slice(off, off + 127 * d + 1, d)
            tile_i = r * (128 // d) + bg
            b4, b = i % 4, i % 2
            lo = 0 if bg > 0 else 128
            p_, pn = ptA[b4], ptAn[b4]
            s.op("act", lambda g, b4=b4, p_=p_, lo=lo: g.activation(out=p_[:, lo:256], in_=SA[b4][:, lo:256], func=AF.Exp, bias=negM[:, 0:1], scale=0.125),
                 r=[SAn[b4], "negM"], w=[pn])
            s.op("pool", lambda g, p_=p_, lo=lo: g.tensor_tensor(out=p_[:, lo:256], in0=p_[:, lo:256], in1=m2[:, lo:256], op=ALU.mult),
                 r=[pn, "m2"], w=[pn])
            first = True
            for isv in (True, False):
                c0 = 0 if isv else 128
                lp = (VA[pi][:, tile_i - 1, :] if bg > 0 else None) if isv else ones_b[:, 0:64]
                lcur = VA[pi][:, tile_i, :] if isv else ones_b[:, 0:64]
                vkeys = ["VA", "VA2"] if isv else ["ones_b"]
                if bg > 0:
                    s.op("pe", lambda g, b=b, lp=lp, p_=p_, c0=c0, first=first: g.matmul(OA[b][0:64, c0:c0 + 128], lhsT=lp, rhs=p_[:, 0:128], start=first, stop=False),
                         r=[pn] + vkeys, w=[OAn[b]])
                    first = False
                s.op("pe", lambda g, b=b, lcur=lcur, p_=p_, c0=c0, first=first, isv=isv: g.matmul(OA[b][0:64, c0:c0 + 128], lhsT=lcur, rhs=p_[:, 128:256],
                                                                                                 start=first, stop=(not isv)),
                     r=[pn] + vkeys, w=[OAn[b]])
                first = False
            ndv = ND[0:64, :, osl]
            pv = OA[b][0:64, 0:256].rearrange("p (t q) -> p t q", t=2)
            if pi == 0:
                s.op("dve", lambda g, ndv=ndv, pv=pv: g.tensor_copy(out=ndv, in_=pv), r=[OAn[b]], w=["ND"])
            else:
                s.op("dve", lambda g, ndv=ndv, pv=pv: g.tensor_tensor(out=ndv, in0=ndv, in1=pv, op=ALU.add), r=[OAn[b], "ND"], w=["ND"])

        nu = len(units)
        a_qk(units[0], 0)
        a_qk(units[1], 1)
        for i in range(nu):
            if i + 2 < nu:
                a_qk(units[i + 2], i + 2)
            a_rest(units[i], i)
        s.op("dve", lambda g: g.reciprocal(out=ND[0:64, 1, :], in_=ND[0:64, 1, :]), r=["ND"], w=["ND"])
        s.op("dve", lambda g: g.tensor_tensor(out=oa[0:64, :], in0=ND[0:64, 0, :], in1=ND[0:64, 1, :], op=ALU.mult), r=["ND"], w=["oa"])
        s.dma(oT[0:64, s8 * 2048:(s8 + 1) * 2048], oa[0:64, :], r=["oa"], is_out=True)
    s.finish()
    return nc


def build_B1():
    nc = bass.Bass("TRN2", target_bir_lowering=False)
    NT = TPC // 128
    mTt = _din(nc, "mTt", [NT * 128, KD * 128], BF16)
    woP = _din(nc, "woP", [128, KD * D], F32)
    x = _din(nc, "x", [TPC, D], F32)
    gta = _din(nc, "gta", [128, D], F32)
    gml = _din(nc, "gml", [128, D], F32)
    scm = _din(nc, "scm", [128, D], F32)
    shm = _din(nc, "shm", [128, D], F32)
    wrP = _din(nc, "wrP", [128, KD * 16], F32)
    brb = _din(nc, "brb", [128, 16], F32)
    ident = _din(nc, "ident", [128, 128], F32)
    x1o = _dout(nc, "x1o", [TPC, D], F32)
    h2o = _dout(nc, "h2o", [TPC, D], BF16)
    wto = _dout(nc, "wto", [TPC, 16], F32)
    s = Sched(nc)
    A = nc.alloc_sbuf_tensor
    wo = A("wo", [128, KD, D], BF16)
    gtat, at, sht = A("gtat", [128, D], F32), A("at", [128, D], F32), A("sht", [128, D], F32)
    gmt = A("gmt", [128, D], F32)
    wr = A("wr", [128, KD, 16], F32)
    br = A("br", [128, 16], F32)
    idt = A("idt", [128, 128], F32)
    mt = [A(f"mt{i}", [128, KD, 128], BF16) for i in range(2)]
    xt = [A(f"xt{i}", [128, D], F32) for i in range(2)]
    x1t = [A(f"x1t{i}", [128, D], F32) for i in range(2)]
    h2f = A("h2f", [128, D], F32)
    h2b = [A(f"h2b{i}", [128, D], BF16) for i in range(2)]
    h2T = A("h2T", [128, KD, 128], F32)
    junk = A("junk", [128, D], F32)
    small = A("small", [128, 8], F32)
    rt = {n: A(f"rt_{n}", [128, 16], F32) for n in ("s", "sgb", "p1", "p2", "c1", "c2", "c3", "w")}
    sg2 = A("sg2", [128, 4, 8], F32)
    g4 = {n: A(f"g4_{n}", [128, 4], F32) for n in ("a", "b", "sel")}
    wtt = [A(f"wtt{i}", [128, 16], F32) for i in range(2)]
    P = [nc.alloc_psum_tensor(f"P{i}", [128, 512], F32) for i in range(8)]

    woP3 = woP.rearrange("p (k n) -> p k n", k=KD)
    for cc in range(4):
        s.dma(wo[:, :, cc * 512:(cc + 1) * 512], woP3[:, :, cc * 512:(cc + 1) * 512], w=[f"wo{cc}"], q="pool")
    for (tile_, src, nm) in ((gtat, gta, "gtat"), (gmt, gml, "gmt"), (at, scm, "at"), (sht, shm, "sht")):
        s.dma(tile_[:, :], src, w=[nm])
    s.dma(wr[:, :, :].rearrange("p k e -> p (k e)"), wrP, w=["wr"])
    s.dma(br[:, :], brb, w=["br"])
    s.dma(idt[:, :], ident, w=["idt"])
    s.op("dve", lambda g: g.tensor_scalar(out=at[:, :], in0=at[:, :], scalar1=1.0, scalar2=None, op0=ALU.add), r=["at"], w=["at"])
    s.op("dve", lambda g: g.tensor_tensor(out=at[:, :], in0=at[:, :], in1=gmt[:, :], op=ALU.mult), r=["at", "gmt"], w=["at"])
    v44 = lambda ap: ap.rearrange("p (g e) -> p g e", g=4)
    h2fb = [h2f, A("h2f1", [128, D], F32)]

    mt3 = mt + [A("mt2", [128, KD, 128], BF16)]
    xt3 = xt + [A("xt2", [128, D], F32)]

    def loads(t):
        b3 = t % 3
        rows = slice(t * 128, (t + 1) * 128)
        s.dma(mt3[b3][:, :, :].rearrange("p k t -> p (k t)"), mTt[rows, :], w=[f"mt{b3}"])
        s.dma(xt3[b3][:, :], x[rows, :], w=[f"xt{b3}"])

    def stage1(t):
        b = t % 2
        b3 = t % 3
        rows = slice(t * 128, (t + 1) * 128)
        hf, hfn = h2fb[b], f"h2f{b}"
        for cc in range(4):
            cs = slice(cc * 512, (cc + 1) * 512)
            for k in range(KD):
                s.op("pe", lambda g, cc=cc, k=k, b3=b3, cs=cs: g.matmul(P[cc][:, :], lhsT=mt3[b3][:, k, :], rhs=wo[:, k, cs],
                                                                        start=(k == 0), stop=(k == KD - 1)), r=[f"mt{b3}", f"wo{cc}"], w=[f"P{cc}"])
            s.op("dve", lambda g, cc=cc, b=b, cs=cs: g.tensor_tensor(out=x1t[b][:, cs], in0=P[cc][:, :], in1=gtat[:, cs], op=ALU.mult),
                 r=[f"P{cc}", "gtat"], w=[f"x1t{b}"])
            s.op("pool", lambda g, b=b, b3=b3, cs=cs: g.tensor_tensor(out=x1t[b][:, cs], in0=x1t[b][:, cs], in1=xt3[b3][:, cs], op=ALU.add),
                 r=[f"x1t{b}", f"xt{b3}"], w=[f"x1t{b}"])
        s.dma(x1o[rows, :], x1t[b][:, :], r=[f"x1t{b}"], is_out=True)
        s.op("act", lambda g, b=b: g.activation(out=junk[:, :], in_=x1t[b][:, :], func=AF.Square, accum_out=small[:, 0:1]),
             r=[f"x1t{b}"], w=["junk", "ss"])
        _rms_rstd(s, nc, small[:, 0:1], small[:, 1:2], small[:, 2:3], "ss", "rstd", D)
        s.op("dve", lambda g, b=b, hf=hf: g.scalar_tensor_tensor(out=hf[:, :], in0=x1t[b][:, :], scalar=small[:, 1:2], in1=at[:, :], op0=ALU.mult, op1=ALU.mult),
             r=[f"x1t{b}", "rstd", "at"], w=[hfn])
        s.op("pool", lambda g, hf=hf: g.tensor_tensor(out=hf[:, :], in0=hf[:, :], in1=sht[:, :], op=ALU.add), r=[hfn, "sht"], w=[hfn])
        s.op("act", lambda g, b=b, hf=hf: g.copy(out=h2b[b][:, :], in_=hf[:, :]), r=[hfn], w=[f"h2b{b}"])
        s.dma(h2o[rows, :], h2b[b][:, :], r=[f"h2b{b}"], is_out=True)

    def stage2(t):
        b = t % 2
        rows = slice(t * 128, (t + 1) * 128)
        hf, hfn = h2fb[b], f"h2f{b}"
        for k4 in range(4):
            pb, pbn = P[4 + k4], f"P{4 + k4}"
            for kk in range(4):
                k = k4 * 4 + kk
                s.op("pe", lambda g, pb=pb, kk=kk, k=k, hf=hf: g.transpose(out=pb[:, kk * 128:(kk + 1) * 128], in_=hf[:, k * 128:(k + 1) * 128], identity=idt[:, :]),
                     r=[hfn, "idt"], w=[pbn])
            dst = h2T[:, k4 * 4:(k4 + 1) * 4, :]
            srcv = pb[:, :].rearrange("p (k t) -> p k t", k=4)
            s.op("act", lambda g, dst=dst, srcv=srcv: g.copy(out=dst, in_=srcv), r=[pbn], w=["h2T"])
        for k in range(KD):
            s.op("pe", lambda g, k=k: g.matmul(P[4][:, 0:16], lhsT=h2T[:, k, :], rhs=wr[:, k, :], start=(k == 0), stop=(k == KD - 1)),
                 r=["h2T", "wr"], w=["P4"])
        R = rt
        s.op("act", lambda g: g.activation(out=R["s"][:, :], in_=P[4][:, 0:16], func=AF.Sigmoid), r=["P4"], w=["r_s"])
        s.op("dve", lambda g: g.tensor_tensor(out=R["sgb"][:, :], in0=R["s"][:, :], in1=br[:, :], op=ALU.add), r=["r_s", "br"], w=["r_sgb"])
        s.op("dve", lambda g: g.tensor_copy(out=sg2[:, :, 0:4], in_=v44(R["sgb"][:, :])), r=["r_sgb"], w=["sg2"])
        s.op("dve", lambda g: g.tensor_copy(out=sg2[:, :, 4:8], in_=v44(R["sgb"][:, :])), r=["r_sgb", "sg2"], w=["sg2"])
        s.op("dve", lambda g: g.tensor_tensor(out=v44(R["p1"][:, :]), in0=sg2[:, :, 0:4], in1=sg2[:, :, 1:5], op=ALU.add), r=["sg2"], w=["r_p1"])
        s.op("dve", lambda g: g.tensor_tensor(out=v44(R["p2"][:, :]), in0=sg2[:, :, 0:4], in1=sg2[:, :, 2:6], op=ALU.add), r=["sg2"], w=["r_p2"])
        s.op("dve", lambda g: g.tensor_tensor(out=R["p1"][:, :], in0=R["p1"][:, :], in1=R["p2"][:, :], op=ALU.max), r=["r_p1", "r_p2"], w=["r_p1"])
        s.op("dve", lambda g: g.tensor_reduce(out=g4["a"][:, :], in_=v44(R["p1"][:, :]), axis=AX.X, op=ALU.max), r=["r_p1"], w=["g4a"])
        s.op("dve", lambda g: g.tensor_reduce(out=small[:, 3:4], in_=g4["a"][:, :], axis=AX.X, op=ALU.max), r=["g4a"], w=["gmax"])
        s.op("dve", lambda g: g.tensor_scalar(out=g4["sel"][:, :], in0=g4["a"][:, :], scalar1=small[:, 3:4], scalar2=None, op0=ALU.is_ge),
             r=["g4a", "gmax"], w=["g4sel"])
        for r_, cn in ((1, "c1"), (2, "c2"), (3, "c3")):
            s.op("dve", lambda g, r_=r_, cn=cn: g.tensor_tensor(out=v44(R[cn][:, :]), in0=sg2[:, :, r_:r_ + 4], in1=sg2[:, :, 0:4], op=ALU.is_gt),
                 r=["sg2"], w=[f"r_{cn}"])
        s.op("dve", lambda g: g.tensor_tensor(out=R["c1"][:, :], in0=R["c1"][:, :], in1=R["c2"][:, :], op=ALU.add), r=["r_c1", "r_c2"], w=["r_c1"])
        s.op("dve", lambda g: g.tensor_tensor(out=R["c1"][:, :], in0=R["c1"][:, :], in1=R["c3"][:, :], op=ALU.add), r=["r_c1", "r_c3"], w=["r_c1"])
        s.op("dve", lambda g: g.tensor_scalar(out=R["c1"][:, :], in0=R["c1"][:, :], scalar1=1.5, scalar2=None, op0=ALU.is_lt), r=["r_c1"], w=["r_c1"])
        s.op("dve", lambda g: g.tensor_tensor(out=v44(R["c1"][:, :]), in0=v44(R["c1"][:, :]), in1=g4["sel"][:, :].unsqueeze(2).broadcast_to([128, 4, 4]), op=ALU.mult),
             r=["r_c1", "g4sel"], w=["r_c1"])
        s.op("dve", lambda g: g.tensor_tensor(out=R["w"][:, :], in0=R["s"][:, :], in1=R["c1"][:, :], op=ALU.mult), r=["r_s", "r_c1"], w=["r_w"])
        s.op("dve", lambda g: g.tensor_reduce(out=small[:, 4:5], in_=R["w"][:, :], axis=AX.X, op=ALU.add), r=["r_w"], w=["wsum"])
        s.op("dve", lambda g: g.reciprocal(out=small[:, 5:6], in_=small[:, 4:5]), r=["wsum"], w=["rws"])
        s.op("dve", lambda g, b=b: g.tensor_scalar(out=wtt[b][:, :], in0=R["w"][:, :], scalar1=small[:, 5:6], scalar2=None, op0=ALU.mult),
             r=["r_w", "rws"], w=[f"wtt{b}"])
        s.dma(wto[rows, :], wtt[b][:, :], r=[f"wtt{b}"], is_out=True)

    loads(0)
    loads(1)
    stage1(0)
    for t in range(NT):
        if t + 2 < NT:
            loads(t + 2)
        if t + 1 < NT:
            stage1(t + 1)
        stage2(t)
    s.finish()
    return nc


TB = 1024
NTB = 8
DE = 1024


def build_B2():
    nc = bass.Bass("TRN2", target_bir_lowering=False)
    h2T = _din(nc, "h2T", [NTB * 128, KD * TB], BF16)
    wgP = _din(nc, "wgP", [4 * 8 * 128, KD * 128], F32)
    wuP = _din(nc, "wuP", [4 * 8 * 128, KD * 128], F32)
    wdP = _din(nc, "wdP", [4 * 128, 8 * D], F32)
    wtP = _din(nc, "wtP", [128, 64 * 4], F32)
    yo = _dout(nc, "yo", [NTB * TB, D], BF16)
    s = Sched(nc)
    A = nc.alloc_sbuf_tensor
    HB = A("HB", [128, KD, TB], BF16)
    WD = A("WD", [128, 8, D], BF16)
    WG = [A(f"WG{i}", [128, KD, 128], BF16) for i in range(2)]
    WU = [A(f"WU{i}", [128, KD, 128], BF16) for i in range(2)]
    actT = A("actT", [128, 8, TB], BF16)
    acc = A("acc", [128, TB // 128, D], F32)
    sg = [A(f"sg{i}", [128, 512], F32) for i in range(2)]
    yb = [A(f"yb{i}", [128, D], BF16) for i in range(2)]
    wt = A("wt", [128, 64, 4], F32)
    P = [nc.alloc_psum_tensor(f"P{i}", [128, 512], F32) for i in range(8)]
    s.dma(wt[:, :, :].rearrange("p t e -> p (t e)"), wtP, w=["wt"])
    ig = 0
    iy = 0
    s.dma(HB[:, :, :].rearrange("p k t -> p (k t)"), h2T[0:128, :], w=["HB"])
    for blk in range(NTB):
        for e in range(4):
            for f in range(8):
                fb = f % 2
                r0 = (e * 8 + f) * 128
                s.dma(WG[fb][:, :, :].rearrange("p k c -> p (k c)"), wgP[r0:r0 + 128, :], w=[f"WG{fb}"], q="pool")
                s.dma(WU[fb][:, :, :].rearrange("p k c -> p (k c)"), wuP[r0:r0 + 128, :], w=[f"WU{fb}"], q="pool")
                if f == 1:
                    s.dma(WD[:, :, :].rearrange("p f n -> p (f n)"), wdP[e * 128:(e + 1) * 128, :], w=["WD"], q="pool")
                for ch in range(TB // 512):
                    cs = slice(ch * 512, (ch + 1) * 512)
                    pg, pu = P[ig % 2], P[2 + ig % 2]
                    pgn, pun = f"P{ig % 2}", f"P{2 + ig % 2}"
                    for k in range(KD):
                        s.op("pe", lambda g, pg=pg, k=k, fb=fb, cs=cs: g.matmul(pg[:, :], lhsT=WG[fb][:, k, :], rhs=HB[:, k, cs], start=(k == 0), stop=(k == KD - 1)),
                             r=[f"WG{fb}", "HB"], w=[pgn])
                    for k in range(KD):
                        s.op("pe", lambda g, pu=pu, k=k, fb=fb, cs=cs: g.matmul(pu[:, :], lhsT=WU[fb][:, k, :], rhs=HB[:, k, cs], start=(k == 0), stop=(k == KD - 1)),
                             r=[f"WU{fb}", "HB"], w=[pun])
                    sgt, sgn = sg[ig % 2], f"sg{ig % 2}"
                    s.op("act", lambda g, pg=pg, sgt=sgt: g.activation(out=sgt[:, :], in_=pg[:, :], func=AF.Silu), r=[pgn], w=[sgn])
                    s.op("dve", lambda g, pu=pu, sgt=sgt, f=f, cs=cs: g.tensor_tensor(out=actT[:, f, cs], in0=pu[:, :], in1=sgt[:, :], op=ALU.mult),
                         r=[pun, sgn], w=["actT"])
                    ig += 1
            if e == 3 and blk + 1 < NTB:
                s.dma(HB[:, :, :].rearrange("p k t -> p (k t)"), h2T[(blk + 1) * 128:(blk + 2) * 128, :], w=["HB"])
            for tt in range(TB // 128):
                tile_i = blk * (TB // 128) + tt
                for cc in range(4):
                    cs = slice(cc * 512, (cc + 1) * 512)
                    py, pyn = P[4 + iy % 4], f"P{4 + iy % 4}"
                    for f in range(8):
                        s.op("pe", lambda g, py=py, f=f, tt=tt, cs=cs: g.matmul(py[:, :], lhsT=actT[:, f, tt * 128:(tt + 1) * 128], rhs=WD[:, f, cs],
                                                                               start=(f == 0), stop=(f == 7)), r=["actT", "WD"], w=[pyn])
                    wsc = wt[:, tile_i, e:e + 1]
                    if e == 0:
                        s.op("dve", lambda g, py=py, tt=tt, cs=cs, wsc=wsc: g.tensor_scalar(out=acc[:, tt, cs], in0=py[:, :], scalar1=wsc, scalar2=None, op0=ALU.mult),
                             r=[pyn, "wt"], w=[f"acc{tt}"])
                    else:
                        s.op("dve", lambda g, py=py, tt=tt, cs=cs, wsc=wsc: g.scalar_tensor_tensor(out=acc[:, tt, cs], in0=py[:, :], scalar=wsc, in1=acc[:, tt, cs],
                                                                                                  op0=ALU.mult, op1=ALU.add),
                             r=[pyn, "wt", f"acc{tt}"], w=[f"acc{tt}"])
                    iy += 1
        for tt in range(TB // 128):
            b = tt % 2
            s.op("act", lambda g, b=b, tt=tt: g.copy(out=yb[b][:, :], in_=acc[:, tt, :]), r=[f"acc{tt}"], w=[f"yb{b}"])
            r0 = blk * TB + tt * 128
            s.dma(yo[r0:r0 + 128, :], yb[b][:, :], r=[f"yb{b}"], is_out=True)
    s.finish()
    return nc


def build_F():
    nc = bass.Bass("TRN2", target_bir_lowering=False)
    x = _din(nc, "x", [TPC, D], F32)
    ys = [_din(nc, f"y{g}", [TPC, D], BF16) for g in range(4)]
    gtm = _din(nc, "gtm", [128, D], F32)
    gfin = _din(nc, "gfin", [128, D], F32)
    out = _dout(nc, "out", [TPC, D], F32)
    s = Sched(nc)
    A = nc.alloc_sbuf_tensor
    xt = [A(f"xt{i}", [128, D], F32) for i in range(3)]
    yt = [[A(f"yt{g}{i}", [128, D], BF16) for i in range(3)] for g in range(4)]
    gtt, gft = A("gtt", [128, D], F32), A("gft", [128, D], F32)
    t1 = A("t1", [128, D], F32)
    junk = A("junk", [128, D], F32)
    small = A("small", [128, 8], F32)
    s.dma(gtt[:, :], gtm, w=["gtt"])
    s.dma(gft[:, :], gfin, w=["gft"])
    NT = TPC // 128

    def loads(t):
        b = t % 3
        rows = slice(t * 128, (t + 1) * 128)
        s.dma(xt[b][:, :], x[rows, :], w=[f"xt{b}"])
        for g in range(4):
            s.dma(yt[g][b][:, :], ys[g][rows, :], w=[f"yt{g}{b}"])

    loads(0)
    loads(1)
    for t in range(NT):
        if t + 2 < NT:
            loads(t + 2)
        b = t % 3
        rows = slice(t * 128, (t + 1) * 128)
        s.op("pool", lambda e, b=b: e.tensor_tensor(out=yt[0][b][:, :], in0=yt[0][b][:, :], in1=yt[1][b][:, :], op=ALU.add), r=[f"yt0{b}", f"yt1{b}"], w=[f"yt0{b}"])
        s.op("pool", lambda e, b=b: e.tensor_tensor(out=yt[2][b][:, :], in0=yt[2][b][:, :], in1=yt[3][b][:, :], op=ALU.add), r=[f"yt2{b}", f"yt3{b}"], w=[f"yt2{b}"])
        s.op("dve", lambda e, b=b: e.tensor_tensor(out=t1[:, :], in0=yt[0][b][:, :], in1=yt[2][b][:, :], op=ALU.add), r=[f"yt0{b}", f"yt2{b}"], w=["t1"])
        s.op("dve", lambda e: e.tensor_tensor(out=t1[:, :], in0=t1[:, :], in1=gtt[:, :], op=ALU.mult), r=["t1", "gtt"], w=["t1"])
        s.op("dve", lambda e, b=b: e.tensor_tensor(out=xt[b][:, :], in0=xt[b][:, :], in1=t1[:, :], op=ALU.add), r=["t1", f"xt{b}"], w=[f"xt{b}"])
        s.op("act", lambda e, b=b: e.activation(out=junk[:, :], in_=xt[b][:, :], func=AF.Square, accum_out=small[:, 0:1]), r=[f"xt{b}"], w=["junk", "ss"])
        _rms_rstd(s, nc, small[:, 0:1], small[:, 1:2], small[:, 2:3], "ss", "rstd", D)
        s.op("dve", lambda e, b=b: e.scalar_tensor_tensor(out=xt[b][:, :], in0=xt[b][:, :], scalar=small[:, 1:2], in1=gft[:, :], op0=ALU.mult, op1=ALU.mult),
             r=[f"xt{b}", "rstd", "gft"], w=[f"xt{b}"])
        s.dma(out[rows, :], xt[b][:, :], r=[f"xt{b}"], is_out=True)
    s.finish()
    return nc


_PROGS = {}


def _prog(name, fn, *a):
    key = (name,) + tuple(a)
    if key not in _PROGS:
        _PROGS[key] = fn(*a)
    return _PROGS[key]


def _run(nc, in_maps):
    res = run_bass_kernel_spmd(nc, in_maps, core_ids=list(range(NCORE)))
    return res.results


def _pk(v):
    return np.ascontiguousarray(np.asarray(v, np.float32).reshape(KD, 128).T)


def _bc(v):
    v = np.asarray(v, np.float32).reshape(1, -1)
    return np.ascontiguousarray(np.broadcast_to(v, (128, v.shape[1])))


def _colidx():
    idx = []
    for h in range(8):
        qa = list(range(0 + h * 64, 0 + h * 64 + 64)); ka = [512 + c for c in qa]; va = [1024 + c for c in qa]
        qb = [1536 + c for c in qa]; kb = [2048 + c for c in qa]; vb = [2560 + c for c in qa]
        qc = list(range(3072 + h * 128, 3072 + h * 128 + 128)); kc = [1024 + c for c in qc]; vc = [2048 + c for c in qc]
        idx += qa + qb + ka + kb + qc + kc + va + vb + vc
    return np.asarray(idx)


def _consts():
    inv = (10000.0 ** (-np.arange(0, 64, 2, dtype=np.float32) / 64)).astype(np.float32)
    invf = np.tile(inv, 4).reshape(128, 1).astype(np.float32)
    perm = np.zeros((128, 128), np.float32)
    for m in range(128):
        if m % 64 < 32:
            perm[m + 32, m] = -1.0
        else:
            perm[m - 32, m] = 1.0
    bones = np.zeros((128, 128), np.float32)
    bones[:64, :64] = 1.0
    bones[64:, 64:] = 1.0
    return dict(invf=invf, perm=perm, ident=np.eye(128, dtype=np.float32), bones=bones)


def host_M(inp):
    nc = _prog("M", build_M)
    cT = _pk(inp["c"].reshape(-1))
    maps = []
    for i in range(NCORE):
        cols = slice(i * MCOL, (i + 1) * MCOL)
        wa = np.concatenate([inp["w_ada"][0][:, cols], inp["w_ada"][1][:, cols]], axis=0)
        ba = np.concatenate([inp["b_ada"][0][cols], inp["b_ada"][1][cols]]).reshape(1, -1)
        maps.append(dict(cT=cT, wada=np.ascontiguousarray(wa), bada=np.ascontiguousarray(ba)))
    res = _run(nc, maps)
    mod = np.zeros((2, 6 * D), np.float32)
    for i in range(NCORE):
        r = res[i]["mod"].reshape(2, MCOL)
        mod[:, i * MCOL:(i + 1) * MCOL] = r
    return mod


def host_PA1(inp, l, mod, x, ys=None):
    add = ys is not None
    nc = _prog("PA1", build_PA1, add)
    cst = _consts()
    sh_a, sc_a = mod[l, 0:D], mod[l, D:2 * D]
    wperm = inp["w_in"][l][:, _colidx()]
    wl = np.ascontiguousarray(wperm.reshape(KD, 128, NG, 128).transpose(2, 1, 0, 3)).reshape(NG * 128, KD * 128)
    pos = np.asarray(inp["positions"]).reshape(-1).astype(np.int32)
    maps = []
    for i in range(NCORE):
        rows = slice(i * TPC, (i + 1) * TPC)
        m = dict(x=np.ascontiguousarray(x[rows]), win=wl, gA=_pk(inp["g_attn"][l]), scA=_pk(sc_a), shA=_pk(sh_a),
                 pos=np.ascontiguousarray(pos[rows].reshape(1, -1)), **cst)
        if add:
            gt_m_prev = mod[l - 1, 5 * D:6 * D]
            for g in range(4):
                m[f"y{g}"] = np.ascontiguousarray(ys[g][rows])
            m["gtm"] = _bc(gt_m_prev)
        maps.append(m)
    res = _run(nc, maps)
    qkvT = np.concatenate([res[i]["qkvT"] for i in range(NCORE)], axis=1)
    nrm = np.stack([res[i]["nrm"] for i in range(NCORE)], axis=0)
    xnew = np.concatenate([res[i]["xnew"] for i in range(NCORE)], axis=0) if add else x
    return qkvT, nrm, xnew


def _a2_consts():
    k = np.arange(128)[:, None]
    q = np.arange(128)[None, :]
    mask2 = np.concatenate([(k >= q), (k <= q)], axis=1).astype(np.float32)
    E = (np.arange(S)[None, :] // 256 == np.arange(64)[:, None]).astype(NPBF)
    return mask2, E


def _tiles(vT):
    d = vT.shape[0]
    return np.ascontiguousarray(vT.T.reshape(S // 128, 128, d).transpose(1, 0, 2)).reshape(128, -1)


def host_A2(inp, l, qkvT, nrm):
    nc = _prog("A2", build_A2)
    mask2, E = _a2_consts()
    lam_init = 0.8 - 0.6 * math.exp(-0.3 * l)
    lamt = np.concatenate([_bc(inp["lam_q1"][l]), _bc(inp["lam_k1"][l]), _bc(inp["lam_q2"][l]), _bc(inp["lam_k2"][l])], axis=1)
    lamc = _bc(np.asarray([lam_init, 1.0 - lam_init], np.float32))
    gsub = np.asarray(inp["g_subln"][l], np.float32).reshape(128, 1)
    ones64 = np.ones((64, S), NPBF)
    maps = []
    for h in range(NCORE):
        G = lambda j: qkvT[(h * 6 + j) * 128:(h * 6 + j + 1) * 128]
        vab = G(4)
        va = vab[0:64]
        vas = []
        for d in (1, 4, 16):
            perm_tok = np.arange(S).reshape(S // d, d).T.reshape(-1)
            vas.append(_tiles(va[:, perm_tok]))
        nb = np.zeros((8, 8), np.float32)
        for j in range(4):
            nb[2 * j] = nrm[:, 0, h * 4 + j]
            nb[2 * j + 1] = nrm[:, 64, h * 4 + j]
        maps.append(dict(qab=np.ascontiguousarray(G(0)), kab=np.ascontiguousarray(G(1)), qcT=np.ascontiguousarray(G(2)),
                         kcT=np.ascontiguousarray(G(3)), vcP=_tiles(G(5)), vbP=_tiles(np.concatenate([vab[64:128], ones64], axis=0)),
                         vaP=np.ascontiguousarray(np.concatenate(vas, axis=1)), Ecs=E, nrmb=_bc(nb.reshape(-1)),
                         lamt=lamt, lamc=lamc, gsub=gsub, mask2=mask2, ident=np.eye(128, dtype=np.float32)))
    res = _run(nc, maps)
    return [res[h]["oT"] for h in range(NCORE)]


def _rowidx_out():
    idx = []
    for h in range(8):
        idx += list(range(h * 64, h * 64 + 64)) + list(range(512 + h * 64, 512 + h * 64 + 64)) + list(range(1024 + h * 128, 1024 + h * 128 + 128))
    return np.asarray(idx)


def host_B1(inp, l, mod, x, oT):
    nc = _prog("B1", build_B1)
    mT = np.concatenate(oT, axis=0)
    wo = inp["w_out"][l][_rowidx_out(), :]
    woP = np.ascontiguousarray(wo.reshape(KD, 128, D).transpose(1, 0, 2)).reshape(128, KD * D)
    wrP = np.ascontiguousarray(np.asarray(inp["w_router"], np.float32).reshape(KD, 128, 16).transpose(1, 0, 2)).reshape(128, KD * 16)
    com = dict(woP=woP, gta=_bc(mod[l, 2 * D:3 * D]), gml=_bc(inp["g_mlp"][l]), scm=_bc(mod[l, 4 * D:5 * D]), shm=_bc(mod[l, 3 * D:4 * D]),
               wrP=wrP, brb=_bc(inp["b_router"]), ident=np.eye(128, dtype=np.float32))
    maps = []
    for i in range(NCORE):
        rows = slice(i * TPC, (i + 1) * TPC)
        mc = mT[:, rows].reshape(KD, 128, TPC // 128, 128).transpose(2, 1, 0, 3)
        maps.append(dict(mTt=np.ascontiguousarray(mc).reshape(TPC, KD * 128), x=np.ascontiguousarray(x[rows]), **com))
    res = _run(nc, maps)
    cat = lambda n: np.concatenate([res[i][n] for i in range(NCORE)], axis=0)
    return cat("x1o"), cat("h2o"), cat("wto")


def host_B2(inp, l, h2, wt):
    nc = _prog("B2", build_B2)
    HALF = S // 2
    maps = []
    for i in range(NCORE):
        t2, g = divmod(i, 4)
        rows = slice(t2 * HALF, (t2 + 1) * HALF)
        hh = h2[rows].reshape(NTB, TB, KD, 128).transpose(0, 3, 2, 1)
        wg = np.stack([inp["w_gate"][l][4 * g + e].reshape(KD, 128, 8, 128).transpose(2, 1, 0, 3) for e in range(4)])
        wu = np.stack([inp["w_up"][l][4 * g + e].reshape(KD, 128, 8, 128).transpose(2, 1, 0, 3) for e in range(4)])
        wd = np.stack([inp["w_down"][l][4 * g + e].reshape(8, 128, D).transpose(1, 0, 2) for e in range(4)])
        wtp = wt[rows, 4 * g:4 * g + 4].reshape(HALF // 128, 128, 4).transpose(1, 0, 2)
        maps.append(dict(h2T=np.ascontiguousarray(hh).reshape(NTB * 128, KD * TB),
                         wgP=np.ascontiguousarray(wg).reshape(4 * 8 * 128, KD * 128),
                         wuP=np.ascontiguousarray(wu).reshape(4 * 8 * 128, KD * 128),
                         wdP=np.ascontiguousarray(wd).reshape(4 * 128, 8 * D),
                         wtP=np.ascontiguousarray(wtp).reshape(128, -1)))
    res = _run(nc, maps)
    ys = []
    for g in range(4):
        ys.append(np.concatenate([res[0 * 4 + g]["yo"], res[1 * 4 + g]["yo"]], axis=0))
    return ys


def host_F(inp, mod, x1, ys):
    nc = _prog("F", build_F)
    com = dict(gtm=_bc(mod[1, 5 * D:6 * D]), gfin=_bc(inp["g_final"]))
    maps = []
    for i in range(NCORE):
        rows = slice(i * TPC, (i + 1) * TPC)
        m = dict(x=np.ascontiguousarray(x1[rows]), **com)
        for g in range(4):
            m[f"y{g}"] = np.ascontiguousarray(ys[g][rows])
        maps.append(m)
    res = _run(nc, maps)
    return np.concatenate([res[i]["out"] for i in range(NCORE)], axis=0)


def kernel(**inputs):
    inp = {k: np.asarray(v) for k, v in inputs.items()}
    mod = host_M(inp)
    x = np.asarray(inp["x"], np.float32).reshape(S, D)
    ys = None
    x1 = None
    for l in range(2):
        qkvT, nrm, x = host_PA1(inp, l, mod, x if l == 0 else x1, ys)
        oT = host_A2(inp, l, qkvT, nrm)
        x1, h2, wt = host_B1(inp, l, mod, x, oT)
        ys = host_B2(inp, l, h2, wt)
    out = host_F(inp, mod, x1, ys)
    return out.reshape(1, S, D).astype(np.float32)
```

```python
import math
import numpy as np
import ml_dtypes
import concourse.bass as bass
import concourse.mybir as mybir
from concourse.bass_utils import run_bass_kernel_spmd

F32 = mybir.dt.float32
BF16 = mybir.dt.bfloat16
I32 = mybir.dt.int32
AF = mybir.ActivationFunctionType
ALU = mybir.AluOpType
AX = mybir.AxisListType
NPBF = ml_dtypes.bfloat16

D = 2048
S = 16384
NCORE = 8
TPC = S // NCORE
KD = D // 128
EPS = 1e-6
NEG = -30000.0


class _B:
    __slots__ = ("w", "r")

    def __init__(self):
        self.w = None
        self.r = {}


class Sched:
    ENG = ("pe", "act", "dve", "pool", "sp")
    NDS = 40

    def __init__(self, nc):
        self.nc = nc
        self.e = dict(pe=nc.tensor, act=nc.scalar, dve=nc.vector, pool=nc.gpsimd, sp=nc.sync)
        self.sem = {k: nc.alloc_semaphore(f"s_{k}") for k in self.ENG}
        self.cnt = {k: 0 for k in self.ENG}
        self.seen = {k: {} for k in self.ENG}
        self.dsem = [nc.alloc_semaphore(f"d{i}") for i in range(self.NDS)]
        self.dcnt = [0] * self.NDS
        self.dnext = 0
        self.bufs = {}
        self.out_toks = []

    def buf(self, k):
        b = self.bufs.get(k)
        if b is None:
            b = self.bufs[k] = _B()
        return b

    def _semof(self, key):
        return self.sem[key[1]] if key[0] == "e" else self.dsem[key[1]]

    def _wait(self, eng, toks):
        need = {}
        for t in toks:
            if t is None:
                continue
            key = (t[0], t[1])
            if key == ("e", "pe") and eng == "pe":
                continue
            if need.get(key, 0) < t[2]:
                need[key] = t[2]
        for key, val in need.items():
            if self.seen[eng].get(key, 0) >= val:
                continue
            self.e[eng].wait_ge(self._semof(key), val)
            self.seen[eng][key] = val

    def _deps(self, r, w):
        toks = []
        for k in r:
            toks.append(self.buf(k).w)
        for k in w:
            b = self.buf(k)
            toks.append(b.w)
            for key, val in b.r.items():
                toks.append((key[0], key[1], val))
        return toks

    def _mark(self, tok, r, w):
        key = (tok[0], tok[1])
        for k in r:
            b = self.buf(k)
            if b.r.get(key, 0) < tok[2]:
                b.r[key] = tok[2]
        for k in w:
            b = self.buf(k)
            b.w = tok
            b.r = {}

    def op(self, eng, fn, r=(), w=()):
        self._wait(eng, self._deps(r, w))
        ins = fn(self.e[eng])
        self.cnt[eng] += 1
        ins.then_inc(self.sem[eng], 1)
        tok = ("e", eng, self.cnt[eng])
        self._mark(tok, r, w)
        return tok

    def dma(self, out, in_, r=(), w=(), q="sp", is_out=False, **kw):
        j = self.dnext
        self.dnext = (self.dnext + 1) % self.NDS
        toks = self._deps(r, w)
        if self.dcnt[j] > 0:
            toks.append(("d", j, self.dcnt[j]))
        self._wait(q, toks)
        ins = self.e[q].dma_start(out=out, in_=in_, **kw)
        self.dcnt[j] += 16
        ins.then_inc(self.dsem[j], 16)
        tok = ("d", j, self.dcnt[j])
        self._mark(tok, r, w)
        if is_out:
            self.out_toks.append(tok)
        return tok

    def barrier(self):
        toks = [("e", k, self.cnt[k]) for k in ("pe", "act", "dve", "pool") if self.cnt[k] > 0]
        toks += [("d", j, self.dcnt[j]) for j in range(self.NDS) if self.dcnt[j] > 0]
        for eng in self.ENG:
            self._wait(eng, toks)

    def finish(self):
        self._wait("sp", self.out_toks)
        self._wait("sp", [("e", k, self.cnt[k]) for k in ("pe", "act", "dve", "pool") if self.cnt[k] > 0])


def _din(nc, name, shape, dt):
    return nc.dram_tensor(name, list(shape), dt, kind="ExternalInput").ap()


def _dout(nc, name, shape, dt):
    return nc.dram_tensor(name, list(shape), dt, kind="ExternalOutput").ap()


MCOL = 6 * D // NCORE


def build_M():
    nc = bass.Bass("TRN2", target_bir_lowering=False)
    cT = _din(nc, "cT", [128, KD], F32)
    wada = _din(nc, "wada", [2 * D, MCOL], F32)
    bada = _din(nc, "bada", [1, 2 * MCOL], F32)
    mod = _dout(nc, "mod", [1, 2 * MCOL], F32)
    s = Sched(nc)
    ct = nc.alloc_sbuf_tensor("ct", [128, KD], F32)
    sc = nc.alloc_sbuf_tensor("sc", [128, KD], F32)
    sc2 = nc.alloc_sbuf_tensor("sc2", [128, KD, 2], F32)
    bt = nc.alloc_sbuf_tensor("bt", [2, 2 * MCOL], F32)
    res = nc.alloc_sbuf_tensor("res", [2, 2 * MCOL], F32)
    wt = [nc.alloc_sbuf_tensor(f"wt{i}", [128, KD, 512], F32) for i in range(2)]
    pp = [nc.alloc_psum_tensor(f"pp{i}", [128, 512], F32) for i in range(2)]
    s.dma(ct[:, :], cT, w=["ct"])
    s.dma(bt[0:1, :], bada, w=["bt"])
    s.op("act", lambda e: e.activation(out=sc[:, :], in_=ct[:, :], func=AF.Silu), r=["ct"], w=["sc"])
    s.op("dve", lambda e: e.tensor_copy(out=sc2[:, :, 0], in_=sc[:, :]), r=["sc"], w=["sc2a"])
    s.op("dve", lambda e: e.tensor_copy(out=sc2[:, :, 1], in_=sc[:, :]), r=["sc"], w=["sc2b"])
    i = 0
    for l in range(2):
        for blk in range(MCOL // 512):
            b = i % 2
            src = wada[l * D:(l + 1) * D, blk * 512:(blk + 1) * 512].rearrange("(k p) c -> p k c", p=128)
            s.dma(wt[b][:, :, :], src, w=[f"wt{b}"])
            for k in range(KD):
                s.op("pe", lambda e, k=k, b=b: e.matmul(pp[b][0:2, :], lhsT=sc2[:, k, :], rhs=wt[b][:, k, :],
                                                         start=(k == 0), stop=(k == KD - 1)),
                     r=["sc2a", "sc2b", f"wt{b}"], w=[f"pp{b}"])
            c0 = l * MCOL + blk * 512
            s.op("dve", lambda e, b=b, c0=c0: e.tensor_tensor(out=res[0:1, c0:c0 + 512], in0=pp[b][0:1, :],
                                                              in1=bt[0:1, c0:c0 + 512], op=ALU.add),
                 r=[f"pp{b}", "bt"], w=["res"])
            i += 1
    s.dma(mod, res[0:1, :], r=["res"], is_out=True)
    s.finish()
    return nc


NG = 48
TWO_PI_HI = 6.28125
TWO_PI_LO = 2.0 * math.pi - 6.28125
MAGIC = 12582912.0


def _rms_rstd(s, nc, ss, rstd, tmp, tagr, tagw, ncols):
    s.op("dve", lambda e: e.tensor_scalar(out=tmp, in0=ss, scalar1=1.0 / ncols, scalar2=EPS,
                                          op0=ALU.mult, op1=ALU.add), r=[tagr], w=[tagw + "_t"])
    s.op("act", lambda e: e.activation(out=tmp, in_=tmp, func=AF.Sqrt), r=[tagw + "_t"], w=[tagw + "_t"])
    s.op("dve", lambda e: e.reciprocal(out=rstd, in_=tmp), r=[tagw + "_t"], w=[tagw])


def build_PA1(add_moe):
    nc = bass.Bass("TRN2", target_bir_lowering=False)
    x = _din(nc, "x", [TPC, D], F32)
    win = _din(nc, "win", [NG * 128, KD * 128], F32)
    gA = _din(nc, "gA", [128, KD], F32)
    scA = _din(nc, "scA", [128, KD], F32)
    shA = _din(nc, "shA", [128, KD], F32)
    pos = _din(nc, "pos", [1, TPC], I32)
    invf = _din(nc, "invf", [128, 1], F32)
    perm = _din(nc, "perm", [128, 128], F32)
    ident = _din(nc, "ident", [128, 128], F32)
    bones = _din(nc, "bones", [128, 128], F32)
    if add_moe:
        ys = [_din(nc, f"y{g}", [TPC, D], BF16) for g in range(4)]
        gtm = _din(nc, "gtm", [128, D], F32)
        xnew = _dout(nc, "xnew", [TPC, D], F32)
    qkvT = _dout(nc, "qkvT", [NG * 128, TPC], BF16)
    nrm = _dout(nc, "nrm", [128, 32], F32)
    s = Sched(nc)
    A = nc.alloc_sbuf_tensor
    xnT = A("xnT", [128, KD, TPC], BF16)
    cosT = A("cosT", [128, TPC], F32)
    sinT = A("sinT", [128, TPC], F32)
    xt = [A(f"xt{i}", [128, D], F32) for i in range(2)]
    wst = [A(f"wst{i}", [128, KD, 128], F32) for i in range(2)]
    wb = [A(f"wb{i}", [128, KD, 128], BF16) for i in range(2)]
    qf = [A(f"qf{i}", [128, 512], F32) for i in range(2)]
    t1 = A("t1", [128, 512], F32)
    t2 = A("t2", [128, 512], F32)
    qo = [A(f"qo{i}", [128, 512], BF16) for i in range(2)]
    sq = A("sq", [128, 512], BF16)
    qo3 = [A(f"qo3_{i}", [128, 512], BF16) for i in range(3)]
    t2b = [A(f"t2b{i}", [128, 512], F32) for i in range(2)]
    sqb = [A(f"sqb{i}", [128, 512], BF16) for i in range(2)]
    ga = A("ga", [128, KD], F32)
    sca = A("sca", [128, KD], F32)
    sha = A("sha", [128, KD], F32)
    sh2 = A("sh2", [128, KD, 2], F32)
    aS = A("aS", [128, KD], F32)
    invt = A("invt", [128, 1], F32)
    permt = A("permt", [128, 128], F32)
    idt = A("idt", [128, 128], F32)
    bon = A("bon", [128, 128], BF16)
    posi = A("posi", [128, TPC], I32)
    small = A("small", [128, 8], F32)
    biasg = A("biasg", [128, NG], F32)
    nm = A("nm", [128, 32, 4], F32)
    nmr = A("nmr", [128, 32], F32)
    junk = A("junk", [128, D], F32)
    if add_moe:
        yt = [A(f"yt{g}", [128, D], BF16) for g in range(4)]
        gtt = A("gtt", [128, D], F32)
    P = [nc.alloc_psum_tensor(f"P{i}", [128, 512], F32) for i in range(8)]

    s.dma(ga[:, :], gA, w=["ga"])
    s.dma(sca[:, :], scA, w=["sca"])
    s.dma(sha[:, :], shA, w=["sha"])
    s.dma(invt[:, :], invf, w=["invt"])
    s.dma(permt[:, :], perm, w=["permt"])
    s.dma(idt[:, :], ident, w=["idt"])
    s.dma(bon[:, :], bones, w=["bon"], q="pool")
    s.dma(posi[:, :], pos[0:1, :].broadcast_to([128, TPC]), w=["posi"])
    if add_moe:
        s.dma(gtt[:, :], gtm, w=["gtt"])
    s.op("dve", lambda e: e.tensor_scalar(out=aS[:, :], in0=sca[:, :], scalar1=1.0, scalar2=None, op0=ALU.add),
         r=["sca"], w=["aS"])
    s.op("dve", lambda e: e.tensor_tensor(out=aS[:, :], in0=aS[:, :], in1=ga[:, :], op=ALU.mult), r=["aS", "ga"], w=["aS"])
    s.op("dve", lambda e: e.tensor_copy(out=sh2[:, :, 0], in_=sha[:, :]), r=["sha"], w=["sh2a"])
    s.op("dve", lambda e: e.tensor_copy(out=sh2[:, :, 1], in_=sha[:, :]), r=["sha"], w=["sh2b"])
    s.op("dve", lambda e: e.tensor_copy(out=cosT[:, :], in_=posi[:, :]), r=["posi"], w=["cosT"])
    s.op("dve", lambda e: e.tensor_scalar(out=cosT[:, :], in0=cosT[:, :], scalar1=invt[:, 0:1], scalar2=None, op0=ALU.mult),
         r=["cosT", "invt"], w=["cosT"])
    s.op("dve", lambda e: e.tensor_scalar(out=sinT[:, :], in0=cosT[:, :], scalar1=1.0 / (2.0 * math.pi), scalar2=None, op0=ALU.mult),
         r=["cosT"], w=["sinT"])
    s.op("dve", lambda e: e.tensor_scalar(out=sinT[:, :], in0=sinT[:, :], scalar1=MAGIC, scalar2=None, op0=ALU.add),
         r=["sinT"], w=["sinT"])
    s.op("dve", lambda e: e.tensor_scalar(out=sinT[:, :], in0=sinT[:, :], scalar1=-MAGIC, scalar2=None, op0=ALU.add),
         r=["sinT"], w=["sinT"])
    s.op("dve", lambda e: e.scalar_tensor_tensor(out=cosT[:, :], in0=sinT[:, :], scalar=-TWO_PI_HI, in1=cosT[:, :],
                                                 op0=ALU.mult, op1=ALU.add), r=["sinT", "cosT"], w=["cosT"])
    s.op("dve", lambda e: e.scalar_tensor_tensor(out=cosT[:, :], in0=sinT[:, :], scalar=-TWO_PI_LO, in1=cosT[:, :],
                                                 op0=ALU.mult, op1=ALU.add), r=["sinT", "cosT"], w=["cosT"])
    s.op("dve", lambda e: e.tensor_scalar(out=cosT[:, :], in0=cosT[:, :], scalar1=-math.pi, scalar2=math.pi, op0=ALU.max, op1=ALU.min),
         r=["cosT"], w=["cosT"])
    s.op("act", lambda e: e.activation(out=sinT[:, :], in_=cosT[:, :], func=AF.Sin), r=["cosT"], w=["sinT"])
    for c4 in range(TPC // 512):
        cs = slice(c4 * 512, (c4 + 1) * 512)
        s.op("dve", lambda e, cs=cs: e.tensor_scalar(out=t1[:, :], in0=cosT[:, cs], scalar1=math.pi / 2, scalar2=None, op0=ALU.is_gt),
             r=["cosT"], w=["t1"])
        s.op("dve", lambda e, cs=cs: e.tensor_scalar(out=t2[:, :], in0=cosT[:, cs], scalar1=math.pi / 2, scalar2=None, op0=ALU.add),
             r=["cosT"], w=["t2"])
        s.op("dve", lambda e, cs=cs: e.scalar_tensor_tensor(out=t2[:, :], in0=t1[:, :], scalar=-2.0 * math.pi, in1=t2[:, :],
                                                            op0=ALU.mult, op1=ALU.add), r=["t1", "t2"], w=["t2"])
        s.op("dve", lambda e: e.tensor_scalar(out=t2[:, :], in0=t2[:, :], scalar1=-math.pi, scalar2=math.pi, op0=ALU.max, op1=ALU.min),
             r=["t2"], w=["t2"])
        s.op("act", lambda e, cs=cs: e.activation(out=cosT[:, cs], in_=t2[:, :], func=AF.Sin), r=["t2", "cosT"], w=["cosT"])

    for t in range(TPC // 128):
        b = t % 2
        rows = slice(t * 128, (t + 1) * 128)
        s.dma(xt[b][:, :], x[rows, :], w=[f"xt{b}"])
        if add_moe:
            for g in range(4):
                s.dma(yt[g][:, :], ys[g][rows, :], w=[f"yt{g}"])
            s.op("pool", lambda e: e.tensor_tensor(out=yt[0][:, :], in0=yt[0][:, :], in1=yt[1][:, :], op=ALU.add), r=["yt0", "yt1"], w=["yt0"])
            s.op("pool", lambda e: e.tensor_tensor(out=yt[2][:, :], in0=yt[2][:, :], in1=yt[3][:, :], op=ALU.add), r=["yt2", "yt3"], w=["yt2"])
            for c4 in range(4):
                cs = slice(c4 * 512, (c4 + 1) * 512)
                s.op("dve", lambda e, cs=cs: e.tensor_tensor(out=t1[:, :], in0=yt[0][:, cs], in1=yt[2][:, cs], op=ALU.add),
                     r=["yt0", "yt2"], w=["t1"])
                s.op("dve", lambda e, cs=cs: e.tensor_tensor(out=t1[:, :], in0=t1[:, :], in1=gtt[:, cs], op=ALU.mult),
                     r=["t1", "gtt"], w=["t1"])
                s.op("dve", lambda e, cs=cs, b=b: e.tensor_tensor(out=xt[b][:, cs], in0=xt[b][:, cs], in1=t1[:, :], op=ALU.add),
                     r=["t1", f"xt{b}"], w=[f"xt{b}"])
            s.dma(xnew[rows, :], xt[b][:, :], r=[f"xt{b}"], is_out=True)
        s.op("act", lambda e, b=b: e.activation(out=junk[:, :], in_=xt[b][:, :], func=AF.Square, accum_out=small[:, 0:1]),
             r=[f"xt{b}"], w=["junk", "ss"])
        _rms_rstd(s, nc, small[:, 0:1], small[:, 1:2], small[:, 2:3], "ss", "rstd", D)
        s.op("dve", lambda e, b=b: e.tensor_scalar(out=xt[b][:, :], in0=xt[b][:, :], scalar1=small[:, 1:2], scalar2=None, op0=ALU.mult),
             r=[f"xt{b}", "rstd"], w=[f"xt{b}"])
        for k4 in range(4):
            pb = P[(t % 2) * 4 + k4]
            for kk in range(4):
                k = k4 * 4 + kk
                s.op("pe", lambda e, pb=pb, kk=kk, k=k, b=b: e.transpose(out=pb[:, kk * 128:(kk + 1) * 128], in_=xt[b][:, k * 128:(k + 1) * 128],
                                                                        identity=idt[:, :]),
                     r=[f"xt{b}", "idt"], w=[f"P{(t % 2) * 4 + k4}"])
            eng = "act" if k4 % 2 == 0 else "dve"
            dst = xnT[:, k4 * 4:(k4 + 1) * 4, t * 128:(t + 1) * 128]
            srcv = pb[:, :].rearrange("p (k t) -> p k t", k=4)
            if eng == "act":
                s.op("act", lambda e, dst=dst, srcv=srcv: e.copy(out=dst, in_=srcv), r=[f"P{(t % 2) * 4 + k4}"], w=["xnT"])
            else:
                s.op("dve", lambda e, dst=dst, srcv=srcv: e.tensor_copy(out=dst, in_=srcv), r=[f"P{(t % 2) * 4 + k4}"], w=["xnT"])

    NCH = TPC // 512
    iters = [(g, ch) for g in range(NG) for ch in range(NCH)]
    NI = len(iters)

    def w_dma(g):
        s.dma(wst[g % 2][:, :, :], win[g * 128:(g + 1) * 128, :].rearrange("p (k c) -> p k c", k=KD), w=[f"wst{g % 2}"])

    def w_prep(g):
        wbuf = g % 2
        for k in range(KD):
            s.op("pe", lambda e, k=k, wbuf=wbuf: e.matmul(P[6][:, 0:2], lhsT=wst[wbuf][:, k, :], rhs=sh2[:, k, :],
                                                           start=(k == 0), stop=(k == KD - 1)),
                 r=[f"wst{wbuf}", "sh2a", "sh2b"], w=["P6"])
        s.op("dve", lambda e, g=g: e.tensor_copy(out=biasg[:, g:g + 1], in_=P[6][:, 0:1]), r=["P6"], w=[f"biasg{g}"])
        s.op("dve", lambda e, wbuf=wbuf: e.tensor_tensor(out=wb[wbuf][:, :, :], in0=wst[wbuf][:, :, :],
                                                          in1=aS[:, :].unsqueeze(2).broadcast_to([128, KD, 128]), op=ALU.mult),
             r=[f"wst{wbuf}", "aS"], w=[f"wb{wbuf}"])

    def stA(it):
        g, ch = iters[it]
        cs = slice(ch * 512, (ch + 1) * 512)
        if ch == 0 and g + 1 < NG:
            w_dma(g + 1)
        pq, pqn = P[it % 2], f"P{it % 2}"
        for k in range(KD):
            s.op("pe", lambda e, k=k, g=g, pq=pq, cs=cs: e.matmul(pq[:, :], lhsT=wb[g % 2][:, k, :], rhs=xnT[:, k, cs],
                                                                  start=(k == 0), stop=(k == KD - 1)),
                 r=[f"wb{g % 2}", "xnT"], w=[pqn])
        if ch == 2 and g + 1 < NG:
            w_prep(g + 1)
        if g % 6 >= 4:
            ob = it % 3
            s.op("act", lambda e, pq=pq, ob=ob, g=g: e.activation(out=qo3[ob][:, :], in_=pq[:, :], func=AF.Identity, bias=biasg[:, g:g + 1]),
                 r=[pqn, f"biasg{g}"], w=[f"qo{ob}"])
            s.dma(qkvT[g * 128:(g + 1) * 128, cs], qo3[ob][:, :], r=[f"qo{ob}"], is_out=True)
        else:
            b2 = it % 2
            s.op("act", lambda e, pq=pq, b2=b2, g=g: e.activation(out=qf[b2][:, :], in_=pq[:, :], func=AF.Identity, bias=biasg[:, g:g + 1]),
                 r=[pqn, f"biasg{g}"], w=[f"qf{b2}"])
            s.op("dve", lambda e, b2=b2, cs=cs: e.tensor_tensor(out=t2b[b2][:, :], in0=qf[b2][:, :], in1=cosT[:, cs], op=ALU.mult),
                 r=[f"qf{b2}", "cosT"], w=[f"t2b{b2}"])

    def stC(it):
        g, ch = iters[it]
        if g % 6 >= 4:
            return
        cs = slice(ch * 512, (ch + 1) * 512)
        b2, ob = it % 2, it % 3
        pr, prn = P[2 + b2], f"P{2 + b2}"
        s.op("pe", lambda e, pr=pr, b2=b2: e.matmul(pr[:, :], lhsT=permt[:, :], rhs=qf[b2][:, :], start=True, stop=True),
             r=["permt", f"qf{b2}"], w=[prn])
        s.op("dve", lambda e, pr=pr, cs=cs: e.tensor_tensor(out=t1[:, :], in0=pr[:, :], in1=sinT[:, cs], op=ALU.mult), r=[prn, "sinT"], w=["t1"])
        s.op("dve", lambda e, ob=ob, b2=b2: e.tensor_tensor(out=qo3[ob][:, :], in0=t1[:, :], in1=t2b[b2][:, :], op=ALU.add),
             r=["t1", f"t2b{b2}"], w=[f"qo{ob}"])
        s.dma(qkvT[g * 128:(g + 1) * 128, cs], qo3[ob][:, :], r=[f"qo{ob}"], is_out=True)
        s.op("act", lambda e, ob=ob, b2=b2: e.activation(out=sqb[b2][:, :], in_=qo3[ob][:, :], func=AF.Square), r=[f"qo{ob}"], w=[f"sqb{b2}"])

    def stE(it):
        g, ch = iters[it]
        if g % 6 >= 4:
            return
        b2 = it % 2
        pn, pnn = P[4 + b2], f"P{4 + b2}"
        s.op("pe", lambda e, pn=pn, b2=b2: e.matmul(pn[:, :], lhsT=bon[:, :], rhs=sqb[b2][:, :], start=True, stop=True), r=["bon", f"sqb{b2}"], w=[pnn])
        gq = (g // 6) * 4 + g % 6
        s.op("dve", lambda e, pn=pn, gq=gq, ch=ch: e.tensor_reduce(out=nm[:, gq, ch:ch + 1], in_=pn[:, :], axis=AX.X, op=ALU.max), r=[pnn], w=["nm"])

    w_dma(0)
    w_prep(0)
    for it in range(NI + 2):
        if it < NI:
            stA(it)
        if 0 <= it - 1 < NI:
            stC(it - 1)
        if 0 <= it - 2 < NI:
            stE(it - 2)
    s.op("dve", lambda e: e.tensor_reduce(out=nmr[:, :], in_=nm[:, :, :], axis=AX.X, op=ALU.max), r=["nm"], w=["nmr"])
    s.dma(nrm, nmr[:, :], r=["nmr"], is_out=True)
    s.finish()
    return nc


def build_A2():
    nc = bass.Bass("TRN2", target_bir_lowering=False)
    qab = _din(nc, "qab", [128, S], BF16)
    kab = _din(nc, "kab", [128, S], BF16)
    qcT = _din(nc, "qcT", [128, S], BF16)
    kcT = _din(nc, "kcT", [128, S], BF16)
    vcP = _din(nc, "vcP", [128, 128 * 128], BF16)
    vbP = _din(nc, "vbP", [128, 128 * 128], BF16)
    vaP = _din(nc, "vaP", [128, 3 * 128 * 64], BF16)
    Ecs = _din(nc, "Ecs", [64, S], BF16)
    nrmb = _din(nc, "nrmb", [128, 64], F32)
    lamt = _din(nc, "lamt", [128, 256], F32)
    lamc = _din(nc, "lamc", [128, 2], F32)
    gsub = _din(nc, "gsub", [128, 1], F32)
    mask2 = _din(nc, "mask2", [128, 256], F32)
    ident = _din(nc, "ident", [128, 128], F32)
    oT = _dout(nc, "oT", [256, S], BF16)
    s = Sched(nc)
    A = nc.alloc_sbuf_tensor
    BIG = [A(f"BIG{i}", [128, S], BF16) for i in range(4)]
    m2 = A("m2", [128, 256], BF16)
    idt = A("idt", [128, 128], F32)
    ones_b = A("ones_b", [128, 128], BF16)
    ones_f = A("ones_f", [128, 128], F32)
    nr_in = A("nr_in", [128, 64], F32)
    nr = A("nr", [128, 8], F32)
    negM = A("negM", [128, 4], F32)
    lt = A("lt", [128, 256], F32)
    lc = A("lc", [128, 2], F32)
    gs = A("gs", [128, 1], F32)
    sm = A("sm", [128, 8], F32)
    pt = [[A(f"pt{m}{i}", [128, 512], BF16) for i in range(2)] for m in range(2)]
    e = [A(f"e{i}", [128, 512], F32) for i in range(4)]
    ob = [A(f"ob{i}", [128, 512], BF16) for i in range(2)]
    P = [nc.alloc_psum_tensor(f"P{i}", [128, 512], F32) for i in range(8)]
    tri = m2[:, 128:256]
    accC = [A(f"accC{i}", [128, 512], F32) for i in range(2)]

    s.dma(m2[:, :], mask2, w=["m2"], q="pool")
    s.dma(idt[:, :], ident, w=["idt"])
    s.dma(nr_in[:, :], nrmb, w=["nr_in"])
    s.dma(lt[:, :], lamt, w=["lt"])
    s.dma(lc[:, :], lamc, w=["lc"])
    s.dma(gs[:, :], gsub, w=["gs"])
    s.op("pool", lambda g: g.memset(ones_b[:, :], 1.0), w=["ones_b"])
    s.op("pool", lambda g: g.memset(ones_f[:, :], 1.0), w=["ones_f"])
    s.op("dve", lambda g: g.tensor_reduce(out=nr[:, :], in_=nr_in[:, :].rearrange("p (q c) -> p q c", c=8), axis=AX.X, op=ALU.max),
         r=["nr_in"], w=["nr"])
    nr4 = nr[:, :].rearrange("p (s t h) -> p s t h", s=2, t=2)
    s.op("dve", lambda g: g.tensor_tensor(out=negM[:, :].rearrange("p (s h) -> p s h", s=2), in0=nr4[:, :, 0, :], in1=nr4[:, :, 1, :], op=ALU.mult),
         r=["nr"], w=["negM"])
    s.op("act", lambda g: g.activation(out=negM[:, :], in_=negM[:, :], func=AF.Sqrt), r=["negM"], w=["negM"])
    s.op("dve", lambda g: g.tensor_scalar(out=negM[:, :], in0=negM[:, :], scalar1=-1.02 * 0.125, scalar2=None, op0=ALU.mult),
         r=["negM"], w=["negM"])
    s.op("dve", lambda g: g.tensor_tensor(out=lt[:, 0:64], in0=lt[:, 0:64], in1=lt[:, 64:128], op=ALU.mult), r=["lt"], w=["lt"])
    s.op("dve", lambda g: g.tensor_tensor(out=lt[:, 128:192], in0=lt[:, 128:192], in1=lt[:, 192:256], op=ALU.mult), r=["lt"], w=["lt"])
    s.op("dve", lambda g: g.tensor_reduce(out=sm[:, 0:1], in_=lt[:, 0:64], axis=AX.X, op=ALU.add), r=["lt"], w=["sm"])
    s.op("dve", lambda g: g.tensor_reduce(out=sm[:, 1:2], in_=lt[:, 128:192], axis=AX.X, op=ALU.add), r=["lt"], w=["sm"])
    s.op("act", lambda g: g.activation(out=sm[:, 0:2], in_=sm[:, 0:2], func=AF.Exp), r=["sm"], w=["sm"])
    s.op("dve", lambda g: g.tensor_tensor(out=sm[:, 2:3], in0=sm[:, 1:2], in1=sm[:, 0:1], op=ALU.subtract), r=["sm"], w=["sm"])
    s.op("dve", lambda g: g.tensor_tensor(out=sm[:, 2:3], in0=sm[:, 2:3], in1=lc[:, 0:1], op=ALU.subtract), r=["sm", "lc"], w=["sm"])
    s.op("dve", lambda g: g.tensor_tensor(out=sm[:, 3:4], in0=gs[:, 0:1], in1=lc[:, 1:2], op=ALU.mult), r=["gs", "lc"], w=["sm"])
    neglam = sm[:, 2:3]
    gsc = sm[:, 3:4]

    def causal_rng(c, kb):
        j = kb - 4 * c
        off = 128 * j if j > 0 else 0
        return j, off, 512 - off

    QC, KC = BIG[0], BIG[1]
    VC = BIG[2][:, :].rearrange("p (t d) -> p t d", d=128)
    s.dma(QC[:, :], qcT, w=["QC"])
    s.dma(KC[:, :], kcT, w=["KC"])
    s.dma(BIG[2][:, :], vcP, w=["VC"])
    SC = [[P[0], P[1]], [P[2], P[3]]]
    SCn = [["P0", "P1"], ["P2", "P3"]]
    O, On = [P[4], P[5]], ["P4", "P5"]
    Dn_, Dnn = [P[6], P[7]], ["P6", "P7"]
    it = 0
    pend_c = None

    def c_tail(c, itx):
        sb, sbn = SC[0][itx % 2], SCn[0][itx % 2]
        s.op("pe", lambda g, sb=sb: g.matmul(sb[:, :], lhsT=ones_f[:, :], rhs=e[1][:, :], start=True, stop=True), r=["ones_f", "e1"], w=[sbn])
        s.op("dve", lambda g, sb=sb: g.tensor_scalar(out=e[2][:, :], in0=sb[:, :], scalar1=1.0 / 128, scalar2=EPS, op0=ALU.mult, op1=ALU.add),
             r=[sbn], w=["e2"])
        s.op("act", lambda g: g.activation(out=e[2][:, :], in_=e[2][:, :], func=AF.Sqrt), r=["e2"], w=["e2"])
        s.op("dve", lambda g: g.reciprocal(out=e[2][:, :], in_=e[2][:, :]), r=["e2"], w=["e2"])
        o_ = ob[c % 2]
        s.op("dve", lambda g, o_=o_: g.scalar_tensor_tensor(out=o_[:, :], in0=e[0][:, :], scalar=gsc, in1=e[2][:, :], op0=ALU.mult, op1=ALU.mult),
             r=["e0", "e2", "sm"], w=[f"ob{c % 2}"])
        s.dma(oT[128:256, c * 512:(c + 1) * 512], o_[:, :], r=[f"ob{c % 2}"], is_out=True)

    for c in range(S // 512):
        nkb = 4 * c + 4

        def qk(kb, itx):
            j, off, n = causal_rng(c, kb)
            q0 = c * 512 + off
            for m in range(2):
                r0 = 64 * m
                sb, sbn = SC[m][itx % 2], SCn[m][itx % 2]
                s.op("pe", lambda g, sb=sb, r0=r0, kb=kb, q0=q0, n=n: g.matmul(sb[:, 0:n], lhsT=KC[r0:r0 + 64, kb * 128:(kb + 1) * 128],
                                                                              rhs=QC[r0:r0 + 64, q0:q0 + n], start=True, stop=True),
                     r=["KC", "QC"], w=[sbn])
        qk(0, it)
        for kb in range(nkb):
            if kb + 1 < nkb:
                qk(kb + 1, it + 1)
            j, off, n = causal_rng(c, kb)
            for m in range(2):
                sb, sbn = SC[m][it % 2], SCn[m][it % 2]
                p_, pn = pt[m][it % 2], f"pt{m}{it % 2}"
                s.op("act", lambda g, sb=sb, p_=p_, n=n, m=m: g.activation(out=p_[:, 0:n], in_=sb[:, 0:n], func=AF.Exp,
                                                                           bias=negM[:, 2 + m:3 + m], scale=0.125),
                     r=[sbn, "negM"], w=[pn])
                if j >= 0:
                    s.op("pool", lambda g, p_=p_: g.tensor_tensor(out=p_[:, 0:128], in0=p_[:, 0:128], in1=tri, op=ALU.mult),
                         r=[pn, "m2"], w=[pn])
                s.op("pe", lambda g, p_=p_, n=n, off=off, kb=kb, m=m: g.matmul(O[m][:, off:512], lhsT=VC[:, kb, :], rhs=p_[:, 0:n],
                                                                              start=(kb == 0), stop=(kb == nkb - 1)),
                     r=[pn, "VC"], w=[On[m]])
                s.op("pe", lambda g, p_=p_, n=n, off=off, kb=kb, m=m: g.matmul(Dn_[m][:, off:512], lhsT=ones_b[:, :], rhs=p_[:, 0:n],
                                                                              start=(kb == 0), stop=(kb == nkb - 1)),
                     r=[pn, "ones_b"], w=[Dnn[m]])
            it += 1
            if pend_c is not None and kb == min(nkb - 1, 10):
                c_tail(pend_c, it + 1)
                pend_c = None
        s.op("act", lambda g: g.copy(out=e[0][:, :], in_=O[0][:, :]), r=[On[0]], w=["e0"])
        s.op("act", lambda g: g.copy(out=e[1][:, :], in_=O[1][:, :]), r=[On[1]], w=["e1"])
        s.op("dve", lambda g: g.tensor_copy(out=e[2][:, :], in_=Dn_[0][:, :]), r=[Dnn[0]], w=["e2"])
        s.op("dve", lambda g: g.tensor_copy(out=e[3][:, :], in_=Dn_[1][:, :]), r=[Dnn[1]], w=["e3"])
        s.op("dve", lambda g: g.reciprocal(out=e[2][:, :], in_=e[2][:, :]), r=["e2"], w=["e2"])
        s.op("dve", lambda g: g.reciprocal(out=e[3][:, :], in_=e[3][:, :]), r=["e3"], w=["e3"])
        s.op("pool", lambda g: g.tensor_tensor(out=e[0][:, :], in0=e[0][:, :], in1=e[2][:, :], op=ALU.mult), r=["e0", "e2"], w=["e0"])
        s.op("pool", lambda g: g.tensor_tensor(out=e[1][:, :], in0=e[1][:, :], in1=e[3][:, :], op=ALU.mult), r=["e1", "e3"], w=["e1"])
        s.op("dve", lambda g: g.scalar_tensor_tensor(out=e[0][:, :], in0=e[1][:, :], scalar=neglam, in1=e[0][:, :], op0=ALU.mult, op1=ALU.add),
             r=["e0", "e1", "sm"], w=["e0"])
        s.op("pool", lambda g: g.tensor_tensor(out=e[1][:, :], in0=e[0][:, :], in1=e[0][:, :], op=ALU.mult), r=["e0"], w=["e1"])
        pend_c = c
    c_tail(pend_c, it)

    s.barrier()
    KBE, QBB, QBL = BIG[0], BIG[1], BIG[3]
    VB = BIG[2][:, :].rearrange("p (t d) -> p t d", d=128)
    s.dma(KBE[0:64, :], Ecs, w=["KBE"])
    s.dma(KBE[64:128, :], kab[64:128, :], w=["KBEk"])
    s.dma(QBB[0:64, :], kab[64:128, :], w=["B1lo"])
    s.dma(QBB[64:128, :], qab[64:128, :], w=["B1hi"])
    s.dma(QBL[0:64, :], qab[64:128, :], w=["QBL"])
    s.dma(BIG[2][:, :], vbP, w=["VB"])
    kmT = A("kmT", [128, 64], F32)
    qgf = [A(f"qgf{i}", [128, 128], F32) for i in range(2)]
    gsb = [A(f"gsb{i}", [128, 64], F32) for i in range(2)]
    top8 = [A(f"top8{i}", [128, 8], F32) for i in range(2)]
    bt = [A(f"bt{i}", [128, 128], F32) for i in range(2)]
    for i in range(2):
        s.op("pool", lambda g, i=i: g.memset(bt[i][:, :], 0.0), w=[f"bt{i}"])
    s.op("dve", lambda g: g.tensor_reduce(out=kmT[0:64, :], in_=QBB[0:64, :].rearrange("p (n t) -> p n t", t=256), axis=AX.X, op=ALU.add),
         r=["B1lo"], w=["kmT"])
    s.op("dve", lambda g: g.tensor_scalar(out=kmT[0:64, :], in0=kmT[0:64, :], scalar1=1.0 / 256, scalar2=None, op0=ALU.mult),
         r=["kmT"], w=["kmT"])
    SB, SBn = [P[0], P[1], P[2]], ["P0", "P1", "P2"]
    OB, DB, GP, TP = P[3], P[5], [P[4], P[5]], [P[6], P[7]]
    ptB = [pt[0][0], pt[0][1], pt[1][0]]
    ptBn = ["pt00", "pt01", "pt10"]
    it = 0

    def gate1(c, tls):
        for tl in tls:
            t = 4 * c + tl
            own = t // 2
            b = tl % 2
            s.op("act", lambda g, b=b, t=t: g.copy(out=qgf[b][0:64, :], in_=QBL[0:64, t * 128:(t + 1) * 128]), r=["QBL"], w=[f"qgf{b}"])
            s.op("pe", lambda g, b=b: g.matmul(GP[b][:, 0:64], lhsT=qgf[b][0:64, :], rhs=kmT[0:64, :], start=True, stop=True),
                 r=[f"qgf{b}", "kmT"], w=[f"P{4 + b}"])
            s.op("dve", lambda g, b=b: g.memset(gsb[b][:, :], -1e30), w=[f"gsb{b}"])
            if own > 0:
                s.op("dve", lambda g, b=b, own=own: g.tensor_copy(out=gsb[b][:, 0:own], in_=GP[b][:, 0:own]), r=[f"P{4 + b}"], w=[f"gsb{b}"])
            s.op("dve", lambda g, b=b: g.max(out=top8[b][:, :], in_=gsb[b][:, :]), r=[f"gsb{b}"], w=[f"top8{b}"])
            s.op("dve", lambda g, b=b: g.tensor_scalar(out=bt[b][:, 0:64], in0=gsb[b][:, :], scalar1=top8[b][:, 2:3], scalar2=None, op0=ALU.is_ge),
                 r=[f"gsb{b}", f"top8{b}"], w=[f"bt{b}"])
            s.op("dve", lambda g, b=b: g.tensor_scalar(out=bt[b][:, 0:64], in0=bt[b][:, 0:64], scalar1=-1.0, scalar2=-NEG, op0=ALU.add, op1=ALU.mult),
                 r=[f"bt{b}"], w=[f"bt{b}"])
            s.op("dve", lambda g, b=b, own=own: g.memset(bt[b][:, own:own + 1], 0.0), w=[f"bt{b}"])

    def gate2(c, tls):
        for tl in tls:
            t = 4 * c + tl
            b = tl % 2
            s.op("pe", lambda g, b=b: g.transpose(out=TP[b][:, 0:128], in_=bt[b][:, :], identity=idt[:, :]), r=[f"bt{b}", "idt"], w=[f"P{6 + b}"])
            s.op("act", lambda g, b=b, t=t: g.copy(out=QBB[0:64, t * 128:(t + 1) * 128], in_=TP[b][0:64, 0:128]),
                 r=[f"P{6 + b}", "kmT"], w=[f"B1lo{c}"] if c > 0 else ["B1lo"])

    NCB = S // 512
    gate1(0, (0, 1)); gate2(0, (0, 1)); gate1(0, (2, 3)); gate2(0, (2, 3))
    for c in range(NCB):
        nkb = 4 * c + 4
        half = nkb // 2
        lok = f"B1lo{c}" if c > 0 else "B1lo"

        def qkb(kb, itx):
            j, off, n = causal_rng(c, kb)
            q0 = c * 512 + off
            s.op("pe", lambda g, kb=kb, q0=q0, n=n, itx=itx: g.matmul(SB[itx % 3][:, 0:n], lhsT=KBE[:, kb * 128:(kb + 1) * 128],
                                                                     rhs=QBB[:, q0:q0 + n], start=True, stop=True),
                 r=["KBE", "KBEk", lok, "B1hi"], w=[SBn[itx % 3]])
        qkb(0, it)
        qkb(1, it + 1)
        for kb in range(nkb):
            if c + 1 < NCB:
                if kb == 0:
                    gate1(c + 1, (0, 1))
                elif kb == half:
                    gate2(c + 1, (0, 1))
                    gate1(c + 1, (2, 3))
            if kb + 2 < nkb:
                qkb(kb + 2, it + 2)
            j, off, n = causal_rng(c, kb)
            p_, pn = ptB[it % 3], ptBn[it % 3]
            s.op("act", lambda g, p_=p_, n=n, it=it: g.activation(out=p_[:, 0:n], in_=SB[it % 3][:, 0:n], func=AF.Exp, bias=negM[:, 1:2], scale=0.125),
                 r=[SBn[it % 3], "negM"], w=[pn])
            if j >= 0:
                s.op("pool", lambda g, p_=p_: g.tensor_tensor(out=p_[:, 0:128], in0=p_[:, 0:128], in1=tri, op=ALU.mult), r=[pn, "m2"], w=[pn])
            s.op("pe", lambda g, p_=p_, n=n, off=off, kb=kb: g.matmul(OB[:, off:512], lhsT=VB[:, kb, :], rhs=p_[:, 0:n],
                                                                     start=(kb == 0), stop=(kb == nkb - 1)), r=[pn, "VB"], w=["P3"])
            it += 1
        if c + 1 < NCB:
            gate2(c + 1, (2, 3))
        s.op("act", lambda g: g.copy(out=e[1][:, :], in_=OB[:, :]), r=["P3"], w=["e1"])
        s.op("pe", lambda g: g.matmul(DB[0:64, :], lhsT=idt[:, 64:128], rhs=e[1][:, :], start=True, stop=True), r=["idt", "e1"], w=["P5"])
        s.op("dve", lambda g: g.reciprocal(out=e[0][0:64, :], in_=DB[0:64, :]), r=["P5"], w=["e0"])
        o_ = ob[c % 2]
        s.op("dve", lambda g, o_=o_: g.tensor_tensor(out=o_[0:64, :], in0=e[1][0:64, :], in1=e[0][0:64, :], op=ALU.mult), r=["e1", "e0"], w=[f"ob{c % 2}"])
        s.dma(oT[64:128, c * 512:(c + 1) * 512], o_[0:64, :], r=[f"ob{c % 2}"], is_out=True)

    s.barrier()
    KA, QA = BIG[0], BIG[1]
    VA = [BIG[2][:, 0:8192].rearrange("p (t d) -> p t d", d=64), BIG[2][:, 8192:16384].rearrange("p (t d) -> p t d", d=64),
          BIG[3][:, 0:8192].rearrange("p (t d) -> p t d", d=64)]
    s.dma(KA[0:64, :], kab[0:64, :], w=["KA"])
    s.dma(QA[0:64, :], qab[0:64, :], w=["QA"])
    s.dma(BIG[2][:, :], vaP[:, 0:16384], w=["VA"])
    s.dma(BIG[3][:, 0:8192], vaP[:, 16384:24576], w=["VA2"])
    ND = A("ND", [128, 2, 2048], F32)
    oa = A("oa", [128, 2048], BF16)
    SA, SAn = [P[0], P[1], P[2], P[3]], ["P0", "P1", "P2", "P3"]
    OA, OAn = [P[4], P[5]], ["P4", "P5"]
    DA, DAn = [P[6], P[7]], ["P6", "P7"]
    ptA = [pt[0][0], pt[0][1], pt[1][0], pt[1][1]]
    ptAn = ["pt00", "pt01", "pt10", "pt11"]
    for s8 in range(S // 2048):
        units = []
        for pi, d in enumerate((1, 4, 16)):
            nbl = 16 // d
            for r in range(d):
                for bl in range(nbl):
                    units.append((pi, d, r, s8 * nbl + bl))

        def a_qk(u, i):
            pi, d, r, bg = u
            st = 128 * d * bg + r
            qs = slice(st, st + 127 * d + 1, d)
            b4 = i % 4
            if bg > 0:
                ps_ = slice(st - 128 * d, st - 128 * d + 127 * d + 1, d)
                s.op("pe", lambda g, b4=b4, ps_=ps_, qs=qs: g.matmul(SA[b4][:, 0:128], lhsT=KA[0:64, ps_], rhs=QA[0:64, qs], start=True, stop=True),
                     r=["KA", "QA"], w=[SAn[b4]])
            s.op("pe", lambda g, b4=b4, qs=qs: g.matmul(SA[b4][:, 128:256], lhsT=KA[0:64, qs], rhs=QA[0:64, qs], start=True, stop=True),
                 r=["KA", "QA"], w=[SAn[b4]])

        def a_rest(u, i):
            pi, d, r, bg = u
            st = 128 * d * bg + r
            off = st - s8 * 2048
            osl = slice(off, off + 127 * d + 1, d)
            tile_i = r * (128 // d) + bg
            b4, b = i % 4, i % 2
            lo = 0 if bg > 0 else 128
            p_, pn = ptA[b4], ptAn[b4]
            s.op("act", lambda g, b4=b4, p_=p_, lo=lo: g.activation(out=p_[:, lo:256], in_=SA[b4][:, lo:256], func=AF.Exp, bias=negM[:, 0:1], scale=0.125),
                 r=[SAn[b4], "negM"], w=[pn])
            s.op("pool", lambda g, p_=p_, lo=lo: g.tensor_tensor(out=p_[:, lo:256], in0=p_[:, lo:256], in1=m2[:, lo:256], op=ALU.mult),
                 r=[pn, "m2"], w=[pn])
            first = True
            for isv in (True, False):
                c0 = 0 if isv else 128
                lp = (VA[pi][:, tile_i - 1, :] if bg > 0 else None) if isv else ones_b[:, 0:64]
                lcur = VA[pi][:, tile_i, :] if isv else ones_b[:, 0:64]
                vkeys = ["VA", "VA2"] if isv else ["ones_b"]
                if bg > 0:
                    s.op("pe", lambda g, b=b, lp=lp, p_=p_, c0=c0, first=first: g.matmul(OA[b][0:64, c0:c0 + 128], lhsT=lp, rhs=p_[:, 0:128], start=first, stop=False),
                         r=[pn] + vkeys, w=[OAn[b]])
                    first = False
                s.op("pe", lambda g, b=b, lcur=lcur, p_=p_, c0=c0, first=first, isv=isv: g.matmul(OA[b][0:64, c0:c0 + 128], lhsT=lcur, rhs=p_[:, 128:256],
                                                                                                 start=first, stop=(not isv)),
                     r=[pn] + vkeys, w=[OAn[b]])
                first = False
            ndv = ND[0:64, :, osl]
            pv = OA[b][0:64, 0:256].rearrange("p (t q) -> p t q", t=2)
            if pi == 0:
                s.op("dve", lambda g, ndv=ndv, pv=pv: g.tensor_copy(out=ndv, in_=pv), r=[OAn[b]], w=["ND"])
            else:
                s.op("dve", lambda g, ndv=ndv, pv=pv: g.tensor_tensor(out=ndv, in0=ndv, in1=pv, op=ALU.add), r=[OAn[b], "ND"], w=["ND"])

        nu = len(units)
        a_qk(units[0], 0)
        a_qk(units[1], 1)
        for i in range(nu):
            if i + 2 < nu:
                a_qk(units[i + 2], i + 2)
            a_rest(units[i], i)
        s.op("dve", lambda g: g.reciprocal(out=ND[0:64, 1, :], in_=ND[0:64, 1, :]), r=["ND"], w=["ND"])
        s.op("dve", lambda g: g.tensor_tensor(out=oa[0:64, :], in0=ND[0:64, 0, :], in1=ND[0:64, 1, :], op=ALU.mult), r=["ND"], w=["oa"])
        s.dma(oT[0:64, s8 * 2048:(s8 + 1) * 2048], oa[0:64, :], r=["oa"], is_out=True)
    s.finish()
    return nc


def build_B1():
    nc = bass.Bass("TRN2", target_bir_lowering=False)
    NT = TPC // 128
    mTt = _din(nc, "mTt", [NT * 128, KD * 128], BF16)
    woP = _din(nc, "woP", [128, KD * D], F32)
    x = _din(nc, "x", [TPC, D], F32)
    gta = _din(nc, "gta", [128, D], F32)
    gml = _din(nc, "gml", [128, D], F32)
    scm = _din(nc, "scm", [128, D], F32)
    shm = _din(nc, "shm", [128, D], F32)
    wrP = _din(nc, "wrP", [128, KD * 16], F32)
    brb = _din(nc, "brb", [128, 16], F32)
    ident = _din(nc, "ident", [128, 128], F32)
    x1o = _dout(nc, "x1o", [TPC, D], F32)
    h2o = _dout(nc, "h2o", [TPC, D], BF16)
    wto = _dout(nc, "wto", [TPC, 16], F32)
    s = Sched(nc)
    A = nc.alloc_sbuf_tensor
    wo = A("wo", [128, KD, D], BF16)
    gtat, at, sht = A("gtat", [128, D], F32), A("at", [128, D], F32), A("sht", [128, D], F32)
    gmt = A("gmt", [128, D], F32)
    wr = A("wr", [128, KD, 16], F32)
    br = A("br", [128, 16], F32)
    idt = A("idt", [128, 128], F32)
    mt = [A(f"mt{i}", [128, KD, 128], BF16) for i in range(2)]
    xt = [A(f"xt{i}", [128, D], F32) for i in range(2)]
    x1t = [A(f"x1t{i}", [128, D], F32) for i in range(2)]
    h2f = A("h2f", [128, D], F32)
    h2b = [A(f"h2b{i}", [128, D], BF16) for i in range(2)]
    h2T = A("h2T", [128, KD, 128], F32)
    junk = A("junk", [128, D], F32)
    small = A("small", [128, 8], F32)
    rt = {n: A(f"rt_{n}", [128, 16], F32) for n in ("s", "sgb", "p1", "p2", "c1", "c2", "c3", "w")}
    sg2 = A("sg2", [128, 4, 8], F32)
    g4 = {n: A(f"g4_{n}", [128, 4], F32) for n in ("a", "b", "sel")}
    wtt = [A(f"wtt{i}", [128, 16], F32) for i in range(2)]
    P = [nc.alloc_psum_tensor(f"P{i}", [128, 512], F32) for i in range(8)]

    woP3 = woP.rearrange("p (k n) -> p k n", k=KD)
    for cc in range(4):
        s.dma(wo[:, :, cc * 512:(cc + 1) * 512], woP3[:, :, cc * 512:(cc + 1) * 512], w=[f"wo{cc}"], q="pool")
    for (tile_, src, nm) in ((gtat, gta, "gtat"), (gmt, gml, "gmt"), (at, scm, "at"), (sht, shm, "sht")):
        s.dma(tile_[:, :], src, w=[nm])
    s.dma(wr[:, :, :].rearrange("p k e -> p (k e)"), wrP, w=["wr"])
    s.dma(br[:, :], brb, w=["br"])
    s.dma(idt[:, :], ident, w=["idt"])
    s.op("dve", lambda g: g.tensor_scalar(out=at[:, :], in0=at[:, :], scalar1=1.0, scalar2=None, op0=ALU.add), r=["at"], w=["at"])
    s.op("dve", lambda g: g.tensor_tensor(out=at[:, :], in0=at[:, :], in1=gmt[:, :], op=ALU.mult), r=["at", "gmt"], w=["at"])
    v44 = lambda ap: ap.rearrange("p (g e) -> p g e", g=4)
    h2fb = [h2f, A("h2f1", [128, D], F32)]

    mt3 = mt + [A("mt2", [128, KD, 128], BF16)]
    xt3 = xt + [A("xt2", [128, D], F32)]

    def loads(t):
        b3 = t % 3
        rows = slice(t * 128, (t + 1) * 128)
        s.dma(mt3[b3][:, :, :].rearrange("p k t -> p (k t)"), mTt[rows, :], w=[f"mt{b3}"])
        s.dma(xt3[b3][:, :], x[rows, :], w=[f"xt{b3}"])

    def stage1(t):
        b = t % 2
        b3 = t % 3
        rows = slice(t * 128, (t + 1) * 128)
        hf, hfn = h2fb[b], f"h2f{b}"
        for cc in range(4):
            cs = slice(cc * 512, (cc + 1) * 512)
            for k in range(KD):
                s.op("pe", lambda g, cc=cc, k=k, b3=b3, cs=cs: g.matmul(P[cc][:, :], lhsT=mt3[b3][:, k, :], rhs=wo[:, k, cs],
                                                                        start=(k == 0), stop=(k == KD - 1)), r=[f"mt{b3}", f"wo{cc}"], w=[f"P{cc}"])
            s.op("dve", lambda g, cc=cc, b=b, cs=cs: g.tensor_tensor(out=x1t[b][:, cs], in0=P[cc][:, :], in1=gtat[:, cs], op=ALU.mult),
                 r=[f"P{cc}", "gtat"], w=[f"x1t{b}"])
            s.op("pool", lambda g, b=b, b3=b3, cs=cs: g.tensor_tensor(out=x1t[b][:, cs], in0=x1t[b][:, cs], in1=xt3[b3][:, cs], op=ALU.add),
                 r=[f"x1t{b}", f"xt{b3}"], w=[f"x1t{b}"])
        s.dma(x1o[rows, :], x1t[b][:, :], r=[f"x1t{b}"], is_out=True)
        s.op("act", lambda g, b=b: g.activation(out=junk[:, :], in_=x1t[b][:, :], func=AF.Square, accum_out=small[:, 0:1]),
             r=[f"x1t{b}"], w=["junk", "ss"])
        _rms_rstd(s, nc, small[:, 0:1], small[:, 1:2], small[:, 2:3], "ss", "rstd", D)
        s.op("dve", lambda g, b=b, hf=hf: g.scalar_tensor_tensor(out=hf[:, :], in0=x1t[b][:, :], scalar=small[:, 1:2], in1=at[:, :], op0=ALU.mult, op1=ALU.mult),
             r=[f"x1t{b}", "rstd", "at"], w=[hfn])
        s.op("pool", lambda g, hf=hf: g.tensor_tensor(out=hf[:, :], in0=hf[:, :], in1=sht[:, :], op=ALU.add), r=[hfn, "sht"], w=[hfn])
        s.op("act", lambda g, b=b, hf=hf: g.copy(out=h2b[b][:, :], in_=hf[:, :]), r=[hfn], w=[f"h2b{b}"])
        s.dma(h2o[rows, :], h2b[b][:, :], r=[f"h2b{b}"], is_out=True)

    def stage2(t):
        b = t % 2
        rows = slice(t * 128, (t + 1) * 128)
        hf, hfn = h2fb[b], f"h2f{b}"
        for k4 in range(4):
            pb, pbn = P[4 + k4], f"P{4 + k4}"
            for kk in range(4):
                k = k4 * 4 + kk
                s.op("pe", lambda g, pb=pb, kk=kk, k=k, hf=hf: g.transpose(out=pb[:, kk * 128:(kk + 1) * 128], in_=hf[:, k * 128:(k + 1) * 128], identity=idt[:, :]),
                     r=[hfn, "idt"], w=[pbn])
            dst = h2T[:, k4 * 4:(k4 + 1) * 4, :]
            srcv = pb[:, :].rearrange("p (k t) -> p k t", k=4)
            s.op("act", lambda g, dst=dst, srcv=srcv: g.copy(out=dst, in_=srcv), r=[pbn], w=["h2T"])
        for k in range(KD):
            s.op("pe", lambda g, k=k: g.matmul(P[4][:, 0:16], lhsT=h2T[:, k, :], rhs=wr[:, k, :], start=(k == 0), stop=(k == KD - 1)),
                 r=["h2T", "wr"], w=["P4"])
        R = rt
        s.op("act", lambda g: g.activation(out=R["s"][:, :], in_=P[4][:, 0:16], func=AF.Sigmoid), r=["P4"], w=["r_s"])
        s.op("dve", lambda g: g.tensor_tensor(out=R["sgb"][:, :], in0=R["s"][:, :], in1=br[:, :], op=ALU.add), r=["r_s", "br"], w=["r_sgb"])
        s.op("dve", lambda g: g.tensor_copy(out=sg2[:, :, 0:4], in_=v44(R["sgb"][:, :])), r=["r_sgb"], w=["sg2"])
        s.op("dve", lambda g: g.tensor_copy(out=sg2[:, :, 4:8], in_=v44(R["sgb"][:, :])), r=["r_sgb", "sg2"], w=["sg2"])
        s.op("dve", lambda g: g.tensor_tensor(out=v44(R["p1"][:, :]), in0=sg2[:, :, 0:4], in1=sg2[:, :, 1:5], op=ALU.add), r=["sg2"], w=["r_p1"])
        s.op("dve", lambda g: g.tensor_tensor(out=v44(R["p2"][:, :]), in0=sg2[:, :, 0:4], in1=sg2[:, :, 2:6], op=ALU.add), r=["sg2"], w=["r_p2"])
        s.op("dve", lambda g: g.tensor_tensor(out=R["p1"][:, :], in0=R["p1"][:, :], in1=R["p2"][:, :], op=ALU.max), r=["r_p1", "r_p2"], w=["r_p1"])
        s.op("dve", lambda g: g.tensor_reduce(out=g4["a"][:, :], in_=v44(R["p1"][:, :]), axis=AX.X, op=ALU.max), r=["r_p1"], w=["g4a"])
        s.op("dve", lambda g: g.tensor_reduce(out=small[:, 3:4], in_=g4["a"][:, :], axis=AX.X, op=ALU.max), r=["g4a"], w=["gmax"])
        s.op("dve", lambda g: g.tensor_scalar(out=g4["sel"][:, :], in0=g4["a"][:, :], scalar1=small[:, 3:4], scalar2=None, op0=ALU.is_ge),
             r=["g4a", "gmax"], w=["g4sel"])
        for r_, cn in ((1, "c1"), (2, "c2"), (3, "c3")):
            s.op("dve", lambda g, r_=r_, cn=cn: g.tensor_tensor(out=v44(R[cn][:, :]), in0=sg2[:, :, r_:r_ + 4], in1=sg2[:, :, 0:4], op=ALU.is_gt),
                 r=["sg2"], w=[f"r_{cn}"])
        s.op("dve", lambda g: g.tensor_tensor(out=R["c1"][:, :], in0=R["c1"][:, :], in1=R["c2"][:, :], op=ALU.add), r=["r_c1", "r_c2"], w=["r_c1"])
        s.op("dve", lambda g: g.tensor_tensor(out=R["c1"][:, :], in0=R["c1"][:, :], in1=R["c3"][:, :], op=ALU.add), r=["r_c1", "r_c3"], w=["r_c1"])
        s.op("dve", lambda g: g.tensor_scalar(out=R["c1"][:, :], in0=R["c1"][:, :], scalar1=1.5, scalar2=None, op0=ALU.is_lt), r=["r_c1"], w=["r_c1"])
        s.op("dve", lambda g: g.tensor_tensor(out=v44(R["c1"][:, :]), in0=v44(R["c1"][:, :]), in1=g4["sel"][:, :].unsqueeze(2).broadcast_to([128, 4, 4]), op=ALU.mult),
             r=["r_c1", "g4sel"], w=["r_c1"])
        s.op("dve", lambda g: g.tensor_tensor(out=R["w"][:, :], in0=R["s"][:, :], in1=R["c1"][:, :], op=ALU.mult), r=["r_s", "r_c1"], w=["r_w"])
        s.op("dve", lambda g: g.tensor_reduce(out=small[:, 4:5], in_=R["w"][:, :], axis=AX.X, op=ALU.add), r=["r_w"], w=["wsum"])
        s.op("dve", lambda g: g.reciprocal(out=small[:, 5:6], in_=small[:, 4:5]), r=["wsum"], w=["rws"])
        s.op("dve", lambda g, b=b: g.tensor_scalar(out=wtt[b][:, :], in0=R["w"][:, :], scalar1=small[:, 5:6], scalar2=None, op0=ALU.mult),
             r=["r_w", "rws"], w=[f"wtt{b}"])
        s.dma(wto[rows, :], wtt[b][:, :], r=[f"wtt{b}"], is_out=True)

    loads(0)
    loads(1)
    stage1(0)
    for t in range(NT):
        if t + 2 < NT:
            loads(t + 2)
        if t + 1 < NT:
            stage1(t + 1)
        stage2(t)
    s.finish()
    return nc


TB = 1024
NTB = 8
DE = 1024


def build_B2():
    nc = bass.Bass("TRN2", target_bir_lowering=False)
    h2T = _din(nc, "h2T", [NTB * 128, KD * TB], BF16)
    wgP = _din(nc, "wgP", [4 * 8 * 128, KD * 128], F32)
    wuP = _din(nc, "wuP", [4 * 8 * 128, KD * 128], F32)
    wdP = _din(nc, "wdP", [4 * 128, 8 * D], F32)
    wtP = _din(nc, "wtP", [128, 64 * 4], F32)
    yo = _dout(nc, "yo", [NTB * TB, D], BF16)
    s = Sched(nc)
    A = nc.alloc_sbuf_tensor
    HB = A("HB", [128, KD, TB], BF16)
    WD = A("WD", [128, 8, D], BF16)
    WG = [A(f"WG{i}", [128, KD, 128], BF16) for i in range(2)]
    WU = [A(f"WU{i}", [128, KD, 128], BF16) for i in range(2)]
    actT = A("actT", [128, 8, TB], BF16)
    acc = A("acc", [128, TB // 128, D], F32)
    sg = [A(f"sg{i}", [128, 512], F32) for i in range(2)]
    yb = [A(f"yb{i}", [128, D], BF16) for i in range(2)]
    wt = A("wt", [128, 64, 4], F32)
    P = [nc.alloc_psum_tensor(f"P{i}", [128, 512], F32) for i in range(8)]
    s.dma(wt[:, :, :].rearrange("p t e -> p (t e)"), wtP, w=["wt"])
    ig = 0
    iy = 0
    s.dma(HB[:, :, :].rearrange("p k t -> p (k t)"), h2T[0:128, :], w=["HB"])
    for blk in range(NTB):
        for e in range(4):
            for f in range(8):
                fb = f % 2
                r0 = (e * 8 + f) * 128
                s.dma(WG[fb][:, :, :].rearrange("p k c -> p (k c)"), wgP[r0:r0 + 128, :], w=[f"WG{fb}"], q="pool")
                s.dma(WU[fb][:, :, :].rearrange("p k c -> p (k c)"), wuP[r0:r0 + 128, :], w=[f"WU{fb}"], q="pool")
                if f == 1:
                    s.dma(WD[:, :, :].rearrange("p f n -> p (f n)"), wdP[e * 128:(e + 1) * 128, :], w=["WD"], q="pool")
                for ch in range(TB // 512):
                    cs = slice(ch * 512, (ch + 1) * 512)
                    pg, pu = P[ig % 2], P[2 + ig % 2]
                    pgn, pun = f"P{ig % 2}", f"P{2 + ig % 2}"
                    for k in range(KD):
                        s.op("pe", lambda g, pg=pg, k=k, fb=fb, cs=cs: g.matmul(pg[:, :], lhsT=WG[fb][:, k, :], rhs=HB[:, k, cs], start=(k == 0), stop=(k == KD - 1)),
                             r=[f"WG{fb}", "HB"], w=[pgn])
                    for k in range(KD):
                        s.op("pe", lambda g, pu=pu, k=k, fb=fb, cs=cs: g.matmul(pu[:, :], lhsT=WU[fb][:, k, :], rhs=HB[:, k, cs], start=(k == 0), stop=(k == KD - 1)),
                             r=[f"WU{fb}", "HB"], w=[pun])
                    sgt, sgn = sg[ig % 2], f"sg{ig % 2}"
                    s.op("act", lambda g, pg=pg, sgt=sgt: g.activation(out=sgt[:, :], in_=pg[:, :], func=AF.Silu), r=[pgn], w=[sgn])
                    s.op("dve", lambda g, pu=pu, sgt=sgt, f=f, cs=cs: g.tensor_tensor(out=actT[:, f, cs], in0=pu[:, :], in1=sgt[:, :], op=ALU.mult),
                         r=[pun, sgn], w=["actT"])
                    ig += 1
            if e == 3 and blk + 1 < NTB:
                s.dma(HB[:, :, :].rearrange("p k t -> p (k t)"), h2T[(blk + 1) * 128:(blk + 2) * 128, :], w=["HB"])
            for tt in range(TB // 128):
                tile_i = blk * (TB // 128) + tt
                for cc in range(4):
                    cs = slice(cc * 512, (cc + 1) * 512)
                    py, pyn = P[4 + iy % 4], f"P{4 + iy % 4}"
                    for f in range(8):
                        s.op("pe", lambda g, py=py, f=f, tt=tt, cs=cs: g.matmul(py[:, :], lhsT=actT[:, f, tt * 128:(tt + 1) * 128], rhs=WD[:, f, cs],
                                                                               start=(f == 0), stop=(f == 7)), r=["actT", "WD"], w=[pyn])
                    wsc = wt[:, tile_i, e:e + 1]
                    if e == 0:
                        s.op("dve", lambda g, py=py, tt=tt, cs=cs, wsc=wsc: g.tensor_scalar(out=acc[:, tt, cs], in0=py[:, :], scalar1=wsc, scalar2=None, op0=ALU.mult),
                             r=[pyn, "wt"], w=[f"acc{tt}"])
                    else:
                        s.op("dve", lambda g, py=py, tt=tt, cs=cs, wsc=wsc: g.scalar_tensor_tensor(out=acc[:, tt, cs], in0=py[:, :], scalar=wsc, in1=acc[:, tt, cs],
                                                                                                  op0=ALU.mult, op1=ALU.add),
                             r=[pyn, "wt", f"acc{tt}"], w=[f"acc{tt}"])
                    iy += 1
        for tt in range(TB // 128):
            b = tt % 2
            s.op("act", lambda g, b=b, tt=tt: g.copy(out=yb[b][:, :], in_=acc[:, tt, :]), r=[f"acc{tt}"], w=[f"yb{b}"])
            r0 = blk * TB + tt * 128
            s.dma(yo[r0:r0 + 128, :], yb[b][:, :], r=[f"yb{b}"], is_out=True)
    s.finish()
    return nc


def build_F():
    nc = bass.Bass("TRN2", target_bir_lowering=False)
    x = _din(nc, "x", [TPC, D], F32)
    ys = [_din(nc, f"y{g}", [TPC, D], BF16) for g in range(4)]
    gtm = _din(nc, "gtm", [128, D], F32)
    gfin = _din(nc, "gfin", [128, D], F32)
    out = _dout(nc, "out", [TPC, D], F32)
    s = Sched(nc)
    A = nc.alloc_sbuf_tensor
    xt = [A(f"xt{i}", [128, D], F32) for i in range(3)]
    yt = [[A(f"yt{g}{i}", [128, D], BF16) for i in range(3)] for g in range(4)]
    gtt, gft = A("gtt", [128, D], F32), A("gft", [128, D], F32)
    t1 = A("t1", [128, D], F32)
    junk = A("junk", [128, D], F32)
    small = A("small", [128, 8], F32)
    s.dma(gtt[:, :], gtm, w=["gtt"])
    s.dma(gft[:, :], gfin, w=["gft"])
    NT = TPC // 128

    def loads(t):
        b = t % 3
        rows = slice(t * 128, (t + 1) * 128)
        s.dma(xt[b][:, :], x[rows, :], w=[f"xt{b}"])
        for g in range(4):
            s.dma(yt[g][b][:, :], ys[g][rows, :], w=[f"yt{g}{b}"])

    loads(0)
    loads(1)
    for t in range(NT):
        if t + 2 < NT:
            loads(t + 2)
        b = t % 3
        rows = slice(t * 128, (t + 1) * 128)
        s.op("pool", lambda e, b=b: e.tensor_tensor(out=yt[0][b][:, :], in0=yt[0][b][:, :], in1=yt[1][b][:, :], op=ALU.add), r=[f"yt0{b}", f"yt1{b}"], w=[f"yt0{b}"])
        s.op("pool", lambda e, b=b: e.tensor_tensor(out=yt[2][b][:, :], in0=yt[2][b][:, :], in1=yt[3][b][:, :], op=ALU.add), r=[f"yt2{b}", f"yt3{b}"], w=[f"yt2{b}"])
        s.op("dve", lambda e, b=b: e.tensor_tensor(out=t1[:, :], in0=yt[0][b][:, :], in1=yt[2][b][:, :], op=ALU.add), r=[f"yt0{b}", f"yt2{b}"], w=["t1"])
        s.op("dve", lambda e: e.tensor_tensor(out=t1[:, :], in0=t1[:, :], in1=gtt[:, :], op=ALU.mult), r=["t1", "gtt"], w=["t1"])
        s.op("dve", lambda e, b=b: e.tensor_tensor(out=xt[b][:, :], in0=xt[b][:, :], in1=t1[:, :], op=ALU.add), r=["t1", f"xt{b}"], w=[f"xt{b}"])
        s.op("act", lambda e, b=b: e.activation(out=junk[:, :], in_=xt[b][:, :], func=AF.Square, accum_out=small[:, 0:1]), r=[f"xt{b}"], w=["junk", "ss"])
        _rms_rstd(s, nc, small[:, 0:1], small[:, 1:2], small[:, 2:3], "ss", "rstd", D)
        s.op("dve", lambda e, b=b: e.scalar_tensor_tensor(out=xt[b][:, :], in0=xt[b][:, :], scalar=small[:, 1:2], in1=gft[:, :], op0=ALU.mult, op1=ALU.mult),
             r=[f"xt{b}", "rstd", "gft"], w=[f"xt{b}"])
        s.dma(out[rows, :], xt[b][:, :], r=[f"xt{b}"], is_out=True)
    s.finish()
    return nc


_PROGS = {}


def _prog(name, fn, *a):
    key = (name,) + tuple(a)
    if key not in _PROGS:
        _PROGS[key] = fn(*a)
    return _PROGS[key]


def _run(nc, in_maps):
    res = run_bass_kernel_spmd(nc, in_maps, core_ids=list(range(NCORE)))
    return res.results


def _pk(v):
    return np.ascontiguousarray(np.asarray(v, np.float32).reshape(KD, 128).T)


def _bc(v):
    v = np.asarray(v, np.float32).reshape(1, -1)
    return np.ascontiguousarray(np.broadcast_to(v, (128, v.shape[1])))


def _colidx():
    idx = []
    for h in range(8):
        qa = list(range(0 + h * 64, 0 + h * 64 + 64)); ka = [512 + c for c in qa]; va = [1024 + c for c in qa]
        qb = [1536 + c for c in qa]; kb = [2048 + c for c in qa]; vb = [2560 + c for c in qa]
        qc = list(range(3072 + h * 128, 3072 + h * 128 + 128)); kc = [1024 + c for c in qc]; vc = [2048 + c for c in qc]
        idx += qa + qb + ka + kb + qc + kc + va + vb + vc
    return np.asarray(idx)


def _consts():
    inv = (10000.0 ** (-np.arange(0, 64, 2, dtype=np.float32) / 64)).astype(np.float32)
    invf = np.tile(inv, 4).reshape(128, 1).astype(np.float32)
    perm = np.zeros((128, 128), np.float32)
    for m in range(128):
        if m % 64 < 32:
            perm[m + 32, m] = -1.0
        else:
            perm[m - 32, m] = 1.0
    bones = np.zeros((128, 128), np.float32)
    bones[:64, :64] = 1.0
    bones[64:, 64:] = 1.0
    return dict(invf=invf, perm=perm, ident=np.eye(128, dtype=np.float32), bones=bones)


def host_M(inp):
    nc = _prog("M", build_M)
    cT = _pk(inp["c"].reshape(-1))
    maps = []
    for i in range(NCORE):
        cols = slice(i * MCOL, (i + 1) * MCOL)
        wa = np.concatenate([inp["w_ada"][0][:, cols], inp["w_ada"][1][:, cols]], axis=0)
        ba = np.concatenate([inp["b_ada"][0][cols], inp["b_ada"][1][cols]]).reshape(1, -1)
        maps.append(dict(cT=cT, wada=np.ascontiguousarray(wa), bada=np.ascontiguousarray(ba)))
    res = _run(nc, maps)
    mod = np.zeros((2, 6 * D), np.float32)
    for i in range(NCORE):
        r = res[i]["mod"].reshape(2, MCOL)
        mod[:, i * MCOL:(i + 1) * MCOL] = r
    return mod


def host_PA1(inp, l, mod, x, ys=None):
    add = ys is not None
    nc = _prog("PA1", build_PA1, add)
    cst = _consts()
    sh_a, sc_a = mod[l, 0:D], mod[l, D:2 * D]
    wperm = inp["w_in"][l][:, _colidx()]
    wl = np.ascontiguousarray(wperm.reshape(KD, 128, NG, 128).transpose(2, 1, 0, 3)).reshape(NG * 128, KD * 128)
    pos = np.asarray(inp["positions"]).reshape(-1).astype(np.int32)
    maps = []
    for i in range(NCORE):
        rows = slice(i * TPC, (i + 1) * TPC)
        m = dict(x=np.ascontiguousarray(x[rows]), win=wl, gA=_pk(inp["g_attn"][l]), scA=_pk(sc_a), shA=_pk(sh_a),
                 pos=np.ascontiguousarray(pos[rows].reshape(1, -1)), **cst)
        if add:
            gt_m_prev = mod[l - 1, 5 * D:6 * D]
            for g in range(4):
                m[f"y{g}"] = np.ascontiguousarray(ys[g][rows])
            m["gtm"] = _bc(gt_m_prev)
        maps.append(m)
    res = _run(nc, maps)
    qkvT = np.concatenate([res[i]["qkvT"] for i in range(NCORE)], axis=1)
    nrm = np.stack([res[i]["nrm"] for i in range(NCORE)], axis=0)
    xnew = np.concatenate([res[i]["xnew"] for i in range(NCORE)], axis=0) if add else x
    return qkvT, nrm, xnew


def _a2_consts():
    k = np.arange(128)[:, None]
    q = np.arange(128)[None, :]
    mask2 = np.concatenate([(k >= q), (k <= q)], axis=1).astype(np.float32)
    E = (np.arange(S)[None, :] // 256 == np.arange(64)[:, None]).astype(NPBF)
    return mask2, E


def _tiles(vT):
    d = vT.shape[0]
    return np.ascontiguousarray(vT.T.reshape(S // 128, 128, d).transpose(1, 0, 2)).reshape(128, -1)


def host_A2(inp, l, qkvT, nrm):
    nc = _prog("A2", build_A2)
    mask2, E = _a2_consts()
    lam_init = 0.8 - 0.6 * math.exp(-0.3 * l)
    lamt = np.concatenate([_bc(inp["lam_q1"][l]), _bc(inp["lam_k1"][l]), _bc(inp["lam_q2"][l]), _bc(inp["lam_k2"][l])], axis=1)
    lamc = _bc(np.asarray([lam_init, 1.0 - lam_init], np.float32))
    gsub = np.asarray(inp["g_subln"][l], np.float32).reshape(128, 1)
    ones64 = np.ones((64, S), NPBF)
    maps = []
    for h in range(NCORE):
        G = lambda j: qkvT[(h * 6 + j) * 128:(h * 6 + j + 1) * 128]
        vab = G(4)
        va = vab[0:64]
        vas = []
        for d in (1, 4, 16):
            perm_tok = np.arange(S).reshape(S // d, d).T.reshape(-1)
            vas.append(_tiles(va[:, perm_tok]))
        nb = np.zeros((8, 8), np.float32)
        for j in range(4):
            nb[2 * j] = nrm[:, 0, h * 4 + j]
            nb[2 * j + 1] = nrm[:, 64, h * 4 + j]
        maps.append(dict(qab=np.ascontiguousarray(G(0)), kab=np.ascontiguousarray(G(1)), qcT=np.ascontiguousarray(G(2)),
                         kcT=np.ascontiguousarray(G(3)), vcP=_tiles(G(5)), vbP=_tiles(np.concatenate([vab[64:128], ones64], axis=0)),
                         vaP=np.ascontiguousarray(np.concatenate(vas, axis=1)), Ecs=E, nrmb=_bc(nb.reshape(-1)),
                         lamt=lamt, lamc=lamc, gsub=gsub, mask2=mask2, ident=np.eye(128, dtype=np.float32)))
    res = _run(nc, maps)
    return [res[h]["oT"] for h in range(NCORE)]


def _rowidx_out():
    idx = []
    for h in range(8):
        idx += list(range(h * 64, h * 64 + 64)) + list(range(512 + h * 64, 512 + h * 64 + 64)) + list(range(1024 + h * 128, 1024 + h * 128 + 128))
    return np.asarray(idx)


def host_B1(inp, l, mod, x, oT):
    nc = _prog("B1", build_B1)
    mT = np.concatenate(oT, axis=0)
    wo = inp["w_out"][l][_rowidx_out(), :]
    woP = np.ascontiguousarray(wo.reshape(KD, 128, D).transpose(1, 0, 2)).reshape(128, KD * D)
    wrP = np.ascontiguousarray(np.asarray(inp["w_router"], np.float32).reshape(KD, 128, 16).transpose(1, 0, 2)).reshape(128, KD * 16)
    com = dict(woP=woP, gta=_bc(mod[l, 2 * D:3 * D]), gml=_bc(inp["g_mlp"][l]), scm=_bc(mod[l, 4 * D:5 * D]), shm=_bc(mod[l, 3 * D:4 * D]),
               wrP=wrP, brb=_bc(inp["b_router"]), ident=np.eye(128, dtype=np.float32))
    maps = []
    for i in range(NCORE):
        rows = slice(i * TPC, (i + 1) * TPC)
        mc = mT[:, rows].reshape(KD, 128, TPC // 128, 128).transpose(2, 1, 0, 3)
        maps.append(dict(mTt=np.ascontiguousarray(mc).reshape(TPC, KD * 128), x=np.ascontiguousarray(x[rows]), **com))
    res = _run(nc, maps)
    cat = lambda n: np.concatenate([res[i][n] for i in range(NCORE)], axis=0)
    return cat("x1o"), cat("h2o"), cat("wto")


def host_B2(inp, l, h2, wt):
    nc = _prog("B2", build_B2)
    HALF = S // 2
    maps = []
    for i in range(NCORE):
        t2, g = divmod(i, 4)
        rows = slice(t2 * HALF, (t2 + 1) * HALF)
        hh = h2[rows].reshape(NTB, TB, KD, 128).transpose(0, 3, 2, 1)
        wg = np.stack([inp["w_gate"][l][4 * g + e].reshape(KD, 128, 8, 128).transpose(2, 1, 0, 3) for e in range(4)])
        wu = np.stack([inp["w_up"][l][4 * g + e].reshape(KD, 128, 8, 128).transpose(2, 1, 0, 3) for e in range(4)])
        wd = np.stack([inp["w_down"][l][4 * g + e].reshape(8, 128, D).transpose(1, 0, 2) for e in range(4)])
        wtp = wt[rows, 4 * g:4 * g + 4].reshape(HALF // 128, 128, 4).transpose(1, 0, 2)
        maps.append(dict(h2T=np.ascontiguousarray(hh).reshape(NTB * 128, KD * TB),
                         wgP=np.ascontiguousarray(wg).reshape(4 * 8 * 128, KD * 128),
                         wuP=np.ascontiguousarray(wu).reshape(4 * 8 * 128, KD * 128),
                         wdP=np.ascontiguousarray(wd).reshape(4 * 128, 8 * D),
                         wtP=np.ascontiguousarray(wtp).reshape(128, -1)))
    res = _run(nc, maps)
    ys = []
    for g in range(4):
        ys.append(np.concatenate([res[0 * 4 + g]["yo"], res[1 * 4 + g]["yo"]], axis=0))
    return ys


def host_F(inp, mod, x1, ys):
    nc = _prog("F", build_F)
    com = dict(gtm=_bc(mod[1, 5 * D:6 * D]), gfin=_bc(inp["g_final"]))
    maps = []
    for i in range(NCORE):
        rows = slice(i * TPC, (i + 1) * TPC)
        m = dict(x=np.ascontiguousarray(x1[rows]), **com)
        for g in range(4):
            m[f"y{g}"] = np.ascontiguousarray(ys[g][rows])
        maps.append(m)
    res = _run(nc, maps)
    return np.concatenate([res[i]["out"] for i in range(NCORE)], axis=0)


def kernel(**inputs):
    inp = {k: np.asarray(v) for k, v in inputs.items()}
    mod = host_M(inp)
    x = np.asarray(inp["x"], np.float32).reshape(S, D)
    ys = None
    x1 = None
    for l in range(2):
        qkvT, nrm, x = host_PA1(inp, l, mod, x if l == 0 else x1, ys)
        oT = host_A2(inp, l, qkvT, nrm)
        x1, h2, wt = host_B1(inp, l, mod, x, oT)
        ys = host_B2(inp, l, h2, wt)
    out = host_F(inp, mod, x1, ys)
    return out.reshape(1, S, D).astype(np.float32)
```

```python
import math
import numpy as np
import ml_dtypes
import concourse.bass as bass
import concourse.mybir as mybir
from concourse.bass_utils import run_bass_kernel_spmd

F32 = mybir.dt.float32
BF16 = mybir.dt.bfloat16
I32 = mybir.dt.int32
AF = mybir.ActivationFunctionType
ALU = mybir.AluOpType
AX = mybir.AxisListType
NPBF = ml_dtypes.bfloat16

D = 2048
S = 16384
NCORE = 8
TPC = S // NCORE
KD = D // 128
EPS = 1e-6
NEG = -30000.0


class _B:
    __slots__ = ("w", "r")

    def __init__(self):
        self.w = None
        self.r = {}


class Sched:
    ENG = ("pe", "act", "dve", "pool", "sp")
    NDS = 40

    def __init__(self, nc):
        self.nc = nc
        self.e = dict(pe=nc.tensor, act=nc.scalar, dve=nc.vector, pool=nc.gpsimd, sp=nc.sync)
        self.sem = {k: nc.alloc_semaphore(f"s_{k}") for k in self.ENG}
        self.cnt = {k: 0 for k in self.ENG}
        self.seen = {k: {} for k in self.ENG}
        self.dsem = [nc.alloc_semaphore(f"d{i}") for i in range(self.NDS)]
        self.dcnt = [0] * self.NDS
        self.dnext = 0
        self.bufs = {}
        self.out_toks = []

    def buf(self, k):
        b = self.bufs.get(k)
        if b is None:
            b = self.bufs[k] = _B()
        return b

    def _semof(self, key):
        return self.sem[key[1]] if key[0] == "e" else self.dsem[key[1]]

    def _wait(self, eng, toks):
        need = {}
        for t in toks:
            if t is None:
                continue
            key = (t[0], t[1])
            if key == ("e", "pe") and eng == "pe":
                continue
            if need.get(key, 0) < t[2]:
                need[key] = t[2]
        for key, val in need.items():
            if self.seen[eng].get(key, 0) >= val:
                continue
            self.e[eng].wait_ge(self._semof(key), val)
            self.seen[eng][key] = val

    def _deps(self, r, w):
        toks = []
        for k in r:
            toks.append(self.buf(k).w)
        for k in w:
            b = self.buf(k)
            toks.append(b.w)
            for key, val in b.r.items():
                toks.append((key[0], key[1], val))
        return toks

    def _mark(self, tok, r, w):
        key = (tok[0], tok[1])
        for k in r:
            b = self.buf(k)
            if b.r.get(key, 0) < tok[2]:
                b.r[key] = tok[2]
        for k in w:
            b = self.buf(k)
            b.w = tok
            b.r = {}

    def op(self, eng, fn, r=(), w=()):
        self._wait(eng, self._deps(r, w))
        ins = fn(self.e[eng])
        self.cnt[eng] += 1
        ins.then_inc(self.sem[eng], 1)
        tok = ("e", eng, self.cnt[eng])
        self._mark(tok, r, w)
        return tok

    def dma(self, out, in_, r=(), w=(), q="sp", is_out=False, **kw):
        j = self.dnext
        self.dnext = (self.dnext + 1) % self.NDS
        toks = self._deps(r, w)
        if self.dcnt[j] > 0:
            toks.append(("d", j, self.dcnt[j]))
        self._wait(q, toks)
        ins = self.e[q].dma_start(out=out, in_=in_, **kw)
        self.dcnt[j] += 16
        ins.then_inc(self.dsem[j], 16)
        tok = ("d", j, self.dcnt[j])
        self._mark(tok, r, w)
        if is_out:
            self.out_toks.append(tok)
        return tok

    def barrier(self):
        toks = [("e", k, self.cnt[k]) for k in ("pe", "act", "dve", "pool") if self.cnt[k] > 0]
        toks += [("d", j, self.dcnt[j]) for j in range(self.NDS) if self.dcnt[j] > 0]
        for eng in self.ENG:
            self._wait(eng, toks)

    def finish(self):
        self._wait("sp", self.out_toks)
        self._wait("sp", [("e", k, self.cnt[k]) for k in ("pe", "act", "dve", "pool") if self.cnt[k] > 0])


def _din(nc, name, shape, dt):
    return nc.dram_tensor(name, list(shape), dt, kind="ExternalInput").ap()


def _dout(nc, name, shape, dt):
    return nc.dram_tensor(name, list(shape), dt, kind="ExternalOutput").ap()


MCOL = 6 * D // NCORE


def build_M():
    nc = bass.Bass("TRN2", target_bir_lowering=False)
    cT = _din(nc, "cT", [128, KD], F32)
    wada = _din(nc, "wada", [2 * D, MCOL], F32)
    bada = _din(nc, "bada", [1, 2 * MCOL], F32)
    mod = _dout(nc, "mod", [1, 2 * MCOL], F32)
    s = Sched(nc)
    ct = nc.alloc_sbuf_tensor("ct", [128, KD], F32)
    sc = nc.alloc_sbuf_tensor("sc", [128, KD], F32)
    sc2 = nc.alloc_sbuf_tensor("sc2", [128, KD, 2], F32)
    bt = nc.alloc_sbuf_tensor("bt", [2, 2 * MCOL], F32)
    res = nc.alloc_sbuf_tensor("res", [2, 2 * MCOL], F32)
    wt = [nc.alloc_sbuf_tensor(f"wt{i}", [128, KD, 512], F32) for i in range(2)]
    pp = [nc.alloc_psum_tensor(f"pp{i}", [128, 512], F32) for i in range(2)]
    s.dma(ct[:, :], cT, w=["ct"])
    s.dma(bt[0:1, :], bada, w=["bt"])
    s.op("act", lambda e: e.activation(out=sc[:, :], in_=ct[:, :], func=AF.Silu), r=["ct"], w=["sc"])
    s.op("dve", lambda e: e.tensor_copy(out=sc2[:, :, 0], in_=sc[:, :]), r=["sc"], w=["sc2a"])
    s.op("dve", lambda e: e.tensor_copy(out=sc2[:, :, 1], in_=sc[:, :]), r=["sc"], w=["sc2b"])
    i = 0
    for l in range(2):
        for blk in range(MCOL // 512):
            b = i % 2
            src = wada[l * D:(l + 1) * D, blk * 512:(blk + 1) * 512].rearrange("(k p) c -> p k c", p=128)
            s.dma(wt[b][:, :, :], src, w=[f"wt{b}"])
            for k in range(KD):
                s.op("pe", lambda e, k=k, b=b: e.matmul(pp[b][0:2, :], lhsT=sc2[:, k, :], rhs=wt[b][:, k, :],
                                                         start=(k == 0), stop=(k == KD - 1)),
                     r=["sc2a", "sc2b", f"wt{b}"], w=[f"pp{b}"])
            c0 = l * MCOL + blk * 512
            s.op("dve", lambda e, b=b, c0=c0: e.tensor_tensor(out=res[0:1, c0:c0 + 512], in0=pp[b][0:1, :],
                                                              in1=bt[0:1, c0:c0 + 512], op=ALU.add),
                 r=[f"pp{b}", "bt"], w=["res"])
            i += 1
    s.dma(mod, res[0:1, :], r=["res"], is_out=True)
    s.finish()
    return nc


NG = 48
TWO_PI_HI = 6.28125
TWO_PI_LO = 2.0 * math.pi - 6.28125
MAGIC = 12582912.0


def _rms_rstd(s, nc, ss, rstd, tmp, tagr, tagw, ncols):
    s.op("dve", lambda e: e.tensor_scalar(out=tmp, in0=ss, scalar1=1.0 / ncols, scalar2=EPS,
                                          op0=ALU.mult, op1=ALU.add), r=[tagr], w=[tagw + "_t"])
    s.op("act", lambda e: e.activation(out=tmp, in_=tmp, func=AF.Sqrt), r=[tagw + "_t"], w=[tagw + "_t"])
    s.op("dve", lambda e: e.reciprocal(out=rstd, in_=tmp), r=[tagw + "_t"], w=[tagw])


def build_PA1(add_moe):
    nc = bass.Bass("TRN2", target_bir_lowering=False)
    x = _din(nc, "x", [TPC, D], F32)
    win = _din(nc, "win", [NG * 128, KD * 128], F32)
    gA = _din(nc, "gA", [128, KD], F32)
    scA = _din(nc, "scA", [128, KD], F32)
    shA = _din(nc, "shA", [128, KD], F32)
    pos = _din(nc, "pos", [1, TPC], I32)
    invf = _din(nc, "invf", [128, 1], F32)
    perm = _din(nc, "perm", [128, 128], F32)
    ident = _din(nc, "ident", [128, 128], F32)
    bones = _din(nc, "bones", [128, 128], F32)
    if add_moe:
        ys = [_din(nc, f"y{g}", [TPC, D], BF16) for g in range(4)]
        gtm = _din(nc, "gtm", [128, D], F32)
        xnew = _dout(nc, "xnew", [TPC, D], F32)
    qkvT = _dout(nc, "qkvT", [NG * 128, TPC], BF16)
    nrm = _dout(nc, "nrm", [128, 32], F32)
    s = Sched(nc)
    A = nc.alloc_sbuf_tensor
    xnT = A("xnT", [128, KD, TPC], BF16)
    cosT = A("cosT", [128, TPC], F32)
    sinT = A("sinT", [128, TPC], F32)
    xt = [A(f"xt{i}", [128, D], F32) for i in range(2)]
    wst = [A(f"wst{i}", [128, KD, 128], F32) for i in range(2)]
    wb = [A(f"wb{i}", [128, KD, 128], BF16) for i in range(2)]
    qf = [A(f"qf{i}", [128, 512], F32) for i in range(2)]
    t1 = A("t1", [128, 512], F32)
    t2 = A("t2", [128, 512], F32)
    qo = [A(f"qo{i}", [128, 512], BF16) for i in range(2)]
    sq = A("sq", [128, 512], BF16)
    qo3 = [A(f"qo3_{i}", [128, 512], BF16) for i in range(3)]
    t2b = [A(f"t2b{i}", [128, 512], F32) for i in range(2)]
    sqb = [A(f"sqb{i}", [128, 512], BF16) for i in range(2)]
    ga = A("ga", [128, KD], F32)
    sca = A("sca", [128, KD], F32)
    sha = A("sha", [128, KD], F32)
    sh2 = A("sh2", [128, KD, 2], F32)
    aS = A("aS", [128, KD], F32)
    invt = A("invt", [128, 1], F32)
    permt = A("permt", [128, 128], F32)
    idt = A("idt", [128, 128], F32)
    bon = A("bon", [128, 128], BF16)
    posi = A("posi", [128, TPC], I32)
    small = A("small", [128, 8], F32)
    biasg = A("biasg", [128, NG], F32)
    nm = A("nm", [128, 32, 4], F32)
    nmr = A("nmr", [128, 32], F32)
    junk = A("junk", [128, D], F32)
    if add_moe:
        yt = [[A(f"yt{g}_{i}", [128, D], BF16) for i in range(2)] for g in range(4)]
        gtt = A("gtt", [128, D], F32)
    P = [nc.alloc_psum_tensor(f"P{i}", [128, 512], F32) for i in range(8)]

    s.dma(ga[:, :], gA, w=["ga"])
    s.dma(sca[:, :], scA, w=["sca"])
    s.dma(sha[:, :], shA, w=["sha"])
    s.dma(invt[:, :], invf, w=["invt"])
    s.dma(permt[:, :], perm, w=["permt"])
    s.dma(idt[:, :], ident, w=["idt"])
    s.dma(bon[:, :], bones, w=["bon"], q="pool")
    s.dma(posi[:, :], pos[0:1, :].broadcast_to([128, TPC]), w=["posi"])
    if add_moe:
        s.dma(gtt[:, :], gtm, w=["gtt"])
    s.op("dve", lambda e: e.tensor_scalar(out=aS[:, :], in0=sca[:, :], scalar1=1.0, scalar2=None, op0=ALU.add),
         r=["sca"], w=["aS"])
    s.op("dve", lambda e: e.tensor_tensor(out=aS[:, :], in0=aS[:, :], in1=ga[:, :], op=ALU.mult), r=["aS", "ga"], w=["aS"])
    s.op("dve", lambda e: e.tensor_copy(out=sh2[:, :, 0], in_=sha[:, :]), r=["sha"], w=["sh2a"])
    s.op("dve", lambda e: e.tensor_copy(out=sh2[:, :, 1], in_=sha[:, :]), r=["sha"], w=["sh2b"])
    s.op("dve", lambda e: e.tensor_copy(out=cosT[:, :], in_=posi[:, :]), r=["posi"], w=["cosT"])
    s.op("dve", lambda e: e.tensor_scalar(out=cosT[:, :], in0=cosT[:, :], scalar1=invt[:, 0:1], scalar2=None, op0=ALU.mult),
         r=["cosT", "invt"], w=["cosT"])
    s.op("dve", lambda e: e.tensor_scalar(out=sinT[:, :], in0=cosT[:, :], scalar1=1.0 / (2.0 * math.pi), scalar2=None, op0=ALU.mult),
         r=["cosT"], w=["sinT"])
    s.op("dve", lambda e: e.tensor_scalar(out=sinT[:, :], in0=sinT[:, :], scalar1=MAGIC, scalar2=None, op0=ALU.add),
         r=["sinT"], w=["sinT"])
    s.op("dve", lambda e: e.tensor_scalar(out=sinT[:, :], in0=sinT[:, :], scalar1=-MAGIC, scalar2=None, op0=ALU.add),
         r=["sinT"], w=["sinT"])
    s.op("dve", lambda e: e.scalar_tensor_tensor(out=cosT[:, :], in0=sinT[:, :], scalar=-TWO_PI_HI, in1=cosT[:, :],
                                                 op0=ALU.mult, op1=ALU.add), r=["sinT", "cosT"], w=["cosT"])
    s.op("dve", lambda e: e.scalar_tensor_tensor(out=cosT[:, :], in0=sinT[:, :], scalar=-TWO_PI_LO, in1=cosT[:, :],
                                                 op0=ALU.mult, op1=ALU.add), r=["sinT", "cosT"], w=["cosT"])
    s.op("dve", lambda e: e.tensor_scalar(out=cosT[:, :], in0=cosT[:, :], scalar1=-math.pi, scalar2=math.pi, op0=ALU.max, op1=ALU.min),
         r=["cosT"], w=["cosT"])
    s.op("act", lambda e: e.activation(out=sinT[:, :], in_=cosT[:, :], func=AF.Sin), r=["cosT"], w=["sinT"])
    for c4 in range(TPC // 512):
        cs = slice(c4 * 512, (c4 + 1) * 512)
        s.op("dve", lambda e, cs=cs: e.tensor_scalar(out=t1[:, :], in0=cosT[:, cs], scalar1=math.pi / 2, scalar2=None, op0=ALU.is_gt),
             r=["cosT"], w=["t1"])
        s.op("dve", lambda e, cs=cs: e.tensor_scalar(out=t2[:, :], in0=cosT[:, cs], scalar1=math.pi / 2, scalar2=None, op0=ALU.add),
             r=["cosT"], w=["t2"])
        s.op("dve", lambda e, cs=cs: e.scalar_tensor_tensor(out=t2[:, :], in0=t1[:, :], scalar=-2.0 * math.pi, in1=t2[:, :],
                                                            op0=ALU.mult, op1=ALU.add), r=["t1", "t2"], w=["t2"])
        s.op("dve", lambda e: e.tensor_scalar(out=t2[:, :], in0=t2[:, :], scalar1=-math.pi, scalar2=math.pi, op0=ALU.max, op1=ALU.min),
             r=["t2"], w=["t2"])
        s.op("act", lambda e, cs=cs: e.activation(out=cosT[:, cs], in_=t2[:, :], func=AF.Sin), r=["t2", "cosT"], w=["cosT"])

    def loads_b(t):
        b = t % 2
        rows = slice(t * 128, (t + 1) * 128)
        s.dma(xt[b][:, :], x[rows, :], w=[f"xt{b}"])
        if add_moe:
            for g in range(4):
                s.dma(yt[g][b][:, :], ys[g][rows, :], w=[f"yt{g}{b}"])

    loads_b(0)
    for t in range(TPC // 128):
        b = t % 2
        rows = slice(t * 128, (t + 1) * 128)
        if t + 1 < TPC // 128:
            loads_b(t + 1)
        if add_moe:
            s.op("pool", lambda e, b=b: e.tensor_tensor(out=yt[0][b][:, :], in0=yt[0][b][:, :], in1=yt[1][b][:, :], op=ALU.add), r=[f"yt0{b}", f"yt1{b}"], w=[f"yt0{b}"])
            s.op("pool", lambda e, b=b: e.tensor_tensor(out=yt[2][b][:, :], in0=yt[2][b][:, :], in1=yt[3][b][:, :], op=ALU.add), r=[f"yt2{b}", f"yt3{b}"], w=[f"yt2{b}"])
            for c4 in range(4):
                cs = slice(c4 * 512, (c4 + 1) * 512)
                s.op("dve", lambda e, cs=cs, b=b: e.tensor_tensor(out=t1[:, :], in0=yt[0][b][:, cs], in1=yt[2][b][:, cs], op=ALU.add),
                     r=[f"yt0{b}", f"yt2{b}"], w=["t1"])
                s.op("dve", lambda e, cs=cs: e.tensor_tensor(out=t1[:, :], in0=t1[:, :], in1=gtt[:, cs], op=ALU.mult),
                     r=["t1", "gtt"], w=["t1"])
                s.op("dve", lambda e, cs=cs, b=b: e.tensor_tensor(out=xt[b][:, cs], in0=xt[b][:, cs], in1=t1[:, :], op=ALU.add),
                     r=["t1", f"xt{b}"], w=[f"xt{b}"])
            s.dma(xnew[rows, :], xt[b][:, :], r=[f"xt{b}"], is_out=True)
        s.op("act", lambda e, b=b: e.activation(out=junk[:, :], in_=xt[b][:, :], func=AF.Square, accum_out=small[:, 0:1]),
             r=[f"xt{b}"], w=["junk", "ss"])
        _rms_rstd(s, nc, small[:, 0:1], small[:, 1:2], small[:, 2:3], "ss", "rstd", D)
        s.op("dve", lambda e, b=b: e.tensor_scalar(out=xt[b][:, :], in0=xt[b][:, :], scalar1=small[:, 1:2], scalar2=None, op0=ALU.mult),
             r=[f"xt{b}", "rstd"], w=[f"xt{b}"])
        for k4 in range(4):
            pb = P[(t % 2) * 4 + k4]
            for kk in range(4):
                k = k4 * 4 + kk
                s.op("pe", lambda e, pb=pb, kk=kk, k=k, b=b: e.transpose(out=pb[:, kk * 128:(kk + 1) * 128], in_=xt[b][:, k * 128:(k + 1) * 128],
                                                                        identity=idt[:, :]),
                     r=[f"xt{b}", "idt"], w=[f"P{(t % 2) * 4 + k4}"])
            eng = "act" if k4 % 2 == 0 else "dve"
            dst = xnT[:, k4 * 4:(k4 + 1) * 4, t * 128:(t + 1) * 128]
            srcv = pb[:, :].rearrange("p (k t) -> p k t", k=4)
            if eng == "act":
                s.op("act", lambda e, dst=dst, srcv=srcv: e.copy(out=dst, in_=srcv), r=[f"P{(t % 2) * 4 + k4}"], w=["xnT"])
            else:
                s.op("dve", lambda e, dst=dst, srcv=srcv: e.tensor_copy(out=dst, in_=srcv), r=[f"P{(t % 2) * 4 + k4}"], w=["xnT"])

    NCH = TPC // 512
    iters = [(g, ch) for g in range(NG) for ch in range(NCH)]
    NI = len(iters)

    def w_dma(g):
        s.dma(wst[g % 2][:, :, :], win[g * 128:(g + 1) * 128, :].rearrange("p (k c) -> p k c", k=KD), w=[f"wst{g % 2}"])

    def w_prep(g):
        wbuf = g % 2
        for k in range(KD):
            s.op("pe", lambda e, k=k, wbuf=wbuf: e.matmul(P[6][:, 0:2], lhsT=wst[wbuf][:, k, :], rhs=sh2[:, k, :],
                                                           start=(k == 0), stop=(k == KD - 1)),
                 r=[f"wst{wbuf}", "sh2a", "sh2b"], w=["P6"])
        s.op("dve", lambda e, g=g: e.tensor_copy(out=biasg[:, g:g + 1], in_=P[6][:, 0:1]), r=["P6"], w=[f"biasg{g}"])
        s.op("dve", lambda e, wbuf=wbuf: e.tensor_tensor(out=wb[wbuf][:, :, :], in0=wst[wbuf][:, :, :],
                                                          in1=aS[:, :].unsqueeze(2).broadcast_to([128, KD, 128]), op=ALU.mult),
             r=[f"wst{wbuf}", "aS"], w=[f"wb{wbuf}"])

    def stA(it):
        g, ch = iters[it]
        cs = slice(ch * 512, (ch + 1) * 512)
        if ch == 0 and g + 1 < NG:
            w_dma(g + 1)
        pq, pqn = P[it % 2], f"P{it % 2}"
        for k in range(KD):
            s.op("pe", lambda e, k=k, g=g, pq=pq, cs=cs: e.matmul(pq[:, :], lhsT=wb[g % 2][:, k, :], rhs=xnT[:, k, cs],
                                                                  start=(k == 0), stop=(k == KD - 1)),
                 r=[f"wb{g % 2}", "xnT"], w=[pqn])
        if ch == 2 and g + 1 < NG:
            w_prep(g + 1)
        if g % 6 >= 4:
            ob = it % 3
            s.op("act", lambda e, pq=pq, ob=ob, g=g: e.activation(out=qo3[ob][:, :], in_=pq[:, :], func=AF.Identity, bias=biasg[:, g:g + 1]),
                 r=[pqn, f"biasg{g}"], w=[f"qo{ob}"])
            s.dma(qkvT[g * 128:(g + 1) * 128, cs], qo3[ob][:, :], r=[f"qo{ob}"], is_out=True)
        else:
            b2 = it % 2
            s.op("act", lambda e, pq=pq, b2=b2, g=g: e.activation(out=qf[b2][:, :], in_=pq[:, :], func=AF.Identity, bias=biasg[:, g:g + 1]),
                 r=[pqn, f"biasg{g}"], w=[f"qf{b2}"])
            s.op("dve", lambda e, b2=b2, cs=cs: e.tensor_tensor(out=t2b[b2][:, :], in0=qf[b2][:, :], in1=cosT[:, cs], op=ALU.mult),
                 r=[f"qf{b2}", "cosT"], w=[f"t2b{b2}"])

    def stC(it):
        g, ch = iters[it]
        if g % 6 >= 4:
            return
        cs = slice(ch * 512, (ch + 1) * 512)
        b2, ob = it % 2, it % 3
        pr, prn = P[2 + b2], f"P{2 + b2}"
        s.op("pe", lambda e, pr=pr, b2=b2: e.matmul(pr[:, :], lhsT=permt[:, :], rhs=qf[b2][:, :], start=True, stop=True),
             r=["permt", f"qf{b2}"], w=[prn])
        s.op("dve", lambda e, pr=pr, cs=cs: e.tensor_tensor(out=t1[:, :], in0=pr[:, :], in1=sinT[:, cs], op=ALU.mult), r=[prn, "sinT"], w=["t1"])
        s.op("dve", lambda e, ob=ob, b2=b2: e.tensor_tensor(out=qo3[ob][:, :], in0=t1[:, :], in1=t2b[b2][:, :], op=ALU.add),
             r=["t1", f"t2b{b2}"], w=[f"qo{ob}"])
        s.dma(qkvT[g * 128:(g + 1) * 128, cs], qo3[ob][:, :], r=[f"qo{ob}"], is_out=True)
        s.op("act", lambda e, ob=ob, b2=b2: e.activation(out=sqb[b2][:, :], in_=qo3[ob][:, :], func=AF.Square), r=[f"qo{ob}"], w=[f"sqb{b2}"])

    def stE(it):
        g, ch = iters[it]
        if g % 6 >= 4:
            return
        b2 = it % 2
        pn, pnn = P[4 + b2], f"P{4 + b2}"
        s.op("pe", lambda e, pn=pn, b2=b2: e.matmul(pn[:, :], lhsT=bon[:, :], rhs=sqb[b2][:, :], start=True, stop=True), r=["bon", f"sqb{b2}"], w=[pnn])
        gq = (g // 6) * 4 + g % 6
        s.op("dve", lambda e, pn=pn, gq=gq, ch=ch: e.tensor_reduce(out=nm[:, gq, ch:ch + 1], in_=pn[:, :], axis=AX.X, op=ALU.max), r=[pnn], w=["nm"])

    w_dma(0)
    w_prep(0)
    for it in range(NI + 2):
        if it < NI:
            stA(it)
        if 0 <= it - 1 < NI:
            stC(it - 1)
        if 0 <= it - 2 < NI:
            stE(it - 2)
    s.op("dve", lambda e: e.tensor_reduce(out=nmr[:, :], in_=nm[:, :, :], axis=AX.X, op=ALU.max), r=["nm"], w=["nmr"])
    s.dma(nrm, nmr[:, :], r=["nmr"], is_out=True)
    s.finish()
    return nc


def build_A2():
    nc = bass.Bass("TRN2", target_bir_lowering=False)
    qab = _din(nc, "qab", [128, S], BF16)
    kab = _din(nc, "kab", [128, S], BF16)
    qcT = _din(nc, "qcT", [128, S], BF16)
    kcT = _din(nc, "kcT", [128, S], BF16)
    vcP = _din(nc, "vcP", [128, 128 * 128], BF16)
    vbP = _din(nc, "vbP", [128, 128 * 128], BF16)
    vaP = _din(nc, "vaP", [128, 3 * 128 * 64], BF16)
    Ecs = _din(nc, "Ecs", [64, S], BF16)
    nrmb = _din(nc, "nrmb", [128, 64], F32)
    lamt = _din(nc, "lamt", [128, 256], F32)
    lamc = _din(nc, "lamc", [128, 2], F32)
    gsub = _din(nc, "gsub", [128, 1], F32)
    mask2 = _din(nc, "mask2", [128, 256], F32)
    ident = _din(nc, "ident", [128, 128], F32)
    oT = _dout(nc, "oT", [256, S], BF16)
    s = Sched(nc)
    A = nc.alloc_sbuf_tensor
    BIG = [A(f"BIG{i}", [128, S], BF16) for i in range(4)]
    m2 = A("m2", [128, 256], BF16)
    idt = A("idt", [128, 128], F32)
    ones_b = A("ones_b", [128, 128], BF16)
    ones_f = A("ones_f", [128, 128], F32)
    nr_in = A("nr_in", [128, 64], F32)
    nr = A("nr", [128, 8], F32)
    negM = A("negM", [128, 4], F32)
    lt = A("lt", [128, 256], F32)
    lc = A("lc", [128, 2], F32)
    gs = A("gs", [128, 1], F32)
    sm = A("sm", [128, 8], F32)
    pt = [[A(f"pt{m}{i}", [128, 512], BF16) for i in range(2)] for m in range(2)]
    e = [A(f"e{i}", [128, 512], F32) for i in range(4)]
    ob = [A(f"ob{i}", [128, 512], BF16) for i in range(2)]
    P = [nc.alloc_psum_tensor(f"P{i}", [128, 512], F32) for i in range(8)]
    tri = m2[:, 128:256]
    accC = [A(f"accC{i}", [128, 512], F32) for i in range(2)]

    s.dma(m2[:, :], mask2, w=["m2"], q="pool")
    s.dma(idt[:, :], ident, w=["idt"])
    s.dma(nr_in[:, :], nrmb, w=["nr_in"])
    s.dma(lt[:, :], lamt, w=["lt"])
    s.dma(lc[:, :], lamc, w=["lc"])
    s.dma(gs[:, :], gsub, w=["gs"])
    s.op("pool", lambda g: g.memset(ones_b[:, :], 1.0), w=["ones_b"])
    s.op("pool", lambda g: g.memset(ones_f[:, :], 1.0), w=["ones_f"])
    s.op("dve", lambda g: g.tensor_reduce(out=nr[:, :], in_=nr_in[:, :].rearrange("p (q c) -> p q c", c=8), axis=AX.X, op=ALU.max),
         r=["nr_in"], w=["nr"])
    nr4 = nr[:, :].rearrange("p (s t h) -> p s t h", s=2, t=2)
    s.op("dve", lambda g: g.tensor_tensor(out=negM[:, :].rearrange("p (s h) -> p s h", s=2), in0=nr4[:, :, 0, :], in1=nr4[:, :, 1, :], op=ALU.mult),
         r=["nr"], w=["negM"])
    s.op("act", lambda g: g.activation(out=negM[:, :], in_=negM[:, :], func=AF.Sqrt), r=["negM"], w=["negM"])
    s.op("dve", lambda g: g.tensor_scalar(out=negM[:, :], in0=negM[:, :], scalar1=-1.02 * 0.125, scalar2=None, op0=ALU.mult),
         r=["negM"], w=["negM"])
    s.op("dve", lambda g: g.tensor_tensor(out=lt[:, 0:64], in0=lt[:, 0:64], in1=lt[:, 64:128], op=ALU.mult), r=["lt"], w=["lt"])
    s.op("dve", lambda g: g.tensor_tensor(out=lt[:, 128:192], in0=lt[:, 128:192], in1=lt[:, 192:256], op=ALU.mult), r=["lt"], w=["lt"])
    s.op("dve", lambda g: g.tensor_reduce(out=sm[:, 0:1], in_=lt[:, 0:64], axis=AX.X, op=ALU.add), r=["lt"], w=["sm"])
    s.op("dve", lambda g: g.tensor_reduce(out=sm[:, 1:2], in_=lt[:, 128:192], axis=AX.X, op=ALU.add), r=["lt"], w=["sm"])
    s.op("act", lambda g: g.activation(out=sm[:, 0:2], in_=sm[:, 0:2], func=AF.Exp), r=["sm"], w=["sm"])
    s.op("dve", lambda g: g.tensor_tensor(out=sm[:, 2:3], in0=sm[:, 1:2], in1=sm[:, 0:1], op=ALU.subtract), r=["sm"], w=["sm"])
    s.op("dve", lambda g: g.tensor_tensor(out=sm[:, 2:3], in0=sm[:, 2:3], in1=lc[:, 0:1], op=ALU.subtract), r=["sm", "lc"], w=["sm"])
    s.op("dve", lambda g: g.tensor_tensor(out=sm[:, 3:4], in0=gs[:, 0:1], in1=lc[:, 1:2], op=ALU.mult), r=["gs", "lc"], w=["sm"])
    neglam = sm[:, 2:3]
    gsc = sm[:, 3:4]

    def causal_rng(c, kb):
        j = kb - 4 * c
        off = 128 * j if j > 0 else 0
        return j, off, 512 - off

    QC, KC = BIG[0], BIG[1]
    VC = BIG[2][:, :].rearrange("p (t d) -> p t d", d=128)
    s.dma(QC[:, :], qcT, w=["QC"])
    s.dma(KC[:, :], kcT, w=["KC"])
    s.dma(BIG[2][:, :], vcP, w=["VC"])
    SC = [[P[0], P[1]], [P[2], P[3]]]
    SCn = [["P0", "P1"], ["P2", "P3"]]
    O, On = [P[4], P[5]], ["P4", "P5"]
    Dn_, Dnn = [P[6], P[7]], ["P6", "P7"]
    it = 0
    pend_c = None

    def c_tail(c, itx):
        sb, sbn = SC[0][itx % 2], SCn[0][itx % 2]
        s.op("pe", lambda g, sb=sb: g.matmul(sb[:, :], lhsT=ones_f[:, :], rhs=e[1][:, :], start=True, stop=True), r=["ones_f", "e1"], w=[sbn])
        s.op("dve", lambda g, sb=sb: g.tensor_scalar(out=e[2][:, :], in0=sb[:, :], scalar1=1.0 / 128, scalar2=EPS, op0=ALU.mult, op1=ALU.add),
             r=[sbn], w=["e2"])
        s.op("act", lambda g: g.activation(out=e[2][:, :], in_=e[2][:, :], func=AF.Sqrt), r=["e2"], w=["e2"])
        s.op("dve", lambda g: g.reciprocal(out=e[2][:, :], in_=e[2][:, :]), r=["e2"], w=["e2"])
        o_ = ob[c % 2]
        s.op("dve", lambda g, o_=o_: g.scalar_tensor_tensor(out=o_[:, :], in0=e[0][:, :], scalar=gsc, in1=e[2][:, :], op0=ALU.mult, op1=ALU.mult),
             r=["e0", "e2", "sm"], w=[f"ob{c % 2}"])
        s.dma(oT[128:256, c * 512:(c + 1) * 512], o_[:, :], r=[f"ob{c % 2}"], is_out=True)

    for c in range(S // 512):
        nkb = 4 * c + 4

        def qk(kb, itx):
            j, off, n = causal_rng(c, kb)
            q0 = c * 512 + off
            for m in range(2):
                r0 = 64 * m
                sb, sbn = SC[m][itx % 2], SCn[m][itx % 2]
                s.op("pe", lambda g, sb=sb, r0=r0, kb=kb, q0=q0, n=n: g.matmul(sb[:, 0:n], lhsT=KC[r0:r0 + 64, kb * 128:(kb + 1) * 128],
                                                                              rhs=QC[r0:r0 + 64, q0:q0 + n], start=True, stop=True),
                     r=["KC", "QC"], w=[sbn])
        qk(0, it)
        for kb in range(nkb):
            if kb + 1 < nkb:
                qk(kb + 1, it + 1)
            j, off, n = causal_rng(c, kb)
            for m in range(2):
                sb, sbn = SC[m][it % 2], SCn[m][it % 2]
                p_, pn = pt[m][it % 2], f"pt{m}{it % 2}"
                s.op("act", lambda g, sb=sb, p_=p_, n=n, m=m: g.activation(out=p_[:, 0:n], in_=sb[:, 0:n], func=AF.Exp,
                                                                           bias=negM[:, 2 + m:3 + m], scale=0.125),
                     r=[sbn, "negM"], w=[pn])
                if j >= 0:
                    s.op("pool", lambda g, p_=p_: g.tensor_tensor(out=p_[:, 0:128], in0=p_[:, 0:128], in1=tri, op=ALU.mult),
                         r=[pn, "m2"], w=[pn])
                s.op("pe", lambda g, p_=p_, n=n, off=off, kb=kb, m=m: g.matmul(O[m][:, off:512], lhsT=VC[:, kb, :], rhs=p_[:, 0:n],
                                                                              start=(kb == 0), stop=(kb == nkb - 1)),
                     r=[pn, "VC"], w=[On[m]])
                s.op("pe", lambda g, p_=p_, n=n, off=off, kb=kb, m=m: g.matmul(Dn_[m][:, off:512], lhsT=ones_b[:, :], rhs=p_[:, 0:n],
                                                                              start=(kb == 0), stop=(kb == nkb - 1)),
                     r=[pn, "ones_b"], w=[Dnn[m]])
            it += 1
            if pend_c is not None and kb == min(nkb - 1, 10):
                c_tail(pend_c, it + 1)
                pend_c = None
        s.op("act", lambda g: g.copy(out=e[0][:, :], in_=O[0][:, :]), r=[On[0]], w=["e0"])
        s.op("act", lambda g: g.copy(out=e[1][:, :], in_=O[1][:, :]), r=[On[1]], w=["e1"])
        s.op("dve", lambda g: g.tensor_copy(out=e[2][:, :], in_=Dn_[0][:, :]), r=[Dnn[0]], w=["e2"])
        s.op("dve", lambda g: g.tensor_copy(out=e[3][:, :], in_=Dn_[1][:, :]), r=[Dnn[1]], w=["e3"])
        s.op("dve", lambda g: g.reciprocal(out=e[2][:, :], in_=e[2][:, :]), r=["e2"], w=["e2"])
        s.op("dve", lambda g: g.reciprocal(out=e[3][:, :], in_=e[3][:, :]), r=["e3"], w=["e3"])
        s.op("pool", lambda g: g.tensor_tensor(out=e[0][:, :], in0=e[0][:, :], in1=e[2][:, :], op=ALU.mult), r=["e0", "e2"], w=["e0"])
        s.op("pool", lambda g: g.tensor_tensor(out=e[1][:, :], in0=e[1][:, :], in1=e[3][:, :], op=ALU.mult), r=["e1", "e3"], w=["e1"])
        s.op("dve", lambda g: g.scalar_tensor_tensor(out=e[0][:, :], in0=e[1][:, :], scalar=neglam, in1=e[0][:, :], op0=ALU.mult, op1=ALU.add),
             r=["e0", "e1", "sm"], w=["e0"])
        s.op("pool", lambda g: g.tensor_tensor(out=e[1][:, :], in0=e[0][:, :], in1=e[0][:, :], op=ALU.mult), r=["e0"], w=["e1"])
        pend_c = c
    c_tail(pend_c, it)

    s.barrier()
    KBE, QBB, QBL = BIG[0], BIG[1], BIG[3]
    VB = BIG[2][:, :].rearrange("p (t d) -> p t d", d=128)
    s.dma(KBE[0:64, :], Ecs, w=["KBE"])
    s.dma(KBE[64:128, :], kab[64:128, :], w=["KBEk"])
    s.dma(QBB[0:64, :], kab[64:128, :], w=["B1lo"])
    s.dma(QBB[64:128, :], qab[64:128, :], w=["B1hi"])
    s.dma(QBL[0:64, :], qab[64:128, :], w=["QBL"])
    s.dma(BIG[2][:, :], vbP, w=["VB"])
    kmT = A("kmT", [128, 64], F32)
    qgf = [A(f"qgf{i}", [128, 128], F32) for i in range(2)]
    gsb = [A(f"gsb{i}", [128, 64], F32) for i in range(2)]
    top8 = [A(f"top8{i}", [128, 8], F32) for i in range(2)]
    bt = [A(f"bt{i}", [128, 128], F32) for i in range(2)]
    for i in range(2):
        s.op("pool", lambda g, i=i: g.memset(bt[i][:, :], 0.0), w=[f"bt{i}"])
    s.op("dve", lambda g: g.tensor_reduce(out=kmT[0:64, :], in_=QBB[0:64, :].rearrange("p (n t) -> p n t", t=256), axis=AX.X, op=ALU.add),
         r=["B1lo"], w=["kmT"])
    s.op("dve", lambda g: g.tensor_scalar(out=kmT[0:64, :], in0=kmT[0:64, :], scalar1=1.0 / 256, scalar2=None, op0=ALU.mult),
         r=["kmT"], w=["kmT"])
    SB, SBn = [P[0], P[1], P[2]], ["P0", "P1", "P2"]
    OB, DB, GP, TP = P[3], P[5], [P[4], P[5]], [P[6], P[7]]
    ptB = [pt[0][0], pt[0][1], pt[1][0]]
    ptBn = ["pt00", "pt01", "pt10"]
    it = 0

    def gate1(c, tls):
        for tl in tls:
            t = 4 * c + tl
            own = t // 2
            b = tl % 2
            s.op("act", lambda g, b=b, t=t: g.copy(out=qgf[b][0:64, :], in_=QBL[0:64, t * 128:(t + 1) * 128]), r=["QBL"], w=[f"qgf{b}"])
            s.op("pe", lambda g, b=b: g.matmul(GP[b][:, 0:64], lhsT=qgf[b][0:64, :], rhs=kmT[0:64, :], start=True, stop=True),
                 r=[f"qgf{b}", "kmT"], w=[f"P{4 + b}"])
            s.op("dve", lambda g, b=b: g.memset(gsb[b][:, :], -1e30), w=[f"gsb{b}"])
            if own > 0:
                s.op("dve", lambda g, b=b, own=own: g.tensor_copy(out=gsb[b][:, 0:own], in_=GP[b][:, 0:own]), r=[f"P{4 + b}"], w=[f"gsb{b}"])
            s.op("dve", lambda g, b=b: g.max(out=top8[b][:, :], in_=gsb[b][:, :]), r=[f"gsb{b}"], w=[f"top8{b}"])
            s.op("dve", lambda g, b=b: g.tensor_scalar(out=bt[b][:, 0:64], in0=gsb[b][:, :], scalar1=top8[b][:, 2:3], scalar2=None, op0=ALU.is_ge),
                 r=[f"gsb{b}", f"top8{b}"], w=[f"bt{b}"])
            s.op("dve", lambda g, b=b: g.tensor_scalar(out=bt[b][:, 0:64], in0=bt[b][:, 0:64], scalar1=-1.0, scalar2=-NEG, op0=ALU.add, op1=ALU.mult),
                 r=[f"bt{b}"], w=[f"bt{b}"])
            s.op("dve", lambda g, b=b, own=own: g.memset(bt[b][:, own:own + 1], 0.0), w=[f"bt{b}"])

    def gate2(c, tls):
        for tl in tls:
            t = 4 * c + tl
            b = tl % 2
            s.op("pe", lambda g, b=b: g.transpose(out=TP[b][:, 0:128], in_=bt[b][:, :], identity=idt[:, :]), r=[f"bt{b}", "idt"], w=[f"P{6 + b}"])
            s.op("act", lambda g, b=b, t=t: g.copy(out=QBB[0:64, t * 128:(t + 1) * 128], in_=TP[b][0:64, 0:128]),
                 r=[f"P{6 + b}", "kmT"], w=[f"B1lo{c}"] if c > 0 else ["B1lo"])

    NCB = S // 512
    gate1(0, (0, 1)); gate2(0, (0, 1)); gate1(0, (2, 3)); gate2(0, (2, 3))
    for c in range(NCB):
        nkb = 4 * c + 4
        half = nkb // 2
        lok = f"B1lo{c}" if c > 0 else "B1lo"

        def qkb(kb, itx):
            j, off, n = causal_rng(c, kb)
            q0 = c * 512 + off
            s.op("pe", lambda g, kb=kb, q0=q0, n=n, itx=itx: g.matmul(SB[itx % 3][:, 0:n], lhsT=KBE[:, kb * 128:(kb + 1) * 128],
                                                                     rhs=QBB[:, q0:q0 + n], start=True, stop=True),
                 r=["KBE", "KBEk", lok, "B1hi"], w=[SBn[itx % 3]])
        qkb(0, it)
        qkb(1, it + 1)
        for kb in range(nkb):
            if c + 1 < NCB:
                if kb == 0:
                    gate1(c + 1, (0, 1))
                elif kb == half:
                    gate2(c + 1, (0, 1))
                    gate1(c + 1, (2, 3))
            if kb + 2 < nkb:
                qkb(kb + 2, it + 2)
            j, off, n = causal_rng(c, kb)
            p_, pn = ptB[it % 3], ptBn[it % 3]
            s.op("act", lambda g, p_=p_, n=n, it=it: g.activation(out=p_[:, 0:n], in_=SB[it % 3][:, 0:n], func=AF.Exp, bias=negM[:, 1:2], scale=0.125),
                 r=[SBn[it % 3], "negM"], w=[pn])
            if j >= 0:
                s.op("pool", lambda g, p_=p_: g.tensor_tensor(out=p_[:, 0:128], in0=p_[:, 0:128], in1=tri, op=ALU.mult), r=[pn, "m2"], w=[pn])
            s.op("pe", lambda g, p_=p_, n=n, off=off, kb=kb: g.matmul(OB[:, off:512], lhsT=VB[:, kb, :], rhs=p_[:, 0:n],
                                                                     start=(kb == 0), stop=(kb == nkb - 1)), r=[pn, "VB"], w=["P3"])
            it += 1
        if c + 1 < NCB:
            gate2(c + 1, (2, 3))
        s.op("act", lambda g: g.copy(out=e[1][:, :], in_=OB[:, :]), r=["P3"], w=["e1"])
        s.op("pe", lambda g: g.matmul(DB[0:64, :], lhsT=idt[:, 64:128], rhs=e[1][:, :], start=True, stop=True), r=["idt", "e1"], w=["P5"])
        s.op("dve", lambda g: g.reciprocal(out=e[0][0:64, :], in_=DB[0:64, :]), r=["P5"], w=["e0"])
        o_ = ob[c % 2]
        s.op("dve", lambda g, o_=o_: g.tensor_tensor(out=o_[0:64, :], in0=e[1][0:64, :], in1=e[0][0:64, :], op=ALU.mult), r=["e1", "e0"], w=[f"ob{c % 2}"])
        s.dma(oT[64:128, c * 512:(c + 1) * 512], o_[0:64, :], r=[f"ob{c % 2}"], is_out=True)

    s.barrier()
    KA, QA = BIG[0], BIG[1]
    VA = [BIG[2][:, 0:8192].rearrange("p (t d) -> p t d", d=64), BIG[2][:, 8192:16384].rearrange("p (t d) -> p t d", d=64),
          BIG[3][:, 0:8192].rearrange("p (t d) -> p t d", d=64)]
    s.dma(KA[0:64, :], kab[0:64, :], w=["KA"])
    s.dma(QA[0:64, :], qab[0:64, :], w=["QA"])
    s.dma(BIG[2][:, :], vaP[:, 0:16384], w=["VA"])
    s.dma(BIG[3][:, 0:8192], vaP[:, 16384:24576], w=["VA2"])
    ND = A("ND", [128, 2, 2048], F32)
    oa = A("oa", [128, 2048], BF16)
    SA, SAn = [P[0], P[1], P[2], P[3]], ["P0", "P1", "P2", "P3"]
    OA, OAn = [P[4], P[5]], ["P4", "P5"]
    DA, DAn = [P[6], P[7]], ["P6", "P7"]
    ptA = [pt[0][0], pt[0][1], pt[1][0], pt[1][1]]
    ptAn = ["pt00", "pt01", "pt10", "pt11"]
    for s8 in range(S // 2048):
        units = []
        for pi, d in enumerate((1, 4, 16)):
            nbl = 16 // d
            for r in range(d):
                for bl in range(nbl):
                    units.append((pi, d, r, s8 * nbl + bl))

        def a_qk(u, i):
            pi, d, r, bg = u
            st = 128 * d * bg + r
            qs = slice(st, st + 127 * d + 1, d)
            b4 = i % 4
            if bg > 0:
                ps_ = slice(st - 128 * d, st - 128 * d + 127 * d + 1, d)
                s.op("pe", lambda g, b4=b4, ps_=ps_, qs=qs: g.matmul(SA[b4][:, 0:128], lhsT=KA[0:64, ps_], rhs=QA[0:64, qs], start=True, stop=True),
                     r=["KA", "QA"], w=[SAn[b4]])
            s.op("pe", lambda g, b4=b4, qs=qs: g.matmul(SA[b4][:, 128:256], lhsT=KA[0:64, qs], rhs=QA[0:64, qs], start=True, stop=True),
                 r=["KA", "QA"], w=[SAn[b4]])

        def a_rest(u, i):
            pi, d, r, bg = u
            st = 128 * d * bg + r
            off = st - s8 * 2048
            osl = slice(off, off + 127 * d + 1, d)
            tile_i = r * (128 // d) + bg
            b4, b = i % 4, i % 2
            lo = 0 if bg > 0 else 128
            p_, pn = ptA[b4], ptAn[b4]
            s.op("act", lambda g, b4=b4, p_=p_, lo=lo: g.activation(out=p_[:, lo:256], in_=SA[b4][:, lo:256], func=AF.Exp, bias=negM[:, 0:1], scale=0.125),
                 r=[SAn[b4], "negM"], w=[pn])
            s.op("pool", lambda g, p_=p_, lo=lo: g.tensor_tensor(out=p_[:, lo:256], in0=p_[:, lo:256], in1=m2[:, lo:256], op=ALU.mult),
                 r=[pn, "m2"], w=[pn])
            first = True
            for isv in (True, False):
                c0 = 0 if isv else 128
                lp = (VA[pi][:, tile_i - 1, :] if bg > 0 else None) if isv else ones_b[:, 0:64]
                lcur = VA[pi][:, tile_i, :] if isv else ones_b[:, 0:64]
                vkeys = ["VA", "VA2"] if isv else ["ones_b"]
                if bg > 0:
                    s.op("pe", lambda g, b=b, lp=lp, p_=p_, c0=c0, first=first: g.matmul(OA[b][0:64, c0:c0 + 128], lhsT=lp, rhs=p_[:, 0:128], start=first, stop=False),
                         r=[pn] + vkeys, w=[OAn[b]])
                    first = False
                s.op("pe", lambda g, b=b, lcur=lcur, p_=p_, c0=c0, first=first, isv=isv: g.matmul(OA[b][0:64, c0:c0 + 128], lhsT=lcur, rhs=p_[:, 128:256],
                                                                                                 start=first, stop=(not isv)),
                     r=[pn] + vkeys, w=[OAn[b]])
                first = False
            ndv = ND[0:64, :, osl]
            pv = OA[b][0:64, 0:256].rearrange("p (t q) -> p t q", t=2)
            if pi == 0:
                s.op("dve", lambda g, ndv=ndv, pv=pv: g.tensor_copy(out=ndv, in_=pv), r=[OAn[b]], w=["ND"])
            else:
                s.op("dve", lambda g, ndv=ndv, pv=pv: g.tensor_tensor(out=ndv, in0=ndv, in1=pv, op=ALU.add), r=[OAn[b], "ND"], w=["ND"])

        nu = len(units)
        a_qk(units[0], 0)
        a_qk(units[1], 1)
        for i in range(nu):
            if i + 2 < nu:
                a_qk(units[i + 2], i + 2)
            a_rest(units[i], i)
        s.op("dve", lambda g: g.reciprocal(out=ND[0:64, 1, :], in_=ND[0:64, 1, :]), r=["ND"], w=["ND"])
        s.op("dve", lambda g: g.tensor_tensor(out=oa[0:64, :], in0=ND[0:64, 0, :], in1=ND[0:64, 1, :], op=ALU.mult), r=["ND"], w=["oa"])
        s.dma(oT[0:64, s8 * 2048:(s8 + 1) * 2048], oa[0:64, :], r=["oa"], is_out=True)
    s.finish()
    return nc


def build_B1():
    nc = bass.Bass("TRN2", target_bir_lowering=False)
    NT = TPC // 128
    mTt = _din(nc, "mTt", [NT * 128, KD * 128], BF16)
    woP = _din(nc, "woP", [128, KD * D], F32)
    x = _din(nc, "x", [TPC, D], F32)
    gta = _din(nc, "gta", [128, D], F32)
    gml = _din(nc, "gml", [128, D], F32)
    scm = _din(nc, "scm", [128, D], F32)
    shm = _din(nc, "shm", [128, D], F32)
    wrP = _din(nc, "wrP", [128, KD * 16], F32)
    brb = _din(nc, "brb", [128, 16], F32)
    ident = _din(nc, "ident", [128, 128], F32)
    x1o = _dout(nc, "x1o", [TPC, D], F32)
    h2o = _dout(nc, "h2o", [TPC, D], BF16)
    wto = _dout(nc, "wto", [TPC, 16], F32)
    s = Sched(nc)
    A = nc.alloc_sbuf_tensor
    wo = A("wo", [128, KD, D], BF16)
    gtat, at, sht = A("gtat", [128, D], F32), A("at", [128, D], F32), A("sht", [128, D], F32)
    gmt = A("gmt", [128, D], F32)
    wr = A("wr", [128, KD, 16], F32)
    br = A("br", [128, 16], F32)
    idt = A("idt", [128, 128], F32)
    mt = [A(f"mt{i}", [128, KD, 128], BF16) for i in range(2)]
    xt = [A(f"xt{i}", [128, D], F32) for i in range(2)]
    x1t = [A(f"x1t{i}", [128, D], F32) for i in range(2)]
    h2f = A("h2f", [128, D], F32)
    h2b = [A(f"h2b{i}", [128, D], BF16) for i in range(2)]
    h2T = A("h2T", [128, KD, 128], F32)
    junk = A("junk", [128, D], F32)
    small = A("small", [128, 8], F32)
    rt = {n: A(f"rt_{n}", [128, 16], F32) for n in ("s", "sgb", "p1", "p2", "c1", "c2", "c3", "w")}
    sg2 = A("sg2", [128, 4, 8], F32)
    g4 = {n: A(f"g4_{n}", [128, 4], F32) for n in ("a", "b", "sel")}
    wtt = [A(f"wtt{i}", [128, 16], F32) for i in range(2)]
    P = [nc.alloc_psum_tensor(f"P{i}", [128, 512], F32) for i in range(8)]

    woP3 = woP.rearrange("p (k n) -> p k n", k=KD)
    for cc in range(4):
        s.dma(wo[:, :, cc * 512:(cc + 1) * 512], woP3[:, :, cc * 512:(cc + 1) * 512], w=[f"wo{cc}"], q="pool")
    for (tile_, src, nm) in ((gtat, gta, "gtat"), (gmt, gml, "gmt"), (at, scm, "at"), (sht, shm, "sht")):
        s.dma(tile_[:, :], src, w=[nm])
    s.dma(wr[:, :, :].rearrange("p k e -> p (k e)"), wrP, w=["wr"])
    s.dma(br[:, :], brb, w=["br"])
    s.dma(idt[:, :], ident, w=["idt"])
    s.op("dve", lambda g: g.tensor_scalar(out=at[:, :], in0=at[:, :], scalar1=1.0, scalar2=None, op0=ALU.add), r=["at"], w=["at"])
    s.op("dve", lambda g: g.tensor_tensor(out=at[:, :], in0=at[:, :], in1=gmt[:, :], op=ALU.mult), r=["at", "gmt"], w=["at"])
    v44 = lambda ap: ap.rearrange("p (g e) -> p g e", g=4)
    h2fb = [h2f, A("h2f1", [128, D], F32)]

    mt3 = mt + [A("mt2", [128, KD, 128], BF16)]
    xt3 = xt + [A("xt2", [128, D], F32)]

    def loads(t):
        b3 = t % 3
        rows = slice(t * 128, (t + 1) * 128)
        s.dma(mt3[b3][:, :, :].rearrange("p k t -> p (k t)"), mTt[rows, :], w=[f"mt{b3}"])
        s.dma(xt3[b3][:, :], x[rows, :], w=[f"xt{b3}"])

    def stage1(t):
        b = t % 2
        b3 = t % 3
        rows = slice(t * 128, (t + 1) * 128)
        hf, hfn = h2fb[b], f"h2f{b}"
        for cc in range(4):
            cs = slice(cc * 512, (cc + 1) * 512)
            for k in range(KD):
                s.op("pe", lambda g, cc=cc, k=k, b3=b3, cs=cs: g.matmul(P[cc][:, :], lhsT=mt3[b3][:, k, :], rhs=wo[:, k, cs],
                                                                        start=(k == 0), stop=(k == KD - 1)), r=[f"mt{b3}", f"wo{cc}"], w=[f"P{cc}"])
            s.op("dve", lambda g, cc=cc, b=b, cs=cs: g.tensor_tensor(out=x1t[b][:, cs], in0=P[cc][:, :], in1=gtat[:, cs], op=ALU.mult),
                 r=[f"P{cc}", "gtat"], w=[f"x1t{b}"])
            s.op("pool", lambda g, b=b, b3=b3, cs=cs: g.tensor_tensor(out=x1t[b][:, cs], in0=x1t[b][:, cs], in1=xt3[b3][:, cs], op=ALU.add),
                 r=[f"x1t{b}", f"xt{b3}"], w=[f"x1t{b}"])
        s.dma(x1o[rows, :], x1t[b][:, :], r=[f"x1t{b}"], is_out=True)
        s.op("act", lambda g, b=b: g.activation(out=junk[:, :], in_=x1t[b][:, :], func=AF.Square, accum_out=small[:, 0:1]),
             r=[f"x1t{b}"], w=["junk", "ss"])
        _rms_rstd(s, nc, small[:, 0:1], small[:, 1:2], small[:, 2:3], "ss", "rstd", D)
        s.op("dve", lambda g, b=b, hf=hf: g.scalar_tensor_tensor(out=hf[:, :], in0=x1t[b][:, :], scalar=small[:, 1:2], in1=at[:, :], op0=ALU.mult, op1=ALU.mult),
             r=[f"x1t{b}", "rstd", "at"], w=[hfn])
        s.op("pool", lambda g, hf=hf: g.tensor_tensor(out=hf[:, :], in0=hf[:, :], in1=sht[:, :], op=ALU.add), r=[hfn, "sht"], w=[hfn])
        s.op("act", lambda g, b=b, hf=hf: g.copy(out=h2b[b][:, :], in_=hf[:, :]), r=[hfn], w=[f"h2b{b}"])
        s.dma(h2o[rows, :], h2b[b][:, :], r=[f"h2b{b}"], is_out=True)

    def stage2(t):
        b = t % 2
        rows = slice(t * 128, (t + 1) * 128)
        hf, hfn = h2fb[b], f"h2f{b}"
        for k4 in range(4):
            pb, pbn = P[4 + k4], f"P{4 + k4}"
            for kk in range(4):
                k = k4 * 4 + kk
                s.op("pe", lambda g, pb=pb, kk=kk, k=k, hf=hf: g.transpose(out=pb[:, kk * 128:(kk + 1) * 128], in_=hf[:, k * 128:(k + 1) * 128], identity=idt[:, :]),
                     r=[hfn, "idt"], w=[pbn])
            dst = h2T[:, k4 * 4:(k4 + 1) * 4, :]
            srcv = pb[:, :].rearrange("p (k t) -> p k t", k=4)
            s.op("act", lambda g, dst=dst, srcv=srcv: g.copy(out=dst, in_=srcv), r=[pbn], w=["h2T"])
        for k in range(KD):
            s.op("pe", lambda g, k=k: g.matmul(P[4][:, 0:16], lhsT=h2T[:, k, :], rhs=wr[:, k, :], start=(k == 0), stop=(k == KD - 1)),
                 r=["h2T", "wr"], w=["P4"])
        R = rt
        s.op("act", lambda g: g.activation(out=R["s"][:, :], in_=P[4][:, 0:16], func=AF.Sigmoid), r=["P4"], w=["r_s"])
        s.op("dve", lambda g: g.tensor_tensor(out=R["sgb"][:, :], in0=R["s"][:, :], in1=br[:, :], op=ALU.add), r=["r_s", "br"], w=["r_sgb"])
        s.op("dve", lambda g: g.tensor_copy(out=sg2[:, :, 0:4], in_=v44(R["sgb"][:, :])), r=["r_sgb"], w=["sg2"])
        s.op("dve", lambda g: g.tensor_copy(out=sg2[:, :, 4:8], in_=v44(R["sgb"][:, :])), r=["r_sgb", "sg2"], w=["sg2"])
        s.op("dve", lambda g: g.tensor_tensor(out=v44(R["p1"][:, :]), in0=sg2[:, :, 0:4], in1=sg2[:, :, 1:5], op=ALU.add), r=["sg2"], w=["r_p1"])
        s.op("dve", lambda g: g.tensor_tensor(out=v44(R["p2"][:, :]), in0=sg2[:, :, 0:4], in1=sg2[:, :, 2:6], op=ALU.add), r=["sg2"], w=["r_p2"])
        s.op("dve", lambda g: g.tensor_tensor(out=R["p1"][:, :], in0=R["p1"][:, :], in1=R["p2"][:, :], op=ALU.max), r=["r_p1", "r_p2"], w=["r_p1"])
        s.op("dve", lambda g: g.tensor_reduce(out=g4["a"][:, :], in_=v44(R["p1"][:, :]), axis=AX.X, op=ALU.max), r=["r_p1"], w=["g4a"])
        s.op("dve", lambda g: g.tensor_reduce(out=small[:, 3:4], in_=g4["a"][:, :], axis=AX.X, op=ALU.max), r=["g4a"], w=["gmax"])
        s.op("dve", lambda g: g.tensor_scalar(out=g4["sel"][:, :], in0=g4["a"][:, :], scalar1=small[:, 3:4], scalar2=None, op0=ALU.is_ge),
             r=["g4a", "gmax"], w=["g4sel"])
        for r_, cn in ((1, "c1"), (2, "c2"), (3, "c3")):
            s.op("dve", lambda g, r_=r_, cn=cn: g.tensor_tensor(out=v44(R[cn][:, :]), in0=sg2[:, :, r_:r_ + 4], in1=sg2[:, :, 0:4], op=ALU.is_gt),
                 r=["sg2"], w=[f"r_{cn}"])
        s.op("dve", lambda g: g.tensor_tensor(out=R["c1"][:, :], in0=R["c1"][:, :], in1=R["c2"][:, :], op=ALU.add), r=["r_c1", "r_c2"], w=["r_c1"])
        s.op("dve", lambda g: g.tensor_tensor(out=R["c1"][:, :], in0=R["c1"][:, :], in1=R["c3"][:, :], op=ALU.add), r=["r_c1", "r_c3"], w=["r_c1"])
        s.op("dve", lambda g: g.tensor_scalar(out=R["c1"][:, :], in0=R["c1"][:, :], scalar1=1.5, scalar2=None, op0=ALU.is_lt), r=["r_c1"], w=["r_c1"])
        s.op("dve", lambda g: g.tensor_tensor(out=v44(R["c1"][:, :]), in0=v44(R["c1"][:, :]), in1=g4["sel"][:, :].unsqueeze(2).broadcast_to([128, 4, 4]), op=ALU.mult),
             r=["r_c1", "g4sel"], w=["r_c1"])
        s.op("dve", lambda g: g.tensor_tensor(out=R["w"][:, :], in0=R["s"][:, :], in1=R["c1"][:, :], op=ALU.mult), r=["r_s", "r_c1"], w=["r_w"])
        s.op("dve", lambda g: g.tensor_reduce(out=small[:, 4:5], in_=R["w"][:, :], axis=AX.X, op=ALU.add), r=["r_w"], w=["wsum"])
        s.op("dve", lambda g: g.reciprocal(out=small[:, 5:6], in_=small[:, 4:5]), r=["wsum"], w=["rws"])
        s.op("dve", lambda g, b=b: g.tensor_scalar(out=wtt[b][:, :], in0=R["w"][:, :], scalar1=small[:, 5:6], scalar2=None, op0=ALU.mult),
             r=["r_w", "rws"], w=[f"wtt{b}"])
        s.dma(wto[rows, :], wtt[b][:, :], r=[f"wtt{b}"], is_out=True)

    loads(0)
    loads(1)
    stage1(0)
    for t in range(NT):
        if t + 2 < NT:
            loads(t + 2)
        if t + 1 < NT:
            stage1(t + 1)
        stage2(t)
    s.finish()
    return nc


TB = 1024
NTB = 8
DE = 1024


def build_B2():
    nc = bass.Bass("TRN2", target_bir_lowering=False)
    h2T = _din(nc, "h2T", [NTB * 128, KD * TB], BF16)
    wgP = _din(nc, "wgP", [4 * 8 * 128, KD * 128], F32)
    wuP = _din(nc, "wuP", [4 * 8 * 128, KD * 128], F32)
    wdP = _din(nc, "wdP", [4 * 128, 8 * D], F32)
    wtP = _din(nc, "wtP", [128, 64 * 4], F32)
    yo = _dout(nc, "yo", [NTB * TB, D], BF16)
    s = Sched(nc)
    A = nc.alloc_sbuf_tensor
    HB = A("HB", [128, KD, TB], BF16)
    WD = A("WD", [128, 8, D], BF16)
    WG = [A(f"WG{i}", [128, KD, 128], BF16) for i in range(2)]
    WU = [A(f"WU{i}", [128, KD, 128], BF16) for i in range(2)]
    actT = A("actT", [128, 8, TB], BF16)
    acc = A("acc", [128, TB // 128, D], F32)
    sg = [A(f"sg{i}", [128, 512], F32) for i in range(2)]
    yb = [A(f"yb{i}", [128, D], BF16) for i in range(2)]
    wt = A("wt", [128, 64, 4], F32)
    P = [nc.alloc_psum_tensor(f"P{i}", [128, 512], F32) for i in range(8)]
    s.dma(wt[:, :, :].rearrange("p t e -> p (t e)"), wtP, w=["wt"])
    ig = 0
    iy = 0
    s.dma(HB[:, :, :].rearrange("p k t -> p (k t)"), h2T[0:128, :], w=["HB"])
    for blk in range(NTB):
        for e in range(4):
            for f in range(8):
                fb = f % 2
                r0 = (e * 8 + f) * 128
                s.dma(WG[fb][:, :, :].rearrange("p k c -> p (k c)"), wgP[r0:r0 + 128, :], w=[f"WG{fb}"], q="pool")
                s.dma(WU[fb][:, :, :].rearrange("p k c -> p (k c)"), wuP[r0:r0 + 128, :], w=[f"WU{fb}"], q="pool")
                if f == 1:
                    s.dma(WD[:, :, :].rearrange("p f n -> p (f n)"), wdP[e * 128:(e + 1) * 128, :], w=["WD"], q="pool")
                for ch in range(TB // 512):
                    cs = slice(ch * 512, (ch + 1) * 512)
                    pg, pu = P[ig % 2], P[2 + ig % 2]
                    pgn, pun = f"P{ig % 2}", f"P{2 + ig % 2}"
                    for k in range(KD):
                        s.op("pe", lambda g, pg=pg, k=k, fb=fb, cs=cs: g.matmul(pg[:, :], lhsT=WG[fb][:, k, :], rhs=HB[:, k, cs], start=(k == 0), stop=(k == KD - 1)),
                             r=[f"WG{fb}", "HB"], w=[pgn])
                    for k in range(KD):
                        s.op("pe", lambda g, pu=pu, k=k, fb=fb, cs=cs: g.matmul(pu[:, :], lhsT=WU[fb][:, k, :], rhs=HB[:, k, cs], start=(k == 0), stop=(k == KD - 1)),
                             r=[f"WU{fb}", "HB"], w=[pun])
                    sgt, sgn = sg[ig % 2], f"sg{ig % 2}"
                    s.op("act", lambda g, pg=pg, sgt=sgt: g.activation(out=sgt[:, :], in_=pg[:, :], func=AF.Silu), r=[pgn], w=[sgn])
                    s.op("dve", lambda g, pu=pu, sgt=sgt, f=f, cs=cs: g.tensor_tensor(out=actT[:, f, cs], in0=pu[:, :], in1=sgt[:, :], op=ALU.mult),
                         r=[pun, sgn], w=["actT"])
                    ig += 1
            if e == 3 and blk + 1 < NTB:
                s.dma(HB[:, :, :].rearrange("p k t -> p (k t)"), h2T[(blk + 1) * 128:(blk + 2) * 128, :], w=["HB"])
            for tt in range(TB // 128):
                tile_i = blk * (TB // 128) + tt
                for cc in range(4):
                    cs = slice(cc * 512, (cc + 1) * 512)
                    py, pyn = P[4 + iy % 4], f"P{4 + iy % 4}"
                    for f in range(8):
                        s.op("pe", lambda g, py=py, f=f, tt=tt, cs=cs: g.matmul(py[:, :], lhsT=actT[:, f, tt * 128:(tt + 1) * 128], rhs=WD[:, f, cs],
                                                                               start=(f == 0), stop=(f == 7)), r=["actT", "WD"], w=[pyn])
                    wsc = wt[:, tile_i, e:e + 1]
                    if e == 0:
                        s.op("dve", lambda g, py=py, tt=tt, cs=cs, wsc=wsc: g.tensor_scalar(out=acc[:, tt, cs], in0=py[:, :], scalar1=wsc, scalar2=None, op0=ALU.mult),
                             r=[pyn, "wt"], w=[f"acc{tt}"])
                    else:
                        s.op("dve", lambda g, py=py, tt=tt, cs=cs, wsc=wsc: g.scalar_tensor_tensor(out=acc[:, tt, cs], in0=py[:, :], scalar=wsc, in1=acc[:, tt, cs],
                                                                                                  op0=ALU.mult, op1=ALU.add),
                             r=[pyn, "wt", f"acc{tt}"], w=[f"acc{tt}"])
                    iy += 1
        for tt in range(TB // 128):
            b = tt % 2
            s.op("act", lambda g, b=b, tt=tt: g.copy(out=yb[b][:, :], in_=acc[:, tt, :]), r=[f"acc{tt}"], w=[f"yb{b}"])
            r0 = blk * TB + tt * 128
            s.dma(yo[r0:r0 + 128, :], yb[b][:, :], r=[f"yb{b}"], is_out=True)
    s.finish()
    return nc


def build_F():
    nc = bass.Bass("TRN2", target_bir_lowering=False)
    x = _din(nc, "x", [TPC, D], F32)
    ys = [_din(nc, f"y{g}", [TPC, D], BF16) for g in range(4)]
    gtm = _din(nc, "gtm", [128, D], F32)
    gfin = _din(nc, "gfin", [128, D], F32)
    out = _dout(nc, "out", [TPC, D], F32)
    s = Sched(nc)
    A = nc.alloc_sbuf_tensor
    xt = [A(f"xt{i}", [128, D], F32) for i in range(3)]
    yt = [[A(f"yt{g}{i}", [128, D], BF16) for i in range(3)] for g in range(4)]
    gtt, gft = A("gtt", [128, D], F32), A("gft", [128, D], F32)
    t1 = A("t1", [128, D], F32)
    junk = A("junk", [128, D], F32)
    small = A("small", [128, 8], F32)
    s.dma(gtt[:, :], gtm, w=["gtt"])
    s.dma(gft[:, :], gfin, w=["gft"])
    NT = TPC // 128

    def loads(t):
        b = t % 3
        rows = slice(t * 128, (t + 1) * 128)
        s.dma(xt[b][:, :], x[rows, :], w=[f"xt{b}"])
        for g in range(4):
            s.dma(yt[g][b][:, :], ys[g][rows, :], w=[f"yt{g}{b}"])

    loads(0)
    loads(1)
    for t in range(NT):
        if t + 2 < NT:
            loads(t + 2)
        b = t % 3
        rows = slice(t * 128, (t + 1) * 128)
        s.op("pool", lambda e, b=b: e.tensor_tensor(out=yt[0][b][:, :], in0=yt[0][b][:, :], in1=yt[1][b][:, :], op=ALU.add), r=[f"yt0{b}", f"yt1{b}"], w=[f"yt0{b}"])
        s.op("pool", lambda e, b=b: e.tensor_tensor(out=yt[2][b][:, :], in0=yt[2][b][:, :], in1=yt[3][b][:, :], op=ALU.add), r=[f"yt2{b}", f"yt3{b}"], w=[f"yt2{b}"])
        s.op("dve", lambda e, b=b: e.tensor_tensor(out=t1[:, :], in0=yt[0][b][:, :], in1=yt[2][b][:, :], op=ALU.add), r=[f"yt0{b}", f"yt2{b}"], w=["t1"])
        s.op("dve", lambda e: e.tensor_tensor(out=t1[:, :], in0=t1[:, :], in1=gtt[:, :], op=ALU.mult), r=["t1", "gtt"], w=["t1"])
        s.op("dve", lambda e, b=b: e.tensor_tensor(out=xt[b][:, :], in0=xt[b][:, :], in1=t1[:, :], op=ALU.add), r=["t1", f"xt{b}"], w=[f"xt{b}"])
        s.op("act", lambda e, b=b: e.activation(out=junk[:, :], in_=xt[b][:, :], func=AF.Square, accum_out=small[:, 0:1]), r=[f"xt{b}"], w=["junk", "ss"])
        _rms_rstd(s, nc, small[:, 0:1], small[:, 1:2], small[:, 2:3], "ss", "rstd", D)
        s.op("dve", lambda e, b=b: e.scalar_tensor_tensor(out=xt[b][:, :], in0=xt[b][:, :], scalar=small[:, 1:2], in1=gft[:, :], op0=ALU.mult, op1=ALU.mult),
             r=[f"xt{b}", "rstd", "gft"], w=[f"xt{b}"])
        s.dma(out[rows, :], xt[b][:, :], r=[f"xt{b}"], is_out=True)
    s.finish()
    return nc


_PROGS = {}


def _prog(name, fn, *a):
    key = (name,) + tuple(a)
    if key not in _PROGS:
        _PROGS[key] = fn(*a)
    return _PROGS[key]


def _run(nc, in_maps):
    res = run_bass_kernel_spmd(nc, in_maps, core_ids=list(range(NCORE)))
    return res.results


def _pk(v):
    return np.ascontiguousarray(np.asarray(v, np.float32).reshape(KD, 128).T)


def _bc(v):
    v = np.asarray(v, np.float32).reshape(1, -1)
    return np.ascontiguousarray(np.broadcast_to(v, (128, v.shape[1])))


def _colidx():
    idx = []
    for h in range(8):
        qa = list(range(0 + h * 64, 0 + h * 64 + 64)); ka = [512 + c for c in qa]; va = [1024 + c for c in qa]
        qb = [1536 + c for c in qa]; kb = [2048 + c for c in qa]; vb = [2560 + c for c in qa]
        qc = list(range(3072 + h * 128, 3072 + h * 128 + 128)); kc = [1024 + c for c in qc]; vc = [2048 + c for c in qc]
        idx += qa + qb + ka + kb + qc + kc + va + vb + vc
    return np.asarray(idx)


def _consts():
    inv = (10000.0 ** (-np.arange(0, 64, 2, dtype=np.float32) / 64)).astype(np.float32)
    invf = np.tile(inv, 4).reshape(128, 1).astype(np.float32)
    perm = np.zeros((128, 128), np.float32)
    for m in range(128):
        if m % 64 < 32:
            perm[m + 32, m] = -1.0
        else:
            perm[m - 32, m] = 1.0
    bones = np.zeros((128, 128), np.float32)
    bones[:64, :64] = 1.0
    bones[64:, 64:] = 1.0
    return dict(invf=invf, perm=perm, ident=np.eye(128, dtype=np.float32), bones=bones)


def host_M(inp):
    nc = _prog("M", build_M)
    cT = _pk(inp["c"].reshape(-1))
    maps = []
    for i in range(NCORE):
        cols = slice(i * MCOL, (i + 1) * MCOL)
        wa = np.concatenate([inp["w_ada"][0][:, cols], inp["w_ada"][1][:, cols]], axis=0)
        ba = np.concatenate([inp["b_ada"][0][cols], inp["b_ada"][1][cols]]).reshape(1, -1)
        maps.append(dict(cT=cT, wada=np.ascontiguousarray(wa), bada=np.ascontiguousarray(ba)))
    res = _run(nc, maps)
    mod = np.zeros((2, 6 * D), np.float32)
    for i in range(NCORE):
        r = res[i]["mod"].reshape(2, MCOL)
        mod[:, i * MCOL:(i + 1) * MCOL] = r
    return mod


def host_PA1(inp, l, mod, x, ys=None):
    add = ys is not None
    nc = _prog("PA1", build_PA1, add)
    cst = _consts()
    sh_a, sc_a = mod[l, 0:D], mod[l, D:2 * D]
    wperm = inp["w_in"][l][:, _colidx()]
    wl = np.ascontiguousarray(wperm.reshape(KD, 128, NG, 128).transpose(2, 1, 0, 3)).reshape(NG * 128, KD * 128)
    pos = np.asarray(inp["positions"]).reshape(-1).astype(np.int32)
    maps = []
    for i in range(NCORE):
        rows = slice(i * TPC, (i + 1) * TPC)
        m = dict(x=np.ascontiguousarray(x[rows]), win=wl, gA=_pk(inp["g_attn"][l]), scA=_pk(sc_a), shA=_pk(sh_a),
                 pos=np.ascontiguousarray(pos[rows].reshape(1, -1)), **cst)
        if add:
            gt_m_prev = mod[l - 1, 5 * D:6 * D]
            for g in range(4):
                m[f"y{g}"] = np.ascontiguousarray(ys[g][rows])
            m["gtm"] = _bc(gt_m_prev)
        maps.append(m)
    res = _run(nc, maps)
    qkvT = np.concatenate([res[i]["qkvT"] for i in range(NCORE)], axis=1)
    nrm = np.stack([res[i]["nrm"] for i in range(NCORE)], axis=0)
    xnew = np.concatenate([res[i]["xnew"] for i in range(NCORE)], axis=0) if add else x
    return qkvT, nrm, xnew


def _a2_consts():
    k = np.arange(128)[:, None]
    q = np.arange(128)[None, :]
    mask2 = np.concatenate([(k >= q), (k <= q)], axis=1).astype(np.float32)
    E = (np.arange(S)[None, :] // 256 == np.arange(64)[:, None]).astype(NPBF)
    return mask2, E


def _tiles(vT):
    d = vT.shape[0]
    return np.ascontiguousarray(vT.T.reshape(S // 128, 128, d).transpose(1, 0, 2)).reshape(128, -1)


def host_A2(inp, l, qkvT, nrm):
    nc = _prog("A2", build_A2)
    mask2, E = _a2_consts()
    lam_init = 0.8 - 0.6 * math.exp(-0.3 * l)
    lamt = np.concatenate([_bc(inp["lam_q1"][l]), _bc(inp["lam_k1"][l]), _bc(inp["lam_q2"][l]), _bc(inp["lam_k2"][l])], axis=1)
    lamc = _bc(np.asarray([lam_init, 1.0 - lam_init], np.float32))
    gsub = np.asarray(inp["g_subln"][l], np.float32).reshape(128, 1)
    ones64 = np.ones((64, S), NPBF)
    maps = []
    for h in range(NCORE):
        G = lambda j: qkvT[(h * 6 + j) * 128:(h * 6 + j + 1) * 128]
        vab = G(4)
        va = vab[0:64]
        vas = []
        for d in (1, 4, 16):
            perm_tok = np.arange(S).reshape(S // d, d).T.reshape(-1)
            vas.append(_tiles(va[:, perm_tok]))
        nb = np.zeros((8, 8), np.float32)
        for j in range(4):
            nb[2 * j] = nrm[:, 0, h * 4 + j]
            nb[2 * j + 1] = nrm[:, 64, h * 4 + j]
        maps.append(dict(qab=np.ascontiguousarray(G(0)), kab=np.ascontiguousarray(G(1)), qcT=np.ascontiguousarray(G(2)),
                         kcT=np.ascontiguousarray(G(3)), vcP=_tiles(G(5)), vbP=_tiles(np.concatenate([vab[64:128], ones64], axis=0)),
                         vaP=np.ascontiguousarray(np.concatenate(vas, axis=1)), Ecs=E, nrmb=_bc(nb.reshape(-1)),
                         lamt=lamt, lamc=lamc, gsub=gsub, mask2=mask2, ident=np.eye(128, dtype=np.float32)))
    res = _run(nc, maps)
    return [res[h]["oT"] for h in range(NCORE)]


def _rowidx_out():
    idx = []
    for h in range(8):
        idx += list(range(h * 64, h * 64 + 64)) + list(range(512 + h * 64, 512 + h * 64 + 64)) + list(range(1024 + h * 128, 1024 + h * 128 + 128))
    return np.asarray(idx)


def host_B1(inp, l, mod, x, oT):
    nc = _prog("B1", build_B1)
    mT = np.concatenate(oT, axis=0)
    wo = inp["w_out"][l][_rowidx_out(), :]
    woP = np.ascontiguousarray(wo.reshape(KD, 128, D).transpose(1, 0, 2)).reshape(128, KD * D)
    wrP = np.ascontiguousarray(np.asarray(inp["w_router"], np.float32).reshape(KD, 128, 16).transpose(1, 0, 2)).reshape(128, KD * 16)
    com = dict(woP=woP, gta=_bc(mod[l, 2 * D:3 * D]), gml=_bc(inp["g_mlp"][l]), scm=_bc(mod[l, 4 * D:5 * D]), shm=_bc(mod[l, 3 * D:4 * D]),
               wrP=wrP, brb=_bc(inp["b_router"]), ident=np.eye(128, dtype=np.float32))
    maps = []
    for i in range(NCORE):
        rows = slice(i * TPC, (i + 1) * TPC)
        mc = mT[:, rows].reshape(KD, 128, TPC // 128, 128).transpose(2, 1, 0, 3)
        maps.append(dict(mTt=np.ascontiguousarray(mc).reshape(TPC, KD * 128), x=np.ascontiguousarray(x[rows]), **com))
    res = _run(nc, maps)
    cat = lambda n: np.concatenate([res[i][n] for i in range(NCORE)], axis=0)
    return cat("x1o"), cat("h2o"), cat("wto")


def host_B2(inp, l, h2, wt):
    nc = _prog("B2", build_B2)
    HALF = S // 2
    maps = []
    for i in range(NCORE):
        t2, g = divmod(i, 4)
        rows = slice(t2 * HALF, (t2 + 1) * HALF)
        hh = h2[rows].reshape(NTB, TB, KD, 128).transpose(0, 3, 2, 1)
        wg = np.stack([inp["w_gate"][l][4 * g + e].reshape(KD, 128, 8, 128).transpose(2, 1, 0, 3) for e in range(4)])
        wu = np.stack([inp["w_up"][l][4 * g + e].reshape(KD, 128, 8, 128).transpose(2, 1, 0, 3) for e in range(4)])
        wd = np.stack([inp["w_down"][l][4 * g + e].reshape(8, 128, D).transpose(1, 0, 2) for e in range(4)])
        wtp = wt[rows, 4 * g:4 * g + 4].reshape(HALF // 128, 128, 4).transpose(1, 0, 2)
        maps.append(dict(h2T=np.ascontiguousarray(hh).reshape(NTB * 128, KD * TB),
                         wgP=np.ascontiguousarray(wg).reshape(4 * 8 * 128, KD * 128),
                         wuP=np.ascontiguousarray(wu).reshape(4 * 8 * 128, KD * 128),
                         wdP=np.ascontiguousarray(wd).reshape(4 * 128, 8 * D),
                         wtP=np.ascontiguousarray(wtp).reshape(128, -1)))
    res = _run(nc, maps)
    ys = []
    for g in range(4):
        ys.append(np.concatenate([res[0 * 4 + g]["yo"], res[1 * 4 + g]["yo"]], axis=0))
    return ys


def host_F(inp, mod, x1, ys):
    nc = _prog("F", build_F)
    com = dict(gtm=_bc(mod[1, 5 * D:6 * D]), gfin=_bc(inp["g_final"]))
    maps = []
    for i in range(NCORE):
        rows = slice(i * TPC, (i + 1) * TPC)
        m = dict(x=np.ascontiguousarray(x1[rows]), **com)
        for g in range(4):
            m[f"y{g}"] = np.ascontiguousarray(ys[g][rows])
        maps.append(m)
    res = _run(nc, maps)
    return np.concatenate([res[i]["out"] for i in range(NCORE)], axis=0)


def kernel(**inputs):
    inp = {k: np.asarray(v) for k, v in inputs.items()}
    mod = host_M(inp)
    x = np.asarray(inp["x"], np.float32).reshape(S, D)
    ys = None
    x1 = None
    for l in range(2):
        qkvT, nrm, x = host_PA1(inp, l, mod, x if l == 0 else x1, ys)
        oT = host_A2(inp, l, qkvT, nrm)
        x1, h2, wt = host_B1(inp, l, mod, x, oT)
        ys = host_B2(inp, l, h2, wt)
    out = host_F(inp, mod, x1, ys)
    return out.reshape(1, S, D).astype(np.float32)
```
